# Optimizing a Trainium2 kernel written in Bass

```python
import math
import jax, jax.numpy as jnp
from jax import lax
import numpy as np

D_MODEL = 2048
BATCH = 1
SEQ = 8192
DEPTH = 2

N_EVEN = (DEPTH + 1) // 2
N_ODD = DEPTH // 2

S5_WIDTH = D_MODEL // 2
S5_GROUP = 16
S5_GROUPS = S5_WIDTH // S5_GROUP
S5_STATE = 64
S5_DT_MIN = 1e-3
S5_DT_MAX = 1e-1
S5_MAX_RE = -1e-4

GLA_HEADS = 4
GLA_DV = (D_MODEL // 2) // GLA_HEADS
GLA_DK = GLA_DV // 2
GLA_GATE_RANK = 16
GLA_GATE_TEMP = 16.0
GLA_CHUNK = 16
GLA_EPS = 1e-6

GLA_QK = GLA_HEADS * GLA_DK
GLA_V = GLA_HEADS * GLA_DV
AB_IN_WIDTH = S5_WIDTH + 2 * GLA_QK + 2 * GLA_V + GLA_GATE_RANK
AB_OUT_WIDTH = S5_WIDTH + GLA_V

ATT_HEADS = 16
ATT_HEAD_DIM = D_MODEL // ATT_HEADS
DILATED_GROUPS = ((128, 1), (512, 4), (2048, 16))
MAX_WINDOW = 2048
ATT_BLOCK = 128

N_EXPERTS = 32
TOP_K = 4
D_FF = D_MODEL
SWIGLU_LIMIT = 7.0
SWIGLU_ALPHA = 1.702
MOE_BLOCK = 128

DN_ALPHA = (2 * DEPTH) ** 0.25
DN_BETA = (8 * DEPTH) ** -0.25
LN_EPS = 1e-5

kernel_name = "hybrid_s5_gla_dilated_moe_deepnorm"


def layer_norm(x, g, b):
    xf = x.astype(jnp.float32)
    mu = jnp.mean(xf, axis=-1, keepdims=True)
    var = jnp.mean(jnp.square(xf - mu), axis=-1, keepdims=True)
    return ((xf - mu) * lax.rsqrt(var + LN_EPS) * g + b).astype(x.dtype)


def s5_mixer(u, lam_re, lam_im, log_step, b_re, b_im, c_re, c_im, d_skip, w_glu, b_glu):
    f32 = jnp.float32
    bsz, L, _ = u.shape
    uf = u.astype(f32)
    ug = uf.reshape(bsz, L, S5_GROUPS, S5_GROUP)
    lr = jnp.minimum(lam_re.astype(f32), S5_MAX_RE)
    li = lam_im.astype(f32)
    dt = jnp.exp(log_step.astype(f32))[:, None]
    mag = jnp.exp(lr * dt)
    ab_re = mag * jnp.cos(li * dt)
    ab_im = mag * jnp.sin(li * dt)
    den = lr * lr + li * li
    nr = ab_re - 1.0
    f_re = (nr * lr + ab_im * li) / den
    f_im = (ab_im * lr - nr * li) / den
    br = b_re.astype(f32)
    bi = b_im.astype(f32)
    bb_re = f_re[..., None] * br - f_im[..., None] * bi
    bb_im = f_re[..., None] * bi + f_im[..., None] * br
    x_re = jnp.einsum('blgh,gph->blgp', ug, bb_re)
    x_im = jnp.einsum('blgh,gph->blgp', ug, bb_im)
    a_re = jnp.broadcast_to(ab_re, x_re.shape)
    a_im = jnp.broadcast_to(ab_im, x_im.shape)

    def combine(left, right):
        a1r, a1i, b1r, b1i = left
        a2r, a2i, b2r, b2i = right
        return (a2r * a1r - a2i * a1i,
                a2r * a1i + a2i * a1r,
                a2r * b1r - a2i * b1i + b2r,
                a2r * b1i + a2i * b1r + b2i)

    _, _, s_re, s_im = lax.associative_scan(combine, (a_re, a_im, x_re, x_im), axis=1)
    y = (jnp.einsum('blgp,ghp->blgh', s_re, c_re.astype(f32))
         - jnp.einsum('blgp,ghp->blgh', s_im, c_im.astype(f32)))
    y = y.reshape(bsz, L, S5_WIDTH) + d_skip.astype(f32) * uf
    z = jax.nn.gelu(y)
    out = z * jax.nn.sigmoid(z @ w_glu.astype(f32) + b_glu.astype(f32))
    return out.astype(u.dtype)


def gla_mixer(q, k, v, g_low, r, w_gate2, b_gate2, norm_g):
    f32 = jnp.float32
    bsz, L, _ = q.shape
    C = GLA_CHUNK
    nC = L // C
    H, dk, dv = GLA_HEADS, GLA_DK, GLA_DV
    qf = q.astype(f32).reshape(bsz, nC, C, H, dk) * (GLA_DK ** -0.5)
    kf = k.astype(f32).reshape(bsz, nC, C, H, dk)
    vf = v.astype(f32).reshape(bsz, nC, C, H, dv)
    log_a = jax.nn.log_sigmoid(g_low.astype(f32) @ w_gate2.astype(f32) + b_gate2.astype(f32)) / GLA_GATE_TEMP
    log_a = log_a.reshape(bsz, nC, C, H, dk)
    bcum = jnp.cumsum(log_a, axis=2)
    b_last = bcum[:, :, -1]
    causal = jnp.tril(jnp.ones((C, C), dtype=bool))
    diff = bcum[:, :, :, None] - bcum[:, :, None, :]
    diff = jnp.where(causal[:, :, None, None], diff, -jnp.inf)
    att = jnp.sum(qf[:, :, :, None] * kf[:, :, None] * jnp.exp(diff), axis=-1)
    o_intra = jnp.einsum('bnijh,bnjhv->bnihv', att, vf)
    k_dec = kf * jnp.exp(b_last[:, :, None] - bcum)
    kv = jnp.einsum('bnjhk,bnjhv->bnhkv', k_dec, vf)
    decay = jnp.exp(b_last)

    def step(S, inp):
        dec, upd = inp
        return dec[..., None] * S + upd, S

    S0 = jnp.zeros((bsz, H, dk, dv), f32)
    _, S_in = lax.scan(step, S0, (jnp.moveaxis(decay, 1, 0), jnp.moveaxis(kv, 1, 0)))
    S_in = jnp.moveaxis(S_in, 0, 1)
    o_inter = jnp.einsum('bnihk,bnhkv->bnihv', qf * jnp.exp(bcum), S_in)
    o = o_intra + o_inter
    o = o * lax.rsqrt(jnp.mean(o * o, axis=-1, keepdims=True) + GLA_EPS) * norm_g.astype(f32)
    o = o.reshape(bsz, L, H * dv) * jax.nn.silu(r.astype(f32))
    return o.astype(q.dtype)


def even_mixer(x, w_in, lam_re, lam_im, log_step, b_re, b_im, c_re, c_im, d_skip, w_glu, b_glu,
               w_gate2, b_gate2, norm_g, w_out):
    h = x @ w_in
    s1 = S5_WIDTH
    s2 = s1 + GLA_QK
    s3 = s2 + GLA_QK
    s4 = s3 + GLA_V
    s5 = s4 + GLA_GATE_RANK
    u, q, k, v, g_low, r = jnp.split(h, [s1, s2, s3, s4, s5], axis=-1)
    ya = s5_mixer(u, lam_re, lam_im, log_step, b_re, b_im, c_re, c_im, d_skip, w_glu, b_glu)
    yb = gla_mixer(q, k, v, g_low, r, w_gate2, b_gate2, norm_g)
    return jnp.concatenate([ya, yb], axis=-1) @ w_out


def dilated_attention(q, k, v):
    bsz, L, H, Dh = q.shape
    kp = jnp.pad(k, ((0, 0), (MAX_WINDOW, 0), (0, 0), (0, 0)))
    vp = jnp.pad(v, ((0, 0), (MAX_WINDOW, 0), (0, 0), (0, 0)))
    nblk = L // ATT_BLOCK
    qb = jnp.moveaxis(q.reshape(bsz, nblk, ATT_BLOCK, H, Dh), 1, 0)
    starts = jnp.arange(nblk, dtype=jnp.int32) * ATT_BLOCK
    qi = np.arange(ATT_BLOCK, dtype=np.int32)
    scale = Dh ** -0.5

    def block(args):
        q_blk, start = args
        k_win = lax.dynamic_slice_in_dim(kp, start, ATT_BLOCK + MAX_WINDOW, axis=1)
        v_win = lax.dynamic_slice_in_dim(vp, start, ATT_BLOCK + MAX_WINDOW, axis=1)
        outs, lses = [], []
        for window, dil in DILATED_GROUPS:
            j = np.arange(window // dil + 1, dtype=np.int32)
            rel = (qi[:, None] - dil * j[None, :]).astype(np.int32)
            kg = k_win[:, rel + MAX_WINDOW]
            vg = v_win[:, rel + MAX_WINDOW]
            s = jnp.einsum('bqhd,bqjhd->bhqj', q_blk, kg).astype(jnp.float32) * scale
            s = jnp.where((start + rel >= 0)[None, None], s, -jnp.inf)
            lse = jax.nn.logsumexp(s, axis=-1)
            p = jnp.exp(s - lse[..., None])
            outs.append(jnp.einsum('bhqj,bqjhd->bqhd', p, vg.astype(jnp.float32)))
            lses.append(lse)
        w = jax.nn.softmax(jnp.stack(lses), axis=0)
        w = jnp.moveaxis(w, 2, 3)[..., None]
        return jnp.sum(w * jnp.stack(outs), axis=0)

    out = lax.map(block, (qb, starts))
    return jnp.moveaxis(out, 0, 1).reshape(bsz, L, H, Dh).astype(q.dtype)


def odd_mixer(x, w_qkv, w_o):
    bsz, L, _ = x.shape
    qkv = (x @ w_qkv).reshape(bsz, L, 3, ATT_HEADS, ATT_HEAD_DIM)
    y = dilated_attention(qkv[:, :, 0], qkv[:, :, 1], qkv[:, :, 2])
    return y.reshape(bsz, L, D_MODEL) @ w_o


def moe(x, w_router, b_router, w_gu, b_gu, w_down, b_down):
    bsz, L, D = x.shape
    T = bsz * L
    xt = x.reshape(T, D)
    logits = (xt @ w_router + b_router).astype(jnp.float32)
    top_val, top_idx = lax.top_k(logits, TOP_K)
    gates = jax.nn.softmax(top_val, axis=-1)
    A = T * TOP_K
    flat_e = top_idx.reshape(A).astype(jnp.int32)
    flat_t = jnp.arange(A, dtype=jnp.int32) // TOP_K
    flat_g = gates.reshape(A)
    order = jnp.argsort(flat_e)
    se, st, sg = flat_e[order], flat_t[order], flat_g[order]
    counts = jnp.bincount(flat_e, length=N_EXPERTS)
    start = jnp.cumsum(counts) - counts
    padded = (counts + MOE_BLOCK - 1) // MOE_BLOCK * MOE_BLOCK
    pend = jnp.cumsum(padded)
    pstart = pend - padded
    dest = pstart[se] + jnp.arange(A, dtype=jnp.int32) - start[se]
    P = A + N_EXPERTS * MOE_BLOCK
    n_blocks = P // MOE_BLOCK
    buf_t = jnp.zeros((P,), jnp.int32).at[dest].set(st)
    buf_g = jnp.zeros((P,), jnp.float32).at[dest].set(sg)
    blk_e = jnp.minimum(jnp.searchsorted(pend, jnp.arange(n_blocks, dtype=jnp.int32) * MOE_BLOCK, side='right'),
                        N_EXPERTS - 1).astype(jnp.int32)

    def expert_block(args):
        e, tok = args
        h = xt[tok] @ w_gu[e] + b_gu[e]
        gate = jnp.minimum(h[:, :D_FF], SWIGLU_LIMIT)
        up = jnp.clip(h[:, D_FF:], -SWIGLU_LIMIT, SWIGLU_LIMIT)
        act = (up + 1.0) * (gate * jax.nn.sigmoid(SWIGLU_ALPHA * gate))
        return act @ w_down[e] + b_down[e]

    yb = lax.map(expert_block, (blk_e, buf_t.reshape(n_blocks, MOE_BLOCK)))
    y = yb.reshape(P, D) * buf_g[:, None]
    out = jax.ops.segment_sum(y, buf_t, num_segments=T)
    return out.reshape(bsz, L, D).astype(x.dtype)


def setup_inputs(seed: int = 0) -> dict:
    key = jax.random.key(seed)
    ks = jax.random.split(key, 32)
    nrm = jax.random.normal
    f32 = jnp.float32
    lam_im0 = jnp.pi * jnp.arange(S5_STATE, dtype=f32)
    return {
        "x": nrm(ks[0], (BATCH, SEQ, D_MODEL), f32),
        "ab_w_in": nrm(ks[1], (N_EVEN, D_MODEL, AB_IN_WIDTH), f32) * D_MODEL ** -0.5,
        "s5_lam_re": -0.5 + 0.01 * nrm(ks[2], (N_EVEN, S5_GROUPS, S5_STATE), f32),
        "s5_lam_im": lam_im0 + 0.01 * nrm(ks[3], (N_EVEN, S5_GROUPS, S5_STATE), f32),
        "s5_log_step": jax.random.uniform(ks[4], (N_EVEN, S5_GROUPS), f32,
                                          minval=math.log(S5_DT_MIN), maxval=math.log(S5_DT_MAX)),
        "s5_b_re": nrm(ks[5], (N_EVEN, S5_GROUPS, S5_STATE, S5_GROUP), f32) * (2 * S5_GROUP) ** -0.5,
        "s5_b_im": nrm(ks[6], (N_EVEN, S5_GROUPS, S5_STATE, S5_GROUP), f32) * (2 * S5_GROUP) ** -0.5,
        "s5_c_re": nrm(ks[7], (N_EVEN, S5_GROUPS, S5_GROUP, S5_STATE), f32) * (2 * S5_STATE) ** -0.5,
        "s5_c_im": nrm(ks[8], (N_EVEN, S5_GROUPS, S5_GROUP, S5_STATE), f32) * (2 * S5_STATE) ** -0.5,
        "s5_d": nrm(ks[9], (N_EVEN, S5_WIDTH), f32),
        "s5_w_glu": nrm(ks[10], (N_EVEN, S5_WIDTH, S5_WIDTH), f32) * S5_WIDTH ** -0.5,
        "s5_b_glu": 0.01 * nrm(ks[11], (N_EVEN, S5_WIDTH), f32),
        "gla_w_gate2": nrm(ks[12], (N_EVEN, GLA_GATE_RANK, GLA_QK), f32) * GLA_GATE_RANK ** -0.5,
        "gla_b_gate2": 0.01 * nrm(ks[13], (N_EVEN, GLA_QK), f32),
        "gla_norm_g": 1.0 + 0.01 * nrm(ks[14], (N_EVEN, GLA_DV), f32),
        "ab_w_out": nrm(ks[15], (N_EVEN, AB_OUT_WIDTH, D_MODEL), f32) * (AB_OUT_WIDTH ** -0.5 * DN_BETA),
        "c_w_qkv": nrm(ks[16], (N_ODD, D_MODEL, 3 * D_MODEL), f32) * D_MODEL ** -0.5,
        "c_w_o": nrm(ks[17], (N_ODD, D_MODEL, D_MODEL), f32) * (D_MODEL ** -0.5 * DN_BETA),
        "ln1_g": 1.0 + 0.01 * nrm(ks[18], (DEPTH, D_MODEL), f32),
        "ln1_b": 0.01 * nrm(ks[19], (DEPTH, D_MODEL), f32),
        "moe_w_router": nrm(ks[20], (DEPTH, D_MODEL, N_EXPERTS), f32) * D_MODEL ** -0.5,
        "moe_b_router": 0.01 * nrm(ks[21], (DEPTH, N_EXPERTS), f32),
        "moe_w_gu": nrm(ks[22], (DEPTH, N_EXPERTS, D_MODEL, 2 * D_FF), f32) * D_MODEL ** -0.5,
        "moe_b_gu": 0.01 * nrm(ks[23], (DEPTH, N_EXPERTS, 2 * D_FF), f32),
        "moe_w_down": nrm(ks[24], (DEPTH, N_EXPERTS, D_FF, D_MODEL), f32) * (D_FF ** -0.5 * DN_BETA),
        "moe_b_down": 0.01 * nrm(ks[25], (DEPTH, N_EXPERTS, D_MODEL), f32),
        "ln2_g": 1.0 + 0.01 * nrm(ks[26], (DEPTH, D_MODEL), f32),
        "ln2_b": 0.01 * nrm(ks[27], (DEPTH, D_MODEL), f32),
    }


def reference(x, ab_w_in, s5_lam_re, s5_lam_im, s5_log_step, s5_b_re, s5_b_im, s5_c_re, s5_c_im,
              s5_d, s5_w_glu, s5_b_glu, gla_w_gate2, gla_b_gate2, gla_norm_g, ab_w_out,
              c_w_qkv, c_w_o, ln1_g, ln1_b, moe_w_router, moe_b_router, moe_w_gu, moe_b_gu,
              moe_w_down, moe_b_down, ln2_g, ln2_b):
    for layer in range(DEPTH):
        i = layer // 2
        if layer % 2 == 0:
            mix = even_mixer(x, ab_w_in[i], s5_lam_re[i], s5_lam_im[i], s5_log_step[i], s5_b_re[i],
                             s5_b_im[i], s5_c_re[i], s5_c_im[i], s5_d[i], s5_w_glu[i], s5_b_glu[i],
                             gla_w_gate2[i], gla_b_gate2[i], gla_norm_g[i], ab_w_out[i])
        else:
            mix = odd_mixer(x, c_w_qkv[i], c_w_o[i])
        x = layer_norm(DN_ALPHA * x + mix, ln1_g[layer], ln1_b[layer])
        ffn = moe(x, moe_w_router[layer], moe_b_router[layer], moe_w_gu[layer], moe_b_gu[layer],
                  moe_w_down[layer], moe_b_down[layer])
        x = layer_norm(DN_ALPHA * x + ffn, ln2_g[layer], ln2_b[layer])
    return x
```

```python
import contextlib
import numpy as np
import concourse.bass as bass
import concourse.mybir as mybir

F32 = mybir.dt.float32
BF16 = mybir.dt.bfloat16
ALU = mybir.AluOpType
AF = mybir.ActivationFunctionType
AX = mybir.AxisListType


class T:
    def __init__(self, t):
        self.t = t
        self.w = None
        self.r = {}

    def __getitem__(self, idx):
        return self.t[idx]


class KB:
    EPOCH = 30000

    def __init__(self):
        self.nc = bass.Bass("TRN2", target_bir_lowering=False)
        self.st = contextlib.ExitStack()
        nc = self.nc
        self.E = {'pe': nc.tensor, 'act': nc.scalar, 'dve': nc.vector, 'pool': nc.gpsimd, 'sp': nc.sync}
        self.semobj = {}
        self.cur = {}
        self.cnt = {}
        self.epoch = {}
        for e in ('pe', 'act', 'dve', 'pool'):
            self.epoch[e] = 0
            self._new_epoch(e)
        self.waited = {}
        self.dq = {}
        self.dq_i = {}
        self.dval = {}
        self.nsb = 0
        self.scope = None

    def _new_epoch(self, e):
        key = f"{e}{self.epoch[e]}"
        self.epoch[e] += 1
        self.semobj[key] = self.st.enter_context(self.nc.semaphore("s_" + key))
        self.cur[e] = key
        self.cnt[e] = 0

    def dma_queue(self, q, nsem=8):
        keys = []
        for i in range(nsem):
            key = f"d_{q}_{i}"
            self.semobj[key] = self.st.enter_context(self.nc.semaphore(key))
            self.dval[key] = 0
            keys.append(key)
        self.dq[q] = keys
        self.dq_i[q] = 0

    def sb(self, shape, dt, name=None):
        self.nsb += 1
        st = self.scope if self.scope is not None else self.st
        return T(st.enter_context(self.nc.sbuf_tensor(name or f"sb{self.nsb}", list(shape), dt)))

    def push_scope(self):
        assert self.scope is None
        self.scope = contextlib.ExitStack()

    def barrier(self):
        engs = ('pe', 'act', 'dve', 'pool', 'sp')
        for e in engs:
            for o in ('pe', 'act', 'dve', 'pool'):
                if o != e:
                    self._wait(e, self.cur[o], self.cnt[o])
            for q in self.dq:
                for key in self.dq[q]:
                    self._wait(e, key, self.dval[key])

    def pop_scope(self):
        self.barrier()
        self.scope.close()
        self.scope = None

    def ps(self, shape=(128, 512), dt=F32, name=None):
        self.nsb += 1
        return T(self.st.enter_context(self.nc.psum_tensor(name or f"ps{self.nsb}", list(shape), dt)))

    def dram(self, name, shape, dt, kind="Internal"):
        if kind == "Internal":
            return self.nc.dram_tensor(name, list(shape), dt)
        return self.nc.dram_tensor(name, list(shape), dt, kind=kind)

    def _wait(self, eng, semkey, val):
        if self.waited.get((eng, semkey), 0) >= val:
            return
        self.E[eng].wait_ge(self.semobj[semkey], val)
        self.waited[(eng, semkey)] = val

    def _deps(self, eng, reads, writes):
        deps = {}

        def add(ev):
            if ev is None:
                return
            k, v = ev
            if deps.get(k, 0) < v:
                deps[k] = v
        for t in reads:
            add(t.w)
        for t in writes:
            add(t.w)
            for k, v in t.r.items():
                add((k, v))
        for k, v in deps.items():
            if eng == 'pe' and k.startswith('pe'):
                continue
            self._wait(eng, k, v)

    def _mark(self, ev, reads, writes):
        k, v = ev
        for t in reads:
            if t.r.get(k, 0) < v:
                t.r[k] = v
        for t in writes:
            t.w = ev
            t.r = {}

    def op(self, eng, fn, reads=(), writes=()):
        if self.cnt[eng] >= self.EPOCH:
            self._new_epoch(eng)
        self._deps(eng, reads, writes)
        inst = fn(self.E[eng])
        self.cnt[eng] += 1
        key = self.cur[eng]
        inst.then_inc(self.semobj[key], 1)
        self._mark((key, self.cnt[eng]), reads, writes)
        return inst

    def dma(self, q, out, in_, reads=(), writes=(), **kw):
        self._deps(q, reads, writes)
        keys = self.dq[q]
        key = keys[self.dq_i[q] % len(keys)]
        self.dq_i[q] += 1
        self._wait(q, key, self.dval[key])
        self.E[q].dma_start(out=out, in_=in_, **kw).then_inc(self.semobj[key], 16)
        self.dval[key] += 16
        self._mark((key, self.dval[key]), reads, writes)

    def finish(self):
        for q in self.dq:
            for key in self.dq[q]:
                self._wait(q if q in self.E else 'sp', key, self.dval[key])
        for e in ('pe', 'act', 'dve', 'pool'):
            self._wait('sp', self.cur[e], self.cnt[e])
        self.st.close()

    def mm(self, out_t, out_ap, lhsT_t, lhsT_ap, rhs_t, rhs_ap, start, stop):
        return self.op('pe', lambda e: e.matmul(out_ap, lhsT_ap, rhs_ap, start=start, stop=stop),
                       reads=[lhsT_t, rhs_t], writes=[out_t])


def build_moe(D, F, NT, EL, TB=1024):
    kb = KB()
    nc = kb.nc
    KC = D // 128
    FC = F // 128
    NSB = TB // 512
    NB = NT // TB
    x1b = kb.dram("x1b", [D, NT], BF16, "ExternalInput")
    gt = kb.dram("gt", [EL, NT], F32, "ExternalInput")
    wgu = kb.dram("wgu", [EL, 2 * FC, 128, KC * 128], F32, "ExternalInput")
    bgu = kb.dram("bgu", [128, EL * 2 * FC], F32, "ExternalInput")
    wd = kb.dram("wd", [EL, KC, 128, FC * 128], F32, "ExternalInput")
    bd = kb.dram("bd", [EL, D], F32, "ExternalInput")
    sel = kb.dram("sel", [EL, EL * 128], F32, "ExternalInput")
    y = kb.dram("y", [D, NT], F32, "ExternalOutput")
    kb.dma_queue('sp', 8)

    xb = [kb.sb([128, TB], BF16) for _ in range(KC)]
    acc = [kb.sb([128, TB], F32) for _ in range(KC)]
    act = [kb.sb([128, TB], BF16) for _ in range(FC)]
    gb = kb.sb([128, TB], F32)
    gts = kb.sb([EL, TB], F32)
    bgu_s = kb.sb([128, EL * 2 * FC], F32)
    bd_s = kb.sb([EL, D], F32)
    sel_s = kb.sb([EL, EL * 128], F32)
    NSTG = 3
    stg = [kb.sb([128, max(KC, FC) * 128], F32) for _ in range(NSTG)]
    NWB = 4
    wgb = [kb.sb([128, KC * 128], BF16) for _ in range(NWB)]
    wdb = [kb.sb([128, FC * 128], BF16) for _ in range(2)]
    g32 = [kb.sb([128, 512], F32) for _ in range(2)]
    sig = [kb.sb([128, 512], F32) for _ in range(2)]
    u1 = [kb.sb([128, 512], F32) for _ in range(2)]
    pg = [kb.ps() for _ in range(2)]
    pu = [kb.ps() for _ in range(2)]
    pd = [kb.ps() for _ in range(2)]
    pm = [kb.ps() for _ in range(2)]

    kb.dma('sp', bgu_s[:, :], bgu[:, :], writes=[bgu_s])
    kb.dma('sp', bd_s[:, :], bd[:, :], writes=[bd_s])
    kb.dma('sp', sel_s[:, :], sel[:, :], writes=[sel_s])

    cnt = {'stg': 0, 'wg': 0, 'wd': 0, 'u': 0, 'pd': 0, 'pm': 0}

    def load_slab(src_ap, ncols, dst):
        s = stg[cnt['stg'] % NSTG]
        cnt['stg'] += 1
        kb.dma('sp', s[:, 0:ncols], src_ap, writes=[s])
        kb.op('act', lambda e: e.copy(dst[:, 0:ncols], s[:, 0:ncols]), reads=[s], writes=[dst])

    for b in range(NB):
        t0 = b * TB
        for k in range(KC):
            kb.dma('sp', xb[k][:, :], x1b[k * 128:(k + 1) * 128, t0:t0 + TB], writes=[xb[k]])
        kb.dma('sp', gts[:, :], gt[:, t0:t0 + TB], writes=[gts])
        for e in range(EL):
            for sb_ in range(NSB):
                p = pm[cnt['pm'] % 2]
                cnt['pm'] += 1
                kb.mm(p, p[:, :], sel_s, sel_s[:, e * 128:(e + 1) * 128], gts, gts[:, sb_ * 512:(sb_ + 1) * 512], True, True)
                kb.op('act', lambda en, p=p, sb_=sb_: en.copy(gb[:, sb_ * 512:(sb_ + 1) * 512], p[:, :]), reads=[p], writes=[gb])
            for fc in range(FC):
                wg_ = wgb[cnt['wg'] % NWB]
                wu_ = wgb[(cnt['wg'] + 1) % NWB]
                cnt['wg'] += 2
                load_slab(wgu[e, fc, :, :], KC * 128, wg_)
                load_slab(wgu[e, FC + fc, :, :], KC * 128, wu_)
                bg_ap = bgu_s[:, e * 2 * FC + fc: e * 2 * FC + fc + 1]
                bu_ap = bgu_s[:, e * 2 * FC + FC + fc: e * 2 * FC + FC + fc + 1]
                for sb_ in range(NSB):
                    i = cnt['u'] % 2
                    cnt['u'] += 1
                    cs = slice(sb_ * 512, (sb_ + 1) * 512)
                    for k in range(KC):
                        kb.mm(pg[i], pg[i][:, :], wg_, wg_[:, k * 128:(k + 1) * 128], xb[k], xb[k][:, cs], k == 0, k == KC - 1)
                    for k in range(KC):
                        kb.mm(pu[i], pu[i][:, :], wu_, wu_[:, k * 128:(k + 1) * 128], xb[k], xb[k][:, cs], k == 0, k == KC - 1)
                    kb.op('dve', lambda en, i=i: en.tensor_scalar(g32[i][:, :], pg[i][:, :], bg_ap, 7.0, ALU.add, ALU.min),
                          reads=[pg[i], bgu_s], writes=[g32[i]])
                    kb.op('act', lambda en, i=i: en.activation(sig[i][:, :], g32[i][:, :], AF.Sigmoid, scale=1.702),
                          reads=[g32[i]], writes=[sig[i]])
                    kb.op('dve', lambda en, i=i: en.tensor_scalar(u1[i][:, :], pu[i][:, :], bu_ap, 7.0, ALU.add, ALU.min),
                          reads=[pu[i], bgu_s], writes=[u1[i]])
                    kb.op('pool', lambda en, i=i: en.tensor_scalar(u1[i][:, :], u1[i][:, :], -7.0, 1.0, ALU.max, ALU.add),
                          reads=[u1[i]], writes=[u1[i]])
                    kb.op('pool', lambda en, i=i: en.tensor_tensor(g32[i][:, :], g32[i][:, :], sig[i][:, :], ALU.mult),
                          reads=[g32[i], sig[i]], writes=[g32[i]])
                    kb.op('pool', lambda en, i=i: en.tensor_tensor(g32[i][:, :], g32[i][:, :], u1[i][:, :], ALU.mult),
                          reads=[g32[i], u1[i]], writes=[g32[i]])
                    kb.op('dve', lambda en, i=i, cs=cs: en.tensor_tensor(act[fc][:, cs], g32[i][:, :], gb[:, cs], ALU.mult),
                          reads=[g32[i], gb], writes=[act[fc]])
            for dc in range(KC):
                w_ = wdb[cnt['wd'] % 2]
                cnt['wd'] += 1
                load_slab(wd[e, dc, :, :], FC * 128, w_)
                for sb_ in range(NSB):
                    cs = slice(sb_ * 512, (sb_ + 1) * 512)
                    p = pd[cnt['pd'] % 2]
                    cnt['pd'] += 1
                    for f in range(FC):
                        kb.mm(p, p[:, :], w_, w_[:, f * 128:(f + 1) * 128], act[f], act[f][:, cs], f == 0, f == FC - 1)
                    if e == 0:
                        q = pm[cnt['pm'] % 2]
                        cnt['pm'] += 1
                        kb.mm(q, q[:, :], bd_s, bd_s[:, dc * 128:(dc + 1) * 128], gts, gts[:, cs], True, True)
                        kb.op('act', lambda en, q=q, cs=cs: en.copy(acc[dc][:, cs], q[:, :]), reads=[q], writes=[acc[dc]])
                    kb.op('dve', lambda en, p=p, cs=cs: en.tensor_tensor(acc[dc][:, cs], p[:, :], acc[dc][:, cs], ALU.add),
                          reads=[p, acc[dc]], writes=[acc[dc]])
        for dc in range(KC):
            kb.dma('sp', y[dc * 128:(dc + 1) * 128, t0:t0 + TB], acc[dc][:, :], reads=[acc[dc]])
    kb.finish()
    return kb


def moe_host_inputs(x1T, G, w_gu, b_gu, w_down, b_down, c, EL):
    import ml_dtypes
    D = w_gu.shape[1]
    F2 = w_gu.shape[2]
    F = F2 // 2
    KC = D // 128
    FC = F // 128
    es = slice(c * EL, (c + 1) * EL)
    wg = w_gu[es].reshape(EL, KC, 128, 2 * FC, 128).transpose(0, 3, 2, 1, 4).reshape(EL, 2 * FC, 128, KC * 128)
    wdl = w_down[es].reshape(EL, FC, 128, KC, 128).transpose(0, 3, 2, 1, 4).reshape(EL, KC, 128, FC * 128)
    bg = b_gu[es].reshape(EL, 2 * FC, 128).transpose(2, 0, 1).reshape(128, EL * 2 * FC)
    sel = np.zeros((EL, EL * 128), np.float32)
    for e in range(EL):
        sel[e, e * 128:(e + 1) * 128] = 1.0
    return {
        "x1b": x1T,
        "gt": np.ascontiguousarray(G[:, es].T),
        "wgu": np.ascontiguousarray(wg),
        "bgu": np.ascontiguousarray(bg),
        "wd": np.ascontiguousarray(wdl),
        "bd": np.ascontiguousarray(b_down[es]),
        "sel": sel,
    }


DN_ALPHA = 4 ** 0.25
LN_EPS = 1e-5


def ln_fm(kb, r, out32, outb, g_s, b_s, ones32, D, NTK, tmp, pss):
    KC = len(r)
    for sb_ in range(NTK // 512):
        cs = slice(sb_ * 512, (sb_ + 1) * 512)
        ps_sum, ps_sq = pss
        for k in range(KC):
            kb.mm(ps_sum, ps_sum[:, :], ones32, ones32[:, :], r[k], r[k][:, cs], k == 0, k == KC - 1)
        for k in range(KC):
            sq = tmp['sq'][k % 2]
            kb.op('act', lambda e: e.activation(sq[:, :], r[k][:, cs], AF.Square), reads=[r[k]], writes=[sq])
            kb.mm(ps_sq, ps_sq[:, :], ones32, ones32[:, :], sq, sq[:, :], k == 0, k == KC - 1)
        mean, msq, rstd = tmp['mean'], tmp['msq'], tmp['rstd']
        kb.op('act', lambda e: e.mul(mean[:, :], ps_sum[:, :], 1.0 / D), reads=[ps_sum], writes=[mean])
        kb.op('dve', lambda e: e.tensor_tensor(msq[:, :], mean[:, :], mean[:, :], ALU.mult), reads=[mean], writes=[msq])
        kb.op('dve', lambda e: e.scalar_tensor_tensor(msq[:, :], ps_sq[:, :], 1.0 / D, msq[:, :], ALU.mult, ALU.subtract),
              reads=[ps_sq, msq], writes=[msq])
        kb.op('dve', lambda e: e.tensor_scalar(msq[:, :], msq[:, :], LN_EPS, None, ALU.add), reads=[msq], writes=[msq])
        kb.op('act', lambda e: e.activation(rstd[:, :], msq[:, :], AF.Sqrt), reads=[msq], writes=[rstd])
        kb.op('dve', lambda e: e.reciprocal(rstd[:, :], rstd[:, :]), reads=[rstd], writes=[rstd])
        for k in range(KC):
            kb.op('pool', lambda e: e.tensor_tensor(r[k][:, cs], r[k][:, cs], mean[:, :], ALU.subtract),
                  reads=[r[k], mean], writes=[r[k]])
            kb.op('dve', lambda e: e.tensor_tensor(r[k][:, cs], r[k][:, cs], rstd[:, :], ALU.mult),
                  reads=[r[k], rstd], writes=[r[k]])
            dst = out32[k] if out32 is not None else r[k]
            kb.op('dve', lambda e: e.tensor_scalar(dst[:, cs], r[k][:, cs], g_s[:, k:k + 1], b_s[:, k:k + 1], ALU.mult, ALU.add),
                  reads=[r[k], g_s, b_s], writes=[dst])
            if outb is not None:
                kb.op('act', lambda e: e.copy(outb[k][:, cs], dst[:, cs]), reads=[dst], writes=[outb[k]])


def build_sumln(D, NTK, NP):
    kb = KB()
    KC = D // 128
    xin = kb.dram("xin", [D, NTK], F32, "ExternalInput")
    part = kb.dram("part", [NP, D, NTK], F32, "ExternalInput")
    lng = kb.dram("lng", [128, KC], F32, "ExternalInput")
    lnb = kb.dram("lnb", [128, KC], F32, "ExternalInput")
    ones = kb.dram("ones", [128, 128], F32, "ExternalInput")
    xo = kb.dram("xo", [D, NTK], F32, "ExternalOutput")
    xob = kb.dram("xob", [D, NTK], BF16, "ExternalOutput")
    kb.dma_queue('sp', 8)
    r = [kb.sb([128, NTK], F32) for _ in range(KC)]
    ob = [kb.sb([128, NTK], BF16) for _ in range(2)]
    pb = [kb.sb([128, NTK], F32) for _ in range(3)]
    g_s = kb.sb([128, KC], F32)
    b_s = kb.sb([128, KC], F32)
    ones_s = kb.sb([128, 128], F32)
    tmp = {'sq': [kb.sb([128, 512], F32) for _ in range(2)], 'mean': kb.sb([128, 512], F32),
           'msq': kb.sb([128, 512], F32), 'rstd': kb.sb([128, 512], F32)}
    pss = [kb.ps(), kb.ps()]
    kb.dma('sp', g_s[:, :], lng[:, :], writes=[g_s])
    kb.dma('sp', b_s[:, :], lnb[:, :], writes=[b_s])
    kb.dma('sp', ones_s[:, :], ones[:, :], writes=[ones_s])
    n = 0
    for k in range(KC):
        rows = slice(k * 128, (k + 1) * 128)
        kb.dma('sp', r[k][:, :], xin[rows, :], writes=[r[k]])
        for c in range(NP):
            p = pb[n % 3]
            n += 1
            kb.dma('sp', p[:, :], part[c, rows, :], writes=[p])
            eng = 'dve' if c % 2 == 0 else 'pool'
            if c == 0:
                kb.op('dve', lambda e: e.scalar_tensor_tensor(r[k][:, :], r[k][:, :], DN_ALPHA, p[:, :], ALU.mult, ALU.add),
                      reads=[r[k], p], writes=[r[k]])
            else:
                kb.op(eng, lambda e: e.tensor_tensor(r[k][:, :], r[k][:, :], p[:, :], ALU.add), reads=[r[k], p], writes=[r[k]])
    ln_fm(kb, r, None, None, g_s, b_s, ones_s, D, NTK, tmp, pss)
    for k in range(KC):
        rows = slice(k * 128, (k + 1) * 128)
        kb.dma('sp', xo[rows, :], r[k][:, :], reads=[r[k]])
        o = ob[k % 2]
        kb.op('act', lambda e: e.copy(o[:, :], r[k][:, :]), reads=[r[k]], writes=[o])
        kb.dma('sp', xob[rows, :], o[:, :], reads=[o])
    kb.finish()
    return kb


def pp(v, KC):
    return np.ascontiguousarray(v.reshape(KC, 128).T)


def build_post(D, NTK, has_glu, NEXP=32):
    kb = KB()
    KC = D // 128
    HC = KC // 2
    feat = kb.dram("feat", [D, NTK], F32, "ExternalInput")
    wout = kb.dram("wout", [KC, 128, KC * 128], F32, "ExternalInput")
    if has_glu:
        wglu = kb.dram("wglu", [HC, 128, HC * 128], F32, "ExternalInput")
        bglu = kb.dram("bglu", [128, HC], F32, "ExternalInput")
    xin = kb.dram("xin", [D, NTK], F32, "ExternalInput")
    lng = kb.dram("lng", [128, KC], F32, "ExternalInput")
    lnb = kb.dram("lnb", [128, KC], F32, "ExternalInput")
    ones = kb.dram("ones", [128, 128], F32, "ExternalInput")
    wr = kb.dram("wr", [128, KC * NEXP], F32, "ExternalInput")
    brb = kb.dram("brb", [128, NEXP], F32, "ExternalInput")
    xo = kb.dram("xo", [D, NTK], F32, "ExternalOutput")
    xob = kb.dram("xob", [D, NTK], BF16, "ExternalOutput")
    gout = kb.dram("gout", [NTK, NEXP], F32, "ExternalOutput")
    kb.dma_queue('sp', 8)
    fb = [kb.sb([128, NTK], BF16) for _ in range(KC)]
    zb = [kb.sb([128, NTK], BF16) for _ in range(HC)] if has_glu else None
    r = [kb.sb([128, NTK], F32) for _ in range(KC)]
    ob = [kb.sb([128, NTK], BF16) for _ in range(2)]
    st32 = [kb.sb([128, NTK], F32) for _ in range(3)]
    stg = [kb.sb([128, KC * 128], F32) for _ in range(3)]
    wb = [kb.sb([128, KC * 128], BF16) for _ in range(2)]
    g_s = kb.sb([128, KC], F32)
    b_s = kb.sb([128, KC], F32)
    ones_s = kb.sb([128, 128], F32)
    wr_s = kb.sb([128, KC * NEXP], F32)
    brb_s = kb.sb([128, NEXP], F32)
    sg = [kb.sb([128, 512], F32) for _ in range(2)]
    tmp = {'sq': [kb.sb([128, 512], F32) for _ in range(2)], 'mean': kb.sb([128, 512], F32),
           'msq': kb.sb([128, 512], F32), 'rstd': kb.sb([128, 512], F32)}
    pss = [kb.ps(), kb.ps()]
    pmm = [kb.ps(), kb.ps()]
    prt = kb.ps([128, NEXP])
    for s_, d_ in ((g_s, lng), (b_s, lnb), (ones_s, ones), (wr_s, wr), (brb_s, brb)):
        kb.dma('sp', s_[:, :], d_[:, :], writes=[s_])
    if has_glu:
        bglu_s = kb.sb([128, HC], F32)
        kb.dma('sp', bglu_s[:, :], bglu[:, :], writes=[bglu_s])
    c = {'s': 0, 'w': 0, 'p': 0, 'g': 0}

    def load_cast(src_ap, dst, n):
        s = st32[c['s'] % 3]
        c['s'] += 1
        kb.dma('sp', s[:, 0:n], src_ap, writes=[s])
        kb.op('act', lambda e: e.copy(dst[:, 0:n], s[:, 0:n]), reads=[s], writes=[dst])

    def load_w(src_ap, n):
        s = stg[c['w'] % 3]
        w = wb[c['w'] % 2]
        c['w'] += 1
        kb.dma('sp', s[:, 0:n], src_ap, writes=[s])
        kb.op('act', lambda e: e.copy(w[:, 0:n], s[:, 0:n]), reads=[s], writes=[w])
        return w

    for k in range(KC):
        dst = zb[k] if (has_glu and k < HC) else fb[k]
        load_cast(feat[k * 128:(k + 1) * 128, :], dst, NTK)
        kb.dma('sp', r[k][:, :], xin[k * 128:(k + 1) * 128, :], writes=[r[k]])
    if has_glu:
        for fo in range(HC):
            w = load_w(wglu[fo, :, :], HC * 128)
            for sb_ in range(NTK // 512):
                cs = slice(sb_ * 512, (sb_ + 1) * 512)
                p = pmm[c['p'] % 2]
                c['p'] += 1
                for k in range(HC):
                    kb.mm(p, p[:, :], w, w[:, k * 128:(k + 1) * 128], zb[k], zb[k][:, cs], k == 0, k == HC - 1)
                s = sg[c['g'] % 2]
                c['g'] += 1
                kb.op('act', lambda e: e.activation(s[:, :], p[:, :], AF.Sigmoid, bias=bglu_s[:, fo:fo + 1]),
                      reads=[p, bglu_s], writes=[s])
                kb.op('dve', lambda e: e.tensor_tensor(fb[fo][:, cs], zb[fo][:, cs], s[:, :], ALU.mult),
                      reads=[zb[fo], s], writes=[fb[fo]])
    for dc in range(KC):
        w = load_w(wout[dc, :, :], KC * 128)
        for sb_ in range(NTK // 512):
            cs = slice(sb_ * 512, (sb_ + 1) * 512)
            p = pmm[c['p'] % 2]
            c['p'] += 1
            for k in range(KC):
                kb.mm(p, p[:, :], w, w[:, k * 128:(k + 1) * 128], fb[k], fb[k][:, cs], k == 0, k == KC - 1)
            kb.op('dve', lambda e: e.scalar_tensor_tensor(r[dc][:, cs], r[dc][:, cs], DN_ALPHA, p[:, :], ALU.mult, ALU.add),
                  reads=[r[dc], p], writes=[r[dc]])
    ln_fm(kb, r, None, None, g_s, b_s, ones_s, D, NTK, tmp, pss)
    for k in range(KC):
        rows = slice(k * 128, (k + 1) * 128)
        kb.dma('sp', xo[rows, :], r[k][:, :], reads=[r[k]])
        o = ob[k % 2]
        kb.op('act', lambda e: e.copy(o[:, :], r[k][:, :]), reads=[r[k]], writes=[o])
        kb.dma('sp', xob[rows, :], o[:, :], reads=[o])
    lg = [kb.sb([128, NEXP], F32) for _ in range(2)]
    ee = [kb.sb([128, NEXP], F32) for _ in range(2)]
    t8 = [kb.sb([128, 8], F32) for _ in range(2)]
    sc = [kb.sb([128, 4], F32) for _ in range(2)]
    for tt in range(NTK // 128):
        ts_ = slice(tt * 128, (tt + 1) * 128)
        i = tt % 2
        for k in range(KC):
            kb.mm(prt, prt[:, :], r[k], r[k][:, ts_], wr_s, wr_s[:, k * NEXP:(k + 1) * NEXP], k == 0, k == KC - 1)
        kb.op('dve', lambda e: e.tensor_tensor(lg[i][:, :], prt[:, :], brb_s[:, :], ALU.add), reads=[prt, brb_s], writes=[lg[i]])
        kb.op('dve', lambda e: e.max(t8[i][:, :], lg[i][:, :]), reads=[lg[i]], writes=[t8[i]])
        kb.op('dve', lambda e: e.tensor_scalar(sc[i][:, 0:1], t8[i][:, 0:1], -1.0, None, ALU.mult), reads=[t8[i]], writes=[sc[i]])
        kb.op('act', lambda e: e.activation(ee[i][:, :], lg[i][:, :], AF.Exp, bias=sc[i][:, 0:1]), reads=[lg[i], sc[i]], writes=[ee[i]])
        kb.op('dve', lambda e: e.tensor_scalar(lg[i][:, :], lg[i][:, :], t8[i][:, 3:4], None, ALU.is_ge), reads=[lg[i], t8[i]], writes=[lg[i]])
        kb.op('dve', lambda e: e.tensor_tensor(ee[i][:, :], ee[i][:, :], lg[i][:, :], ALU.mult), reads=[ee[i], lg[i]], writes=[ee[i]])
        kb.op('dve', lambda e: e.reduce_sum(sc[i][:, 1:2], ee[i][:, :], AX.X), reads=[ee[i]], writes=[sc[i]])
        kb.op('dve', lambda e: e.reciprocal(sc[i][:, 2:3], sc[i][:, 1:2]), reads=[sc[i]], writes=[sc[i]])
        kb.op('dve', lambda e: e.tensor_scalar(ee[i][:, :], ee[i][:, :], sc[i][:, 2:3], None, ALU.mult), reads=[ee[i], sc[i]], writes=[ee[i]])
        kb.dma('sp', gout[ts_, :], ee[i][:, :], reads=[ee[i]])
    kb.finish()
    return kb


def wslab(w, KI, KO):
    return np.ascontiguousarray(w.reshape(KI, 128, KO, 128).transpose(2, 1, 0, 3).reshape(KO, 128, KI * 128))


GROUPS = ((128, 1), (512, 4), (2048, 16))
PAD = 2048
NEG = -30000.0


def attn_mask_bias():
    cols = []
    i = np.arange(128)[:, None]
    for L, d in GROUPS:
        W = L + 128
        w = np.arange(W)[None, :]
        delta = i + L - w
        ok = (delta >= 0) & (delta <= L) & (delta % d == 0)
        cols.append(np.where(ok, 0.0, NEG).astype(np.float32))
    return np.ascontiguousarray(np.concatenate(cols, 1))


def build_attn(D, NT, HPC, DH=128):
    kb = KB()
    KC = D // 128
    NTI = NT // 128
    NPT = PAD // 128
    WT = sum(L + 128 for L, _ in GROUPS)
    NJ = WT // 128
    x2b = kb.dram("x2b", [D, NT], BF16, "ExternalInput")
    wq = kb.dram("wq", [HPC, 128, KC * 128], F32, "ExternalInput")
    wk = kb.dram("wk", [HPC, 128, KC * 128], F32, "ExternalInput")
    wv = kb.dram("wv", [HPC, 128, KC * 128], F32, "ExternalInput")
    mbd = kb.dram("mb", [128, WT], F32, "ExternalInput")
    idd = kb.dram("ident", [128, 128], BF16, "ExternalInput")
    o = kb.dram("o", [NT, HPC * DH], F32, "ExternalOutput")
    kb.dma_queue('sp', 8)
    xs = [[kb.sb([128, 512], BF16) for _ in range(KC)] for _ in range(2)]
    stg = [kb.sb([128, KC * 128], F32) for _ in range(2)]
    wqb, wkb, wvb = (kb.sb([128, KC * 128], BF16) for _ in range(3))
    qT = kb.sb([128, NT], BF16)
    kT = kb.sb([128, PAD + NT], BF16)
    vtmp = [kb.sb([128, 512], BF16) for _ in range(2)]
    vtok = kb.sb([128, (NPT + NTI) * 128], BF16)
    mb = kb.sb([128, WT], F32)
    ident = kb.sb([128, 128], BF16)
    Sm = [kb.sb([128, WT], F32) for _ in range(2)]
    P = [kb.sb([128, WT], BF16) for _ in range(2)]
    PT = [kb.sb([128, WT], BF16) for _ in range(2)]
    sc = [kb.sb([128, 4], F32) for _ in range(2)]
    osb = [kb.sb([128, DH], F32) for _ in range(2)]
    pp_ = [kb.ps() for _ in range(2)]
    pt_ = [kb.ps([128, 512], BF16) for _ in range(2)]
    po = [kb.ps([128, DH]) for _ in range(2)]
    kb.dma('sp', mb[:, :], mbd[:, :], writes=[mb])
    kb.dma('sp', ident[:, :], idd[:, :], writes=[ident])
    scale = DH ** -0.5
    c = {'p': 0, 't': 0, 'x': 0}

    def nps():
        p = pp_[c['p'] % 2]
        c['p'] += 1
        return p

    def npt():
        p = pt_[c['t'] % 2]
        c['t'] += 1
        return p

    for h in range(HPC):
        for i, (wd_, wb_) in enumerate(((wq, wqb), (wk, wkb), (wv, wvb))):
            s = stg[i % 2]
            kb.dma('sp', s[:, :], wd_[h, :, :], writes=[s])
            kb.op('act', lambda e: e.copy(wb_[:, :], s[:, :]), reads=[s], writes=[wb_])
        kb.op('pool', lambda e: e.memset(kT[:, 0:PAD], 0.0), writes=[kT])
        kb.op('pool', lambda e: e.memset(vtok[:, 0:NPT * 128], 0.0), writes=[vtok])
        for tb in range(NT // 512):
            xb = xs[c['x'] % 2]
            c['x'] += 1
            cs = slice(tb * 512, (tb + 1) * 512)
            for k in range(KC):
                kb.dma('sp', xb[k][:, :], x2b[k * 128:(k + 1) * 128, cs], writes=[xb[k]])
            p = nps()
            for k in range(KC):
                kb.mm(p, p[:, :], wqb, wqb[:, k * 128:(k + 1) * 128], xb[k], xb[k][:, :], k == 0, k == KC - 1)
            kb.op('act', lambda e: e.mul(qT[:, cs], p[:, :], scale), reads=[p], writes=[qT])
            p = nps()
            for k in range(KC):
                kb.mm(p, p[:, :], wkb, wkb[:, k * 128:(k + 1) * 128], xb[k], xb[k][:, :], k == 0, k == KC - 1)
            kb.op('dve', lambda e: e.tensor_copy(kT[:, PAD + tb * 512:PAD + (tb + 1) * 512], p[:, :]), reads=[p], writes=[kT])
            p = nps()
            for k in range(KC):
                kb.mm(p, p[:, :], wvb, wvb[:, k * 128:(k + 1) * 128], xb[k], xb[k][:, :], k == 0, k == KC - 1)
            vt = vtmp[tb % 2]
            kb.op('act', lambda e: e.copy(vt[:, :], p[:, :]), reads=[p], writes=[vt])
            q = npt()
            for j in range(4):
                kb.op('pe', lambda e: e.transpose(q[:, j * 128:(j + 1) * 128], vt[:, j * 128:(j + 1) * 128], ident[:, :]),
                      reads=[vt, ident], writes=[q])
            a0 = (NPT + tb * 4) * 128
            kb.op('dve', lambda e: e.tensor_copy(vtok[:, a0:a0 + 512], q[:, :]), reads=[q], writes=[vtok])
        for ti in range(NTI):
            s0 = ti * 128
            u = ti % 2
            col = 0
            vt_idx = []
            for L, d in GROUPS:
                W = L + 128
                kstart = PAD + s0 - L
                for c0 in range(0, W, 512):
                    n = min(512, W - c0)
                    p = nps()
                    kb.mm(p, p[:, 0:n], qT, qT[:, s0:s0 + 128], kT, kT[:, kstart + c0:kstart + c0 + n], True, True)
                    kb.op('dve', lambda e: e.tensor_tensor(Sm[u][:, col + c0:col + c0 + n], p[:, 0:n], mb[:, col + c0:col + c0 + n], ALU.add),
                          reads=[p, mb], writes=[Sm[u]])
                if s0 < L:
                    kb.op('pool', lambda e: e.memset(Sm[u][:, col:col + (L - s0)], NEG), writes=[Sm[u]])
                vt_idx += list(range(NPT + ti - L // 128, NPT + ti + 1))
                col += W
            kb.op('dve', lambda e: e.reduce_max(sc[u][:, 0:1], Sm[u][:, :], AX.X), reads=[Sm[u]], writes=[sc[u]])
            kb.op('dve', lambda e: e.tensor_scalar(sc[u][:, 1:2], sc[u][:, 0:1], -1.0, None, ALU.mult), reads=[sc[u]], writes=[sc[u]])
            kb.op('act', lambda e: e.activation(P[u][:, :], Sm[u][:, :], AF.Exp, bias=sc[u][:, 1:2], accum_out=sc[u][:, 2:3]),
                  reads=[Sm[u], sc[u]], writes=[P[u], sc[u]])
            for j4 in range(NJ // 4):
                q = npt()
                for j in range(4):
                    jj = j4 * 4 + j
                    kb.op('pe', lambda e: e.transpose(q[:, j * 128:(j + 1) * 128], P[u][:, jj * 128:(jj + 1) * 128], ident[:, :]),
                          reads=[P[u], ident], writes=[q])
                eng = 'act' if j4 % 2 == 0 else 'dve'
                if eng == 'act':
                    kb.op('act', lambda e: e.copy(PT[u][:, j4 * 512:(j4 + 1) * 512], q[:, :]), reads=[q], writes=[PT[u]])
                else:
                    kb.op('dve', lambda e: e.tensor_copy(PT[u][:, j4 * 512:(j4 + 1) * 512], q[:, :]), reads=[q], writes=[PT[u]])
            pq = po[u]
            for jj in range(NJ):
                a = vt_idx[jj]
                kb.mm(pq, pq[:, :], PT[u], PT[u][:, jj * 128:(jj + 1) * 128], vtok, vtok[:, a * 128:(a + 1) * 128], jj == 0, jj == NJ - 1)
            kb.op('dve', lambda e: e.reciprocal(sc[u][:, 3:4], sc[u][:, 2:3]), reads=[sc[u]], writes=[sc[u]])
            kb.op('dve', lambda e: e.tensor_scalar(osb[u][:, :], pq[:, :], sc[u][:, 3:4], None, ALU.mult), reads=[pq, sc[u]], writes=[osb[u]])
            kb.dma('sp', o[s0:s0 + 128, h * DH:(h + 1) * DH], osb[u][:, :], reads=[osb[u]])
    kb.finish()
    return kb


TWO_PI = 6.283185307179586
MAGIC = 12582912.0
GLA_EPS = 1e-6


def s5_host(lam_re, lam_im, log_step, b_re, b_im, c_re, c_im, d_skip, c):
    lr = np.zeros(512, np.float32)
    li = np.zeros(512, np.float32)
    ls = np.zeros(512, np.float32)
    BreT = np.zeros((128, 512), np.float32)
    BimT = np.zeros((128, 512), np.float32)
    Cre = np.zeros((128, 512), np.float32)
    Cim = np.zeros((128, 512), np.float32)
    for q in range(4):
        for half in range(2):
            gl = 2 * q + half
            g = 8 * c + gl
            ms = slice(q * 128 + half * 64, q * 128 + half * 64 + 64)
            lr[ms] = lam_re[g]
            li[ms] = lam_im[g]
            ls[ms] = log_step[g]
            BreT[16 * gl:16 * gl + 16, ms] = b_re[g].T
            BimT[16 * gl:16 * gl + 16, ms] = b_im[g].T
            Cre[half * 64:half * 64 + 64, q * 128 + 16 * gl:q * 128 + 16 * gl + 16] = c_re[g].T
            Cim[half * 64:half * 64 + 64, q * 128 + 16 * gl:q * 128 + 16 * gl + 16] = c_im[g].T
    rep = lambda v: np.ascontiguousarray(np.tile(v[None, :], (128, 1)))
    return {"s5_lr": rep(lr), "s5_li": rep(li), "s5_ls": rep(ls), "s5_bre": BreT, "s5_bim": BimT,
            "s5_cre": Cre, "s5_cim": Cim,
            "s5_d": np.ascontiguousarray(d_skip[128 * c:128 * (c + 1)].reshape(128, 1))}


def build_mix(D, NT, do_s5=True, do_gla=True):
    kb = KB()
    KC = D // 128
    TB = 512
    NBLK = NT // TB
    DK, DV = 128, 256
    xT = kb.dram("xT", [D, NT], F32, "ExternalInput")
    w5 = kb.dram("w5", [5, 128, KC * 128], F32, "ExternalInput")
    wvd = kb.dram("wv", [128, KC * DV], F32, "ExternalInput")
    wgd = kb.dram("wg", [128, KC * 16], F32, "ExternalInput")
    pnames = ["s5_lr", "s5_li", "s5_ls", "s5_bre", "s5_bim", "s5_cre", "s5_cim"]
    pd = {n: kb.dram(n, [128, 512], F32, "ExternalInput") for n in pnames}
    s5d = kb.dram("s5_d", [128, 1], F32, "ExternalInput")
    wg2d = kb.dram("wg2", [16, 128], F32, "ExternalInput")
    bg2d = kb.dram("bg2", [128, 1], F32, "ExternalInput")
    ngd = kb.dram("ng", [128, 2], F32, "ExternalInput")
    cmaskd = kb.dram("cmask", [128, 128], F32, "ExternalInput")
    rmaskd = kb.dram("rmask", [128, 512], F32, "ExternalInput")
    onesd = kb.dram("ones", [128, 512], F32, "ExternalInput")
    identd = kb.dram("ident", [128, 128], F32, "ExternalInput")
    zT = kb.dram("zT", [128, NT], F32, "ExternalOutput")
    ybT = kb.dram("ybT", [DV, NT], F32, "ExternalOutput")
    kb.dma_queue('sp', 8)

    def ld(shape, src, dt=F32):
        t = kb.sb(shape, dt)
        kb.dma('sp', t[:, :], src[:, :], writes=[t])
        return t
    ones = ld([128, 512], onesd)
    ident = ld([128, 128], identd)
    identb = kb.sb([128, 128], BF16)
    kb.op('act', lambda e: e.copy(identb[:, :], ident[:, :]), reads=[ident], writes=[identb])
    onesb = kb.sb([128, 128], BF16)
    kb.op('act', lambda e: e.copy(onesb[:, :], ones[:, 0:128]), reads=[ones], writes=[onesb])

    wsl = [kb.sb([128, KC * 128], BF16) for _ in range(5)]
    wvb = kb.sb([128, KC * DV], BF16)
    wgb = kb.sb([128, KC * 16], BF16)
    if do_s5:
        bbr, bbi = kb.sb([128, 512], BF16), kb.sb([128, 512], BF16)
        creb, cimb = kb.sb([128, 512], BF16), kb.sb([128, 512], BF16)
        rfull = [kb.sb([128, 512], F32) for _ in range(4)]
        Ec = [kb.sb([128, 512], F32) for _ in range(4)]
        Es = [kb.sb([128, 512], F32) for _ in range(4)]
        sin_ = [kb.sb([128, 2], F32) for _ in range(4)]
        s5dv = ld([128, 1], s5d)
    if do_gla:
        wg2 = ld([16, 128], wg2d)
        bg2 = ld([128, 1], bg2d)
        nbg2 = kb.sb([128, 1], F32)
        ng = ld([128, 2], ngd)
        cmask = ld([128, 128], cmaskd)
        rmask = ld([128, 512], rmaskd)
    kb.push_scope()
    stg = [kb.sb([128, KC * DV], F32) for _ in range(2)]
    ns = [0]

    def ldw(src_ap, dst, n):
        s = stg[ns[0] % 2]
        ns[0] += 1
        kb.dma('sp', s[:, 0:n], src_ap, writes=[s])
        kb.op('act', lambda e: e.copy(dst[:, 0:n], s[:, 0:n]), reads=[s], writes=[dst])
    for i in range(5):
        ldw(w5[i, :, :], wsl[i], KC * 128)
    ldw(wvd[:, :], wvb, KC * DV)
    ldw(wgd[:, :], wgb, KC * 16)

    pmm = [kb.ps() for _ in range(2)]
    pc = [0]

    def nps():
        p = pmm[pc[0] % 2]
        pc[0] += 1
        return p

    V = lambda eng, f, r, w: kb.op(eng, f, reads=r, writes=w)

    if do_s5:
        P = {n: ld([128, 512], pd[n]) for n in pnames}
        W = lambda: kb.sb([128, 512], F32)
        lr, li, dt = P["s5_lr"], P["s5_li"], P["s5_ls"]
        V('dve', lambda e: e.tensor_scalar(lr[:, :], lr[:, :], -1e-4, None, ALU.min), [lr], [lr])
        V('act', lambda e: e.activation(dt[:, :], dt[:, :], AF.Exp), [dt], [dt])
        a_, th = W(), W()
        V('dve', lambda e: e.tensor_tensor(a_[:, :], lr[:, :], dt[:, :], ALU.mult), [lr, dt], [a_])
        V('dve', lambda e: e.tensor_tensor(th[:, :], li[:, :], dt[:, :], ALU.mult), [li, dt], [th])
        mag = W()
        V('act', lambda e: e.activation(mag[:, :], a_[:, :], AF.Exp), [a_], [mag])

        def sincos(src, shift, dst):
            t = W()
            V('dve', lambda e: e.tensor_scalar(t[:, :], src[:, :], shift, 1.0 / TWO_PI, ALU.add, ALU.mult), [src], [t])
            k_ = W()
            V('dve', lambda e: e.tensor_scalar(k_[:, :], t[:, :], MAGIC, None, ALU.add), [t], [k_])
            V('dve', lambda e: e.tensor_scalar(k_[:, :], k_[:, :], MAGIC, None, ALU.subtract), [k_], [k_])
            V('dve', lambda e: e.tensor_tensor(t[:, :], t[:, :], k_[:, :], ALU.subtract), [t, k_], [t])
            V('dve', lambda e: e.tensor_scalar(t[:, :], t[:, :], 0.5, -0.5, ALU.min, ALU.max), [t], [t])
            V('act', lambda e: e.activation(dst[:, :], t[:, :], AF.Sin, scale=TWO_PI), [t], [dst])
        sn, cs_ = W(), W()
        sincos(th, 0.0, sn)
        sincos(th, TWO_PI / 4, cs_)
        abr, abi = W(), W()
        V('dve', lambda e: e.tensor_tensor(abr[:, :], mag[:, :], cs_[:, :], ALU.mult), [mag, cs_], [abr])
        V('dve', lambda e: e.tensor_tensor(abi[:, :], mag[:, :], sn[:, :], ALU.mult), [mag, sn], [abi])
        den, t1, t2 = W(), W(), W()
        V('dve', lambda e: e.tensor_tensor(den[:, :], lr[:, :], lr[:, :], ALU.mult), [lr], [den])
        V('dve', lambda e: e.tensor_tensor(t1[:, :], li[:, :], li[:, :], ALU.mult), [li], [t1])
        V('dve', lambda e: e.tensor_tensor(den[:, :], den[:, :], t1[:, :], ALU.add), [den, t1], [den])
        V('dve', lambda e: e.reciprocal(den[:, :], den[:, :]), [den], [den])
        nr = W()
        V('dve', lambda e: e.tensor_scalar(nr[:, :], abr[:, :], -1.0, None, ALU.add), [abr], [nr])
        fre, fim = W(), W()
        V('dve', lambda e: e.tensor_tensor(t1[:, :], nr[:, :], lr[:, :], ALU.mult), [nr, lr], [t1])
        V('dve', lambda e: e.tensor_tensor(t2[:, :], abi[:, :], li[:, :], ALU.mult), [abi, li], [t2])
        V('dve', lambda e: e.tensor_tensor(t1[:, :], t1[:, :], t2[:, :], ALU.add), [t1, t2], [t1])
        V('dve', lambda e: e.tensor_tensor(fre[:, :], t1[:, :], den[:, :], ALU.mult), [t1, den], [fre])
        V('dve', lambda e: e.tensor_tensor(t1[:, :], abi[:, :], lr[:, :], ALU.mult), [abi, lr], [t1])
        V('dve', lambda e: e.tensor_tensor(t2[:, :], nr[:, :], li[:, :], ALU.mult), [nr, li], [t2])
        V('dve', lambda e: e.tensor_tensor(t1[:, :], t1[:, :], t2[:, :], ALU.subtract), [t1, t2], [t1])
        V('dve', lambda e: e.tensor_tensor(fim[:, :], t1[:, :], den[:, :], ALU.mult), [t1, den], [fim])
        bre, bim = P["s5_bre"], P["s5_bim"]
        V('dve', lambda e: e.tensor_tensor(t1[:, :], fre[:, :], bre[:, :], ALU.mult), [fre, bre], [t1])
        V('dve', lambda e: e.tensor_tensor(t2[:, :], fim[:, :], bim[:, :], ALU.mult), [fim, bim], [t2])
        V('dve', lambda e: e.tensor_tensor(bbr[:, :], t1[:, :], t2[:, :], ALU.subtract), [t1, t2], [bbr])
        V('dve', lambda e: e.tensor_tensor(t1[:, :], fre[:, :], bim[:, :], ALU.mult), [fre, bim], [t1])
        V('dve', lambda e: e.tensor_tensor(t2[:, :], fim[:, :], bre[:, :], ALU.mult), [fim, bre], [t2])
        V('dve', lambda e: e.tensor_tensor(bbi[:, :], t1[:, :], t2[:, :], ALU.add), [t1, t2], [bbi])
        V('act', lambda e: e.copy(creb[:, :], P["s5_cre"][:, :]), [P["s5_cre"]], [creb])
        V('act', lambda e: e.mul(cimb[:, :], P["s5_cim"][:, :], -1.0), [P["s5_cim"]], [cimb])
        for q in range(4):
            qs = slice(q * 128, (q + 1) * 128)
            cols = {}
            for nm, src in (("mag", mag), ("cs", cs_), ("sn", sn)):
                p = nps()
                V('pe', lambda e: e.transpose(p[:, 0:128], src[:, qs], ident[:, :]), [src, ident], [p])
                ct = kb.sb([128, 128], F32)
                V('act', lambda e: e.copy(ct[:, :], p[:, 0:128]), [p], [ct])
                cols[nm] = ct
            rf = rfull[q]
            V('dve', lambda e: e.tensor_scalar(rf[:, :], ones[:, :], cols["mag"][:, 0:1], None, ALU.mult), [ones, cols["mag"]], [rf])
            ec, es = Ec[q], Es[q]
            V('act', lambda e: e.copy(ec[:, 0:1], cols["cs"][:, 0:1]), [cols["cs"]], [ec])
            V('act', lambda e: e.copy(es[:, 0:1], cols["sn"][:, 0:1]), [cols["sn"]], [es])
            pw = kb.sb([128, 8], F32)
            V('act', lambda e: e.copy(pw[:, 0:1], cols["cs"][:, 0:1]), [cols["cs"]], [pw])
            V('act', lambda e: e.copy(pw[:, 1:2], cols["sn"][:, 0:1]), [cols["sn"]], [pw])
            n = 1
            tmpw = W()
            while n < TB:
                V('dve', lambda e: e.tensor_scalar(pw[:, 2:3], pw[:, 1:2], -1.0, None, ALU.mult), [pw], [pw])
                V('dve', lambda e: e.tensor_scalar(tmpw[:, 0:n], ec[:, 0:n], pw[:, 0:1], None, ALU.mult), [ec, pw], [tmpw])
                V('dve', lambda e: e.scalar_tensor_tensor(ec[:, n:2 * n], es[:, 0:n], pw[:, 2:3], tmpw[:, 0:n], ALU.mult, ALU.add),
                  [es, pw, tmpw], [ec])
                V('dve', lambda e: e.tensor_scalar(tmpw[:, 0:n], es[:, 0:n], pw[:, 0:1], None, ALU.mult), [es, pw], [tmpw])
                V('dve', lambda e: e.scalar_tensor_tensor(es[:, n:2 * n], ec[:, 0:n], pw[:, 1:2], tmpw[:, 0:n], ALU.mult, ALU.add),
                  [ec, pw, tmpw], [es])
                V('dve', lambda e: e.tensor_tensor(pw[:, 3:4], pw[:, 0:1], pw[:, 0:1], ALU.mult), [pw], [pw])
                V('dve', lambda e: e.tensor_tensor(pw[:, 4:5], pw[:, 1:2], pw[:, 1:2], ALU.mult), [pw], [pw])
                V('dve', lambda e: e.tensor_tensor(pw[:, 5:6], pw[:, 0:1], pw[:, 1:2], ALU.mult), [pw], [pw])
                V('dve', lambda e: e.tensor_tensor(pw[:, 0:1], pw[:, 3:4], pw[:, 4:5], ALU.subtract), [pw], [pw])
                V('dve', lambda e: e.tensor_scalar(pw[:, 1:2], pw[:, 5:6], 2.0, None, ALU.mult), [pw], [pw])
                n *= 2
        for q in range(4):
            V('pool', lambda e: e.memset(sin_[q][:, :], 0.0), [], [sin_[q]])
    kb.pop_scope()
    if do_s5:
        s5w = {n_: [kb.sb([128, 512], F32) for _ in range(2)] for n_ in ("t1", "t2", "xr", "xi", "zr", "zi", "sr", "si")}
        s5b = {n_: [kb.sb([128, 512], BF16) for _ in range(2)] for n_ in ("srb", "sib")}
        yw = [kb.sb([128, 512], F32) for _ in range(4)]
        py = kb.ps()

    if do_gla:
        V('dve', lambda e: e.tensor_scalar(nbg2[:, :], bg2[:, :], -1.0, None, ALU.mult), [bg2], [nbg2])
        S = kb.sb([128, DV], F32)
        Sb = kb.sb([128, DV], BF16)
        V('pool', lambda e: e.memset(S[:, :], 0.0), [], [S])
        V('pool', lambda e: e.memset(Sb[:, :], 0.0), [], [Sb])
        glT = kb.sb([16, 512], F32)
        qt = kb.sb([128, 512], BF16)
        kt = kb.sb([128, 512], BF16)
        q32 = kb.sb([128, 512], F32)
        k32 = kb.sb([128, 512], F32)
        Bc = kb.sb([128, 512], F32)
        eb = kb.sb([128, 512], F32)
        enb = kb.sb([128, 512], F32)
        l1 = kb.sb([128, 512], F32)
        sr_ = [kb.sb([128, 512], F32) for _ in range(2)]
        vtk = [kb.sb([128, DV], BF16) for _ in range(4)]
        kdT = kb.sb([128, 128], BF16)
        kd = kb.sb([128, 128], BF16)
        AT = kb.sb([128, 128], BF16)
        sq = [kb.sb([128, 128], BF16) for _ in range(2)]
        rs = kb.sb([128, 128], F32)
        on = [kb.sb([128, 128], F32) for _ in range(2)]
        ybo = [kb.sb([128, 512], F32) for _ in range(2)]
        pA = kb.ps([128, 128])
        pO = [kb.ps([128, 128]) for _ in range(2)]
        pT = kb.ps([128, 128], BF16)
        pKV = kb.ps([128, DV])

    xst = [kb.sb([128, TB], F32) for _ in range(3)]
    xb = [[kb.sb([128, TB], BF16) for _ in range(KC)] for _ in range(2)]
    ub = [kb.sb([128, TB], BF16) for _ in range(2)]
    nx = [0]

    for blk in range(NBLK):
        cs = slice(blk * TB, (blk + 1) * TB)
        xs = xb[blk % 2]
        for k in range(KC):
            s = xst[nx[0] % 3]
            nx[0] += 1
            kb.dma('sp', s[:, :], xT[k * 128:(k + 1) * 128, cs], writes=[s])
            eng = 'act' if k % 2 == 0 else 'pool'
            if eng == 'act':
                V('act', lambda e: e.copy(xs[k][:, :], s[:, :]), [s], [xs[k]])
            else:
                V('pool', lambda e: e.tensor_copy(xs[k][:, :], s[:, :]), [s], [xs[k]])

        def proj(w, ncols, wcols=128):
            p = nps()
            for k in range(KC):
                kb.mm(p, p[0:ncols, :], w, w[:, k * wcols:k * wcols + ncols], xs[k], xs[k][:, :], k == 0, k == KC - 1)
            return p

        if do_s5:
            u = ub[blk % 2]
            p = proj(wsl[0], 128)
            V('act', lambda e: e.copy(u[:, :], p[:, :]), [p], [u])
            for q in range(4):
                qs = slice(q * 128, (q + 1) * 128)
                i = q % 2
                w_ = {n_: s5w[n_][i] for n_ in s5w}
                pxr, pxi = nps(), nps()
                kb.mm(pxr, pxr[:, :], bbr, bbr[:, qs], u, u[:, :], True, True)
                kb.mm(pxi, pxi[:, :], bbi, bbi[:, qs], u, u[:, :], True, True)
                ec, es = Ec[q], Es[q]
                V('dve', lambda e: e.tensor_tensor(w_["t1"][:, :], pxr[:, :], ec[:, :], ALU.mult), [pxr, ec], [w_["t1"]])
                V('dve', lambda e: e.tensor_tensor(w_["t2"][:, :], pxi[:, :], es[:, :], ALU.mult), [pxi, es], [w_["t2"]])
                V('pool', lambda e: e.tensor_tensor(w_["xr"][:, :], w_["t1"][:, :], w_["t2"][:, :], ALU.add), [w_["t1"], w_["t2"]], [w_["xr"]])
                V('dve', lambda e: e.tensor_tensor(w_["t1"][:, :], pxi[:, :], ec[:, :], ALU.mult), [pxi, ec], [w_["t1"]])
                V('dve', lambda e: e.tensor_tensor(w_["t2"][:, :], pxr[:, :], es[:, :], ALU.mult), [pxr, es], [w_["t2"]])
                V('pool', lambda e: e.tensor_tensor(w_["xi"][:, :], w_["t1"][:, :], w_["t2"][:, :], ALU.subtract), [w_["t1"], w_["t2"]], [w_["xi"]])
                V('dve', lambda e: e.tensor_tensor_scan(w_["zr"][:, :], rfull[q][:, :], w_["xr"][:, :], sin_[q][:, 0:1], ALU.mult, ALU.add),
                  [rfull[q], w_["xr"], sin_[q]], [w_["zr"]])
                V('dve', lambda e: e.tensor_tensor_scan(w_["zi"][:, :], rfull[q][:, :], w_["xi"][:, :], sin_[q][:, 1:2], ALU.mult, ALU.add),
                  [rfull[q], w_["xi"], sin_[q]], [w_["zi"]])
                V('dve', lambda e: e.tensor_tensor(w_["t1"][:, :], w_["zr"][:, :], ec[:, :], ALU.mult), [w_["zr"], ec], [w_["t1"]])
                V('pool', lambda e: e.tensor_tensor(w_["t2"][:, :], w_["zi"][:, :], es[:, :], ALU.mult), [w_["zi"], es], [w_["t2"]])
                V('pool', lambda e: e.tensor_tensor(w_["sr"][:, :], w_["t1"][:, :], w_["t2"][:, :], ALU.subtract), [w_["t1"], w_["t2"]], [w_["sr"]])
                V('dve', lambda e: e.tensor_tensor(w_["t1"][:, :], w_["zr"][:, :], es[:, :], ALU.mult), [w_["zr"], es], [w_["t1"]])
                V('pool', lambda e: e.tensor_tensor(w_["t2"][:, :], w_["zi"][:, :], ec[:, :], ALU.mult), [w_["zi"], ec], [w_["t2"]])
                V('pool', lambda e: e.tensor_tensor(w_["si"][:, :], w_["t1"][:, :], w_["t2"][:, :], ALU.add), [w_["t1"], w_["t2"]], [w_["si"]])
                srb, sib = s5b["srb"][i], s5b["sib"][i]
                V('act', lambda e: e.copy(srb[:, :], w_["sr"][:, :]), [w_["sr"]], [srb])
                V('act', lambda e: e.copy(sib[:, :], w_["si"][:, :]), [w_["si"]], [sib])
                V('act', lambda e: e.copy(sin_[q][:, 0:1], w_["sr"][:, TB - 1:TB]), [w_["sr"]], [sin_[q]])
                V('act', lambda e: e.copy(sin_[q][:, 1:2], w_["si"][:, TB - 1:TB]), [w_["si"]], [sin_[q]])
                kb.mm(py, py[:, :], creb, creb[:, qs], srb, srb[:, :], q == 0, False)
                kb.mm(py, py[:, :], cimb, cimb[:, qs], sib, sib[:, :], False, q == 3)
            y, y2, y3, zz = yw
            V('dve', lambda e: e.scalar_tensor_tensor(y[:, :], u[:, :], s5dv[:, 0:1], py[:, :], ALU.mult, ALU.add), [u, s5dv, py], [y])
            V('pool', lambda e: e.tensor_tensor(y2[:, :], y[:, :], y[:, :], ALU.mult), [y], [y2])
            V('pool', lambda e: e.tensor_scalar(y2[:, :], y2[:, :], 0.044715, 1.0, ALU.mult, ALU.add), [y2], [y2])
            V('pool', lambda e: e.tensor_tensor(y3[:, :], y2[:, :], y[:, :], ALU.mult), [y2, y], [y3])
            V('act', lambda e: e.activation(y3[:, :], y3[:, :], AF.Sigmoid, scale=1.5957691216057308), [y3], [y3])
            V('dve', lambda e: e.tensor_tensor(zz[:, :], y[:, :], y3[:, :], ALU.mult), [y, y3], [zz])
            kb.dma('sp', zT[:, cs], zz[:, :], reads=[zz])

        if do_gla:
            p = proj(wsl[1], 128)
            V('act', lambda e: e.mul(q32[:, :], p[:, :], DK ** -0.5), [p], [q32])
            p = proj(wsl[2], 128)
            V('act', lambda e: e.copy(k32[:, :], p[:, :]), [p], [k32])
            for a in range(2):
                p = proj(wsl[3 + a], 128)
                V('act', lambda e: e.activation(sr_[a][:, :], p[:, :], AF.Sigmoid), [p], [sr_[a]])
                V('dve', lambda e: e.tensor_tensor(sr_[a][:, :], sr_[a][:, :], p[:, :], ALU.mult), [sr_[a], p], [sr_[a]])
            p = proj(wgb, 16, 16)
            V('act', lambda e: e.copy(glT[:, :], p[0:16, :]), [p], [glT])
            for t in range(4):
                pv = nps()
                for k in range(KC):
                    kb.mm(pv, pv[:, 0:DV], xs[k], xs[k][:, t * 128:(t + 1) * 128], wvb, wvb[:, k * DV:(k + 1) * DV], k == 0, k == KC - 1)
                V('act', lambda e: e.copy(vtk[t][:, :], pv[:, 0:DV]), [pv], [vtk[t]])
            p = nps()
            kb.mm(p, p[:, :], wg2, wg2[:, :], glT, glT[:, :], True, True)
            V('act', lambda e: e.activation(l1[:, :], p[:, :], AF.Exp, bias=nbg2[:, 0:1], scale=-1.0), [p, nbg2], [l1])
            V('dve', lambda e: e.tensor_scalar(l1[:, :], l1[:, :], 1.0, None, ALU.add), [l1], [l1])
            V('act', lambda e: e.activation(l1[:, :], l1[:, :], AF.Ln), [l1], [l1])
            V('dve', lambda e: e.tensor_tensor_scan(Bc[:, :], rmask[:, :], l1[:, :], 0.0, ALU.mult, ALU.add), [rmask, l1], [Bc])
            V('act', lambda e: e.activation(eb[:, :], Bc[:, :], AF.Exp, scale=-1.0 / 16), [Bc], [eb])
            V('act', lambda e: e.activation(enb[:, :], Bc[:, :], AF.Exp, scale=1.0 / 16), [Bc], [enb])
            V('dve', lambda e: e.tensor_tensor(qt[:, :], q32[:, :], eb[:, :], ALU.mult), [q32, eb], [qt])
            V('dve', lambda e: e.tensor_tensor(k32[:, :], k32[:, :], enb[:, :], ALU.mult), [k32, enb], [k32])
            V('act', lambda e: e.copy(kt[:, :], k32[:, :]), [k32], [kt])
            yo = ybo
            for c_ in range(4):
                ch = slice(c_ * 128, (c_ + 1) * 128)
                el = eb[:, c_ * 128 + 127:c_ * 128 + 128]
                V('dve', lambda e: e.tensor_scalar(kdT[:, :], k32[:, ch], el, None, ALU.mult), [k32, eb], [kdT])
                V('pe', lambda e: e.transpose(pT[:, :], kdT[:, :], identb[:, :]), [kdT, identb], [pT])
                V('act', lambda e: e.copy(kd[:, :], pT[:, :]), [pT], [kd])
                kb.mm(pA, pA[:, :], kt, kt[:, ch], qt, qt[:, ch], True, True)
                V('dve', lambda e: e.tensor_tensor(AT[:, :], pA[:, :], cmask[:, :], ALU.mult), [pA, cmask], [AT])
                for a in range(2):
                    kb.mm(pO[a], pO[a][:, :], vtk[c_], vtk[c_][:, a * 128:(a + 1) * 128], AT, AT[:, :], True, False)
                    kb.mm(pO[a], pO[a][:, :], Sb, Sb[:, a * 128:(a + 1) * 128], qt, qt[:, ch], False, True)
                kb.mm(pKV, pKV[:, :], kd, kd[:, :], vtk[c_], vtk[c_][:, :], True, True)
                V('dve', lambda e: e.scalar_tensor_tensor(S[:, :], S[:, :], el, pKV[:, :], ALU.mult, ALU.add), [S, eb, pKV], [S])
                V('act', lambda e: e.copy(Sb[:, :], S[:, :]), [S], [Sb])
                for a in range(2):
                    V('act', lambda e: e.activation(sq[a][:, :], pO[a][:, :], AF.Square), [pO[a]], [sq[a]])
                pn = nps()
                for a in range(2):
                    kb.mm(pn, pn[:, 0:128], onesb, onesb[:, :], sq[a], sq[a][:, :], a == 0, a == 1)
                V('dve', lambda e: e.tensor_scalar(rs[:, :], pn[:, 0:128], 1.0 / DV, GLA_EPS, ALU.mult, ALU.add), [pn], [rs])
                V('act', lambda e: e.activation(rs[:, :], rs[:, :], AF.Sqrt), [rs], [rs])
                V('dve', lambda e: e.reciprocal(rs[:, :], rs[:, :]), [rs], [rs])
                for a in range(2):
                    V('dve', lambda e: e.scalar_tensor_tensor(on[a][:, :], pO[a][:, :], ng[:, a:a + 1], rs[:, :], ALU.mult, ALU.mult),
                      [pO[a], ng, rs], [on[a]])
                    V('pool', lambda e: e.tensor_tensor(yo[a][:, ch], on[a][:, :], sr_[a][:, ch], ALU.mult), [on[a], sr_[a]], [yo[a]])
            for a in range(2):
                kb.dma('sp', ybT[a * 128:(a + 1) * 128, cs], yo[a][:, :], reads=[yo[a]])
    kb.finish()
    return kb

import ml_dtypes
from concourse.bass_utils import run_bass_kernel_spmd

NCORES = 8
_PROGS = {}


def _prog(key, fn):
    if key not in _PROGS:
        _PROGS[key] = fn()
    return _PROGS[key]


def _run(kb, ins):
    res = run_bass_kernel_spmd(kb.nc, ins, core_ids=list(range(NCORES)))
    return res.results


def kernel(**inp):
    x = np.asarray(inp["x"])[0]
    NT, D = x.shape
    KC = D // 128
    NTK = NT // NCORES
    F = inp["moe_w_gu"].shape[-1] // 2
    NE = inp["moe_w_gu"].shape[1]
    EL = NE // NCORES
    xT = np.ascontiguousarray(x.T)
    ones512 = np.ones((128, 512), np.float32)
    ones128 = np.ones((128, 128), np.float32)
    ident = np.eye(128, dtype=np.float32)
    ts = [slice(c * NTK, (c + 1) * NTK) for c in range(NCORES)]
    sh = lambda a, c: np.ascontiguousarray(a[:, ts[c]])

    w_in = inp["ab_w_in"][0]
    s1 = 1024
    s2 = s1 + 512
    s3 = s2 + 512
    s4 = s3 + 1024
    s5_ = s4 + 16
    kbA = _prog(("mix", D, NT), lambda: build_mix(D, NT))
    insA = []
    for c in range(NCORES):
        h = c // 2
        sl = lambda a, b: wslab(w_in[:, a:b], KC, 1)[0]
        w5 = np.stack([sl(128 * c, 128 * c + 128), sl(s1 + 128 * h, s1 + 128 * h + 128), sl(s2 + 128 * h, s2 + 128 * h + 128),
                       sl(s5_ + 256 * h, s5_ + 256 * h + 128), sl(s5_ + 256 * h + 128, s5_ + 256 * h + 256)])
        wv = np.ascontiguousarray(w_in[:, s3 + 256 * h:s3 + 256 * h + 256].reshape(KC, 128, 256).transpose(1, 0, 2).reshape(128, KC * 256))
        wg = np.ascontiguousarray(w_in[:, s4:s4 + 16].reshape(KC, 128, 16).transpose(1, 0, 2).reshape(128, KC * 16))
        d = {"xT": xT, "w5": w5, "wv": wv, "wg": wg,
             "wg2": np.ascontiguousarray(inp["gla_w_gate2"][0][:, 128 * h:128 * h + 128]),
             "bg2": np.ascontiguousarray(inp["gla_b_gate2"][0][128 * h:128 * h + 128].reshape(128, 1)),
             "ng": np.ascontiguousarray(inp["gla_norm_g"][0].reshape(2, 128).T),
             "cmask": np.triu(np.ones((128, 128), np.float32)),
             "rmask": np.ascontiguousarray((np.arange(512) % 128 != 0).astype(np.float32)[None, :].repeat(128, 0)),
             "ones": ones512, "ident": ident}
        d.update(s5_host(inp["s5_lam_re"][0], inp["s5_lam_im"][0], inp["s5_log_step"][0], inp["s5_b_re"][0], inp["s5_b_im"][0],
                         inp["s5_c_re"][0], inp["s5_c_im"][0], inp["s5_d"][0], c))
        insA.append(d)
    rA = _run(kbA, insA)
    feat = np.concatenate([rA[c]["zT"] for c in range(NCORES)] + [rA[2 * h]["ybT"] for h in range(4)], 0)
    del rA, insA

    def post(feat, xin_sh, layer, wout, glu):
        kbB = _prog(("post", D, NTK, glu), lambda: build_post(D, NTK, glu, NE))
        wo = wslab(wout, KC, KC)
        wr = np.ascontiguousarray(inp["moe_w_router"][layer].reshape(KC, 128, NE).transpose(1, 0, 2).reshape(128, KC * NE))
        brb = np.ascontiguousarray(np.tile(inp["moe_b_router"][layer][None, :], (128, 1)))
        ins = []
        for c in range(NCORES):
            d = {"feat": sh(feat, c), "wout": wo, "xin": xin_sh[c], "lng": pp(inp["ln1_g"][layer], KC), "lnb": pp(inp["ln1_b"][layer], KC),
                 "ones": ones128, "wr": wr, "brb": brb}
            if glu:
                d["wglu"] = wslab(inp["s5_w_glu"][0], KC // 2, KC // 2)
                d["bglu"] = pp(inp["s5_b_glu"][0], KC // 2)
            ins.append(d)
        r = _run(kbB, ins)
        return [r[c]["xo"] for c in range(NCORES)], np.concatenate([r[c]["xob"] for c in range(NCORES)], 1), \
            np.concatenate([r[c]["gout"] for c in range(NCORES)], 0)

    def moe_ln(x1_sh, x1b, G, layer):
        kbM = _prog(("moe", D, F, NT, EL), lambda: build_moe(D, F, NT, EL, TB=1024))
        ins = [moe_host_inputs(x1b, G, inp["moe_w_gu"][layer], inp["moe_b_gu"][layer], inp["moe_w_down"][layer],
                               inp["moe_b_down"][layer], c, EL) for c in range(NCORES)]
        r = _run(kbM, ins)
        del ins
        parts = [r[c]["y"] for c in range(NCORES)]
        kbN = _prog(("sumln", D, NTK), lambda: build_sumln(D, NTK, NCORES))
        ins = [{"xin": x1_sh[c], "part": np.ascontiguousarray(np.stack([p[:, ts[c]] for p in parts])),
                "lng": pp(inp["ln2_g"][layer], KC), "lnb": pp(inp["ln2_b"][layer], KC), "ones": ones128} for c in range(NCORES)]
        r = _run(kbN, ins)
        return [r[c]["xo"] for c in range(NCORES)], np.concatenate([r[c]["xob"] for c in range(NCORES)], 1)

    x_sh = [sh(xT, c) for c in range(NCORES)]
    x1_sh, x1b, G = post(feat, x_sh, 0, inp["ab_w_out"][0], True)
    x2_sh, x2b = moe_ln(x1_sh, x1b, G, 0)

    NH = D // 128
    HPC = NH // NCORES
    wqkv = inp["c_w_qkv"][0].reshape(D, 3, NH, 128)
    kbC = _prog(("attn", D, NT, HPC), lambda: build_attn(D, NT, HPC))
    mbias = attn_mask_bias()
    identb = ident.astype(ml_dtypes.bfloat16)
    insC = []
    for c in range(NCORES):
        d = {"x2b": x2b, "mb": mbias, "ident": identb}
        for i, nm in enumerate(("wq", "wk", "wv")):
            w = np.ascontiguousarray(wqkv[:, i, c * HPC:(c + 1) * HPC, :].reshape(D, HPC * 128))
            d[nm] = wslab(w, KC, HPC)
        insC.append(d)
    rC = _run(kbC, insC)
    attn = np.concatenate([rC[c]["o"] for c in range(NCORES)], 1)
    feat1 = np.ascontiguousarray(attn.T)
    del rC, insC
    x3_sh, x3b, G1 = post(feat1, x2_sh, 1, inp["c_w_o"][0], False)
    x4_sh, _ = moe_ln(x3_sh, x3b, G1, 1)
    out = np.concatenate(x4_sh, 1)
    return np.ascontiguousarray(out.T)[None].astype(np.float32)
```

```python
import contextlib
import numpy as np
import concourse.bass as bass
import concourse.mybir as mybir

F32 = mybir.dt.float32
BF16 = mybir.dt.bfloat16
ALU = mybir.AluOpType
AF = mybir.ActivationFunctionType
AX = mybir.AxisListType


class T:
    def __init__(self, t):
        self.t = t
        self.w = None
        self.r = {}

    def __getitem__(self, idx):
        return self.t[idx]


class KB:
    EPOCH = 30000

    def __init__(self):
        self.nc = bass.Bass("TRN2", target_bir_lowering=False)
        self.st = contextlib.ExitStack()
        nc = self.nc
        self.E = {'pe': nc.tensor, 'act': nc.scalar, 'dve': nc.vector, 'pool': nc.gpsimd, 'sp': nc.sync}
        self.semobj = {}
        self.cur = {}
        self.cnt = {}
        self.epoch = {}
        for e in ('pe', 'act', 'dve', 'pool'):
            self.epoch[e] = 0
            self._new_epoch(e)
        self.waited = {}
        self.dq = {}
        self.dq_i = {}
        self.dval = {}
        self.nsb = 0
        self.scope = None

    def _new_epoch(self, e):
        key = f"{e}{self.epoch[e]}"
        self.epoch[e] += 1
        self.semobj[key] = self.st.enter_context(self.nc.semaphore("s_" + key))
        self.cur[e] = key
        self.cnt[e] = 0

    def dma_queue(self, q, nsem=8):
        keys = []
        for i in range(nsem):
            key = f"d_{q}_{i}"
            self.semobj[key] = self.st.enter_context(self.nc.semaphore(key))
            self.dval[key] = 0
            keys.append(key)
        self.dq[q] = keys
        self.dq_i[q] = 0

    def sb(self, shape, dt, name=None):
        self.nsb += 1
        st = self.scope if self.scope is not None else self.st
        return T(st.enter_context(self.nc.sbuf_tensor(name or f"sb{self.nsb}", list(shape), dt)))

    def push_scope(self):
        assert self.scope is None
        self.scope = contextlib.ExitStack()

    def barrier(self):
        engs = ('pe', 'act', 'dve', 'pool', 'sp')
        for e in engs:
            for o in ('pe', 'act', 'dve', 'pool'):
                if o != e:
                    self._wait(e, self.cur[o], self.cnt[o])
            for q in self.dq:
                for key in self.dq[q]:
                    self._wait(e, key, self.dval[key])

    def pop_scope(self):
        self.barrier()
        self.scope.close()
        self.scope = None

    def ps(self, shape=(128, 512), dt=F32, name=None):
        self.nsb += 1
        return T(self.st.enter_context(self.nc.psum_tensor(name or f"ps{self.nsb}", list(shape), dt)))

    def dram(self, name, shape, dt, kind="Internal"):
        if kind == "Internal":
            return self.nc.dram_tensor(name, list(shape), dt)
        return self.nc.dram_tensor(name, list(shape), dt, kind=kind)

    def _wait(self, eng, semkey, val):
        if self.waited.get((eng, semkey), 0) >= val:
            return
        self.E[eng].wait_ge(self.semobj[semkey], val)
        self.waited[(eng, semkey)] = val

    def _deps(self, eng, reads, writes):
        deps = {}

        def add(ev):
            if ev is None:
                return
            k, v = ev
            if deps.get(k, 0) < v:
                deps[k] = v
        for t in reads:
            add(t.w)
        for t in writes:
            add(t.w)
            for k, v in t.r.items():
                add((k, v))
        for k, v in deps.items():
            if eng == 'pe' and k.startswith('pe'):
                continue
            self._wait(eng, k, v)

    def _mark(self, ev, reads, writes):
        k, v = ev
        for t in reads:
            if t.r.get(k, 0) < v:
                t.r[k] = v
        for t in writes:
            t.w = ev
            t.r = {}

    def op(self, eng, fn, reads=(), writes=()):
        if self.cnt[eng] >= self.EPOCH:
            self._new_epoch(eng)
        self._deps(eng, reads, writes)
        inst = fn(self.E[eng])
        self.cnt[eng] += 1
        key = self.cur[eng]
        inst.then_inc(self.semobj[key], 1)
        self._mark((key, self.cnt[eng]), reads, writes)
        return inst

    def dma(self, q, out, in_, reads=(), writes=(), **kw):
        self._deps(q, reads, writes)
        keys = self.dq[q]
        key = keys[self.dq_i[q] % len(keys)]
        self.dq_i[q] += 1
        self._wait(q, key, self.dval[key])
        self.E[q].dma_start(out=out, in_=in_, **kw).then_inc(self.semobj[key], 16)
        self.dval[key] += 16
        self._mark((key, self.dval[key]), reads, writes)

    def finish(self):
        for q in self.dq:
            for key in self.dq[q]:
                self._wait(q if q in self.E else 'sp', key, self.dval[key])
        for e in ('pe', 'act', 'dve', 'pool'):
            self._wait('sp', self.cur[e], self.cnt[e])
        self.st.close()

    def mm(self, out_t, out_ap, lhsT_t, lhsT_ap, rhs_t, rhs_ap, start, stop):
        return self.op('pe', lambda e: e.matmul(out_ap, lhsT_ap, rhs_ap, start=start, stop=stop),
                       reads=[lhsT_t, rhs_t], writes=[out_t])


def build_moe(D, F, NT, EL, TB=1024):
    kb = KB()
    nc = kb.nc
    KC = D // 128
    FC = F // 128
    NSB = TB // 512
    NB = NT // TB
    x1b = kb.dram("x1b", [D, NT], BF16, "ExternalInput")
    gt = kb.dram("gt", [EL, NT], F32, "ExternalInput")
    wgu = kb.dram("wgu", [EL, 2 * FC, 128, KC * 128], F32, "ExternalInput")
    bgu = kb.dram("bgu", [128, EL * 2 * FC], F32, "ExternalInput")
    wd = kb.dram("wd", [EL, KC, 128, FC * 128], F32, "ExternalInput")
    bd = kb.dram("bd", [EL, D], F32, "ExternalInput")
    sel = kb.dram("sel", [EL, EL * 128], F32, "ExternalInput")
    y = kb.dram("y", [D, NT], F32, "ExternalOutput")
    kb.dma_queue('sp', 8)

    xb = [kb.sb([128, TB], BF16) for _ in range(KC)]
    acc = [kb.sb([128, TB], F32) for _ in range(KC)]
    act = [kb.sb([128, TB], BF16) for _ in range(FC)]
    gb = kb.sb([128, TB], F32)
    gts = kb.sb([EL, TB], F32)
    bgu_s = kb.sb([128, EL * 2 * FC], F32)
    bd_s = kb.sb([EL, D], F32)
    sel_s = kb.sb([EL, EL * 128], F32)
    NSTG = 3
    stg = [kb.sb([128, max(KC, FC) * 128], F32) for _ in range(NSTG)]
    NWB = 4
    wgb = [kb.sb([128, KC * 128], BF16) for _ in range(NWB)]
    wdb = [kb.sb([128, FC * 128], BF16) for _ in range(2)]
    g32 = [kb.sb([128, 512], F32) for _ in range(2)]
    sig = [kb.sb([128, 512], F32) for _ in range(2)]
    u1 = [kb.sb([128, 512], F32) for _ in range(2)]
    pg = [kb.ps() for _ in range(2)]
    pu = [kb.ps() for _ in range(2)]
    pd = [kb.ps() for _ in range(2)]
    pm = [kb.ps() for _ in range(2)]

    kb.dma('sp', bgu_s[:, :], bgu[:, :], writes=[bgu_s])
    kb.dma('sp', bd_s[:, :], bd[:, :], writes=[bd_s])
    kb.dma('sp', sel_s[:, :], sel[:, :], writes=[sel_s])

    cnt = {'stg': 0, 'wg': 0, 'wd': 0, 'u': 0, 'pd': 0, 'pm': 0}

    def load_slab(src_ap, ncols, dst):
        s = stg[cnt['stg'] % NSTG]
        cnt['stg'] += 1
        kb.dma('sp', s[:, 0:ncols], src_ap, writes=[s])
        kb.op('act', lambda e: e.copy(dst[:, 0:ncols], s[:, 0:ncols]), reads=[s], writes=[dst])

    for b in range(NB):
        t0 = b * TB
        for k in range(KC):
            kb.dma('sp', xb[k][:, :], x1b[k * 128:(k + 1) * 128, t0:t0 + TB], writes=[xb[k]])
        kb.dma('sp', gts[:, :], gt[:, t0:t0 + TB], writes=[gts])
        for e in range(EL):
            for sb_ in range(NSB):
                p = pm[cnt['pm'] % 2]
                cnt['pm'] += 1
                kb.mm(p, p[:, :], sel_s, sel_s[:, e * 128:(e + 1) * 128], gts, gts[:, sb_ * 512:(sb_ + 1) * 512], True, True)
                kb.op('act', lambda en, p=p, sb_=sb_: en.copy(gb[:, sb_ * 512:(sb_ + 1) * 512], p[:, :]), reads=[p], writes=[gb])
            for fc in range(FC):
                wg_ = wgb[cnt['wg'] % NWB]
                wu_ = wgb[(cnt['wg'] + 1) % NWB]
                cnt['wg'] += 2
                load_slab(wgu[e, fc, :, :], KC * 128, wg_)
                load_slab(wgu[e, FC + fc, :, :], KC * 128, wu_)
                bg_ap = bgu_s[:, e * 2 * FC + fc: e * 2 * FC + fc + 1]
                bu_ap = bgu_s[:, e * 2 * FC + FC + fc: e * 2 * FC + FC + fc + 1]
                for sb_ in range(NSB):
                    i = cnt['u'] % 2
                    cnt['u'] += 1
                    cs = slice(sb_ * 512, (sb_ + 1) * 512)
                    for k in range(KC):
                        kb.mm(pg[i], pg[i][:, :], wg_, wg_[:, k * 128:(k + 1) * 128], xb[k], xb[k][:, cs], k == 0, k == KC - 1)
                    for k in range(KC):
                        kb.mm(pu[i], pu[i][:, :], wu_, wu_[:, k * 128:(k + 1) * 128], xb[k], xb[k][:, cs], k == 0, k == KC - 1)
                    kb.op('dve', lambda en, i=i: en.tensor_scalar(g32[i][:, :], pg[i][:, :], bg_ap, 7.0, ALU.add, ALU.min),
                          reads=[pg[i], bgu_s], writes=[g32[i]])
                    kb.op('act', lambda en, i=i: en.activation(sig[i][:, :], g32[i][:, :], AF.Sigmoid, scale=1.702),
                          reads=[g32[i]], writes=[sig[i]])
                    kb.op('dve', lambda en, i=i: en.tensor_scalar(u1[i][:, :], pu[i][:, :], bu_ap, 7.0, ALU.add, ALU.min),
                          reads=[pu[i], bgu_s], writes=[u1[i]])
                    kb.op('dve', lambda en, i=i: en.tensor_scalar(u1[i][:, :], u1[i][:, :], -7.0, 1.0, ALU.max, ALU.add),
                          reads=[u1[i]], writes=[u1[i]])
                    kb.op('pool', lambda en, i=i: en.tensor_tensor(g32[i][:, :], g32[i][:, :], sig[i][:, :], ALU.mult),
                          reads=[g32[i], sig[i]], writes=[g32[i]])
                    kb.op('pool', lambda en, i=i: en.tensor_tensor(g32[i][:, :], g32[i][:, :], u1[i][:, :], ALU.mult),
                          reads=[g32[i], u1[i]], writes=[g32[i]])
                    kb.op('dve', lambda en, i=i, cs=cs: en.tensor_tensor(act[fc][:, cs], g32[i][:, :], gb[:, cs], ALU.mult),
                          reads=[g32[i], gb], writes=[act[fc]])
            for dc in range(KC):
                w_ = wdb[cnt['wd'] % 2]
                cnt['wd'] += 1
                load_slab(wd[e, dc, :, :], FC * 128, w_)
                for sb_ in range(NSB):
                    cs = slice(sb_ * 512, (sb_ + 1) * 512)
                    p = pd[cnt['pd'] % 2]
                    cnt['pd'] += 1
                    for f in range(FC):
                        kb.mm(p, p[:, :], w_, w_[:, f * 128:(f + 1) * 128], act[f], act[f][:, cs], f == 0, f == FC - 1)
                    if e == 0:
                        q = pm[cnt['pm'] % 2]
                        cnt['pm'] += 1
                        kb.mm(q, q[:, :], bd_s, bd_s[:, dc * 128:(dc + 1) * 128], gts, gts[:, cs], True, True)
                        kb.op('act', lambda en, q=q, cs=cs: en.copy(acc[dc][:, cs], q[:, :]), reads=[q], writes=[acc[dc]])
                    kb.op('dve', lambda en, p=p, cs=cs: en.tensor_tensor(acc[dc][:, cs], p[:, :], acc[dc][:, cs], ALU.add),
                          reads=[p, acc[dc]], writes=[acc[dc]])
        for dc in range(KC):
            kb.dma('sp', y[dc * 128:(dc + 1) * 128, t0:t0 + TB], acc[dc][:, :], reads=[acc[dc]])
    kb.finish()
    return kb


def moe_host_inputs(x1T, G, w_gu, b_gu, w_down, b_down, c, EL):
    import ml_dtypes
    D = w_gu.shape[1]
    F2 = w_gu.shape[2]
    F = F2 // 2
    KC = D // 128
    FC = F // 128
    es = slice(c * EL, (c + 1) * EL)
    wg = w_gu[es].reshape(EL, KC, 128, 2 * FC, 128).transpose(0, 3, 2, 1, 4).reshape(EL, 2 * FC, 128, KC * 128)
    wdl = w_down[es].reshape(EL, FC, 128, KC, 128).transpose(0, 3, 2, 1, 4).reshape(EL, KC, 128, FC * 128)
    bg = b_gu[es].reshape(EL, 2 * FC, 128).transpose(2, 0, 1).reshape(128, EL * 2 * FC)
    sel = np.zeros((EL, EL * 128), np.float32)
    for e in range(EL):
        sel[e, e * 128:(e + 1) * 128] = 1.0
    return {
        "x1b": x1T,
        "gt": np.ascontiguousarray(G[:, es].T),
        "wgu": np.ascontiguousarray(wg),
        "bgu": np.ascontiguousarray(bg),
        "wd": np.ascontiguousarray(wdl),
        "bd": np.ascontiguousarray(b_down[es]),
        "sel": sel,
    }


DN_ALPHA = 4 ** 0.25
LN_EPS = 1e-5


def ln_fm(kb, r, out32, outb, g_s, b_s, ones32, D, NTK, tmp, pss):
    KC = len(r)
    for sb_ in range(NTK // 512):
        cs = slice(sb_ * 512, (sb_ + 1) * 512)
        ps_sum, ps_sq = pss
        for k in range(KC):
            kb.mm(ps_sum, ps_sum[:, :], ones32, ones32[:, :], r[k], r[k][:, cs], k == 0, k == KC - 1)
        for k in range(KC):
            sq = tmp['sq'][k % 2]
            kb.op('act', lambda e: e.activation(sq[:, :], r[k][:, cs], AF.Square), reads=[r[k]], writes=[sq])
            kb.mm(ps_sq, ps_sq[:, :], ones32, ones32[:, :], sq, sq[:, :], k == 0, k == KC - 1)
        mean, msq, rstd = tmp['mean'], tmp['msq'], tmp['rstd']
        kb.op('act', lambda e: e.mul(mean[:, :], ps_sum[:, :], 1.0 / D), reads=[ps_sum], writes=[mean])
        kb.op('dve', lambda e: e.tensor_tensor(msq[:, :], mean[:, :], mean[:, :], ALU.mult), reads=[mean], writes=[msq])
        kb.op('dve', lambda e: e.scalar_tensor_tensor(msq[:, :], ps_sq[:, :], 1.0 / D, msq[:, :], ALU.mult, ALU.subtract),
              reads=[ps_sq, msq], writes=[msq])
        kb.op('dve', lambda e: e.tensor_scalar(msq[:, :], msq[:, :], LN_EPS, None, ALU.add), reads=[msq], writes=[msq])
        kb.op('act', lambda e: e.activation(rstd[:, :], msq[:, :], AF.Sqrt), reads=[msq], writes=[rstd])
        kb.op('dve', lambda e: e.reciprocal(rstd[:, :], rstd[:, :]), reads=[rstd], writes=[rstd])
        for k in range(KC):
            kb.op('pool', lambda e: e.tensor_tensor(r[k][:, cs], r[k][:, cs], mean[:, :], ALU.subtract),
                  reads=[r[k], mean], writes=[r[k]])
            kb.op('dve', lambda e: e.tensor_tensor(r[k][:, cs], r[k][:, cs], rstd[:, :], ALU.mult),
                  reads=[r[k], rstd], writes=[r[k]])
            dst = out32[k] if out32 is not None else r[k]
            kb.op('dve', lambda e: e.tensor_scalar(dst[:, cs], r[k][:, cs], g_s[:, k:k + 1], b_s[:, k:k + 1], ALU.mult, ALU.add),
                  reads=[r[k], g_s, b_s], writes=[dst])
            if outb is not None:
                kb.op('act', lambda e: e.copy(outb[k][:, cs], dst[:, cs]), reads=[dst], writes=[outb[k]])


def build_sumln(D, NTK, NP):
    kb = KB()
    KC = D // 128
    xin = kb.dram("xin", [D, NTK], F32, "ExternalInput")
    part = kb.dram("part", [NP, D, NTK], F32, "ExternalInput")
    lng = kb.dram("lng", [128, KC], F32, "ExternalInput")
    lnb = kb.dram("lnb", [128, KC], F32, "ExternalInput")
    ones = kb.dram("ones", [128, 128], F32, "ExternalInput")
    xo = kb.dram("xo", [D, NTK], F32, "ExternalOutput")
    xob = kb.dram("xob", [D, NTK], BF16, "ExternalOutput")
    kb.dma_queue('sp', 8)
    r = [kb.sb([128, NTK], F32) for _ in range(KC)]
    ob = [kb.sb([128, NTK], BF16) for _ in range(2)]
    pb = [kb.sb([128, NTK], F32) for _ in range(3)]
    g_s = kb.sb([128, KC], F32)
    b_s = kb.sb([128, KC], F32)
    ones_s = kb.sb([128, 128], F32)
    tmp = {'sq': [kb.sb([128, 512], F32) for _ in range(2)], 'mean': kb.sb([128, 512], F32),
           'msq': kb.sb([128, 512], F32), 'rstd': kb.sb([128, 512], F32)}
    pss = [kb.ps(), kb.ps()]
    kb.dma('sp', g_s[:, :], lng[:, :], writes=[g_s])
    kb.dma('sp', b_s[:, :], lnb[:, :], writes=[b_s])
    kb.dma('sp', ones_s[:, :], ones[:, :], writes=[ones_s])
    n = 0
    for k in range(KC):
        rows = slice(k * 128, (k + 1) * 128)
        kb.dma('sp', r[k][:, :], xin[rows, :], writes=[r[k]])
        for c in range(NP):
            p = pb[n % 3]
            n += 1
            kb.dma('sp', p[:, :], part[c, rows, :], writes=[p])
            eng = 'dve' if c % 2 == 0 else 'pool'
            if c == 0:
                kb.op('dve', lambda e: e.scalar_tensor_tensor(r[k][:, :], r[k][:, :], DN_ALPHA, p[:, :], ALU.mult, ALU.add),
                      reads=[r[k], p], writes=[r[k]])
            else:
                kb.op(eng, lambda e: e.tensor_tensor(r[k][:, :], r[k][:, :], p[:, :], ALU.add), reads=[r[k], p], writes=[r[k]])
    ln_fm(kb, r, None, None, g_s, b_s, ones_s, D, NTK, tmp, pss)
    for k in range(KC):
        rows = slice(k * 128, (k + 1) * 128)
        kb.dma('sp', xo[rows, :], r[k][:, :], reads=[r[k]])
        o = ob[k % 2]
        kb.op('act', lambda e: e.copy(o[:, :], r[k][:, :]), reads=[r[k]], writes=[o])
        kb.dma('sp', xob[rows, :], o[:, :], reads=[o])
    kb.finish()
    return kb


def pp(v, KC):
    return np.ascontiguousarray(v.reshape(KC, 128).T)


def build_post(D, NTK, has_glu, NEXP=32):
    kb = KB()
    KC = D // 128
    HC = KC // 2
    feat = kb.dram("feat", [D, NTK], F32, "ExternalInput")
    wout = kb.dram("wout", [KC, 128, KC * 128], F32, "ExternalInput")
    if has_glu:
        wglu = kb.dram("wglu", [HC, 128, HC * 128], F32, "ExternalInput")
        bglu = kb.dram("bglu", [128, HC], F32, "ExternalInput")
    xin = kb.dram("xin", [D, NTK], F32, "ExternalInput")
    lng = kb.dram("lng", [128, KC], F32, "ExternalInput")
    lnb = kb.dram("lnb", [128, KC], F32, "ExternalInput")
    ones = kb.dram("ones", [128, 128], F32, "ExternalInput")
    wr = kb.dram("wr", [128, KC * NEXP], F32, "ExternalInput")
    brb = kb.dram("brb", [128, NEXP], F32, "ExternalInput")
    xo = kb.dram("xo", [D, NTK], F32, "ExternalOutput")
    xob = kb.dram("xob", [D, NTK], BF16, "ExternalOutput")
    gout = kb.dram("gout", [NTK, NEXP], F32, "ExternalOutput")
    kb.dma_queue('sp', 8)
    fb = [kb.sb([128, NTK], BF16) for _ in range(KC)]
    zb = [kb.sb([128, NTK], BF16) for _ in range(HC)] if has_glu else None
    r = [kb.sb([128, NTK], F32) for _ in range(KC)]
    ob = [kb.sb([128, NTK], BF16) for _ in range(2)]
    st32 = [kb.sb([128, NTK], F32) for _ in range(3)]
    stg = [kb.sb([128, KC * 128], F32) for _ in range(3)]
    wb = [kb.sb([128, KC * 128], BF16) for _ in range(2)]
    g_s = kb.sb([128, KC], F32)
    b_s = kb.sb([128, KC], F32)
    ones_s = kb.sb([128, 128], F32)
    wr_s = kb.sb([128, KC * NEXP], F32)
    brb_s = kb.sb([128, NEXP], F32)
    sg = [kb.sb([128, 512], F32) for _ in range(2)]
    tmp = {'sq': [kb.sb([128, 512], F32) for _ in range(2)], 'mean': kb.sb([128, 512], F32),
           'msq': kb.sb([128, 512], F32), 'rstd': kb.sb([128, 512], F32)}
    pss = [kb.ps(), kb.ps()]
    pmm = [kb.ps(), kb.ps()]
    prt = kb.ps([128, NEXP])
    for s_, d_ in ((g_s, lng), (b_s, lnb), (ones_s, ones), (wr_s, wr), (brb_s, brb)):
        kb.dma('sp', s_[:, :], d_[:, :], writes=[s_])
    if has_glu:
        bglu_s = kb.sb([128, HC], F32)
        kb.dma('sp', bglu_s[:, :], bglu[:, :], writes=[bglu_s])
    c = {'s': 0, 'w': 0, 'p': 0, 'g': 0}

    def load_cast(src_ap, dst, n):
        s = st32[c['s'] % 3]
        c['s'] += 1
        kb.dma('sp', s[:, 0:n], src_ap, writes=[s])
        kb.op('act', lambda e: e.copy(dst[:, 0:n], s[:, 0:n]), reads=[s], writes=[dst])

    def load_w(src_ap, n):
        s = stg[c['w'] % 3]
        w = wb[c['w'] % 2]
        c['w'] += 1
        kb.dma('sp', s[:, 0:n], src_ap, writes=[s])
        kb.op('act', lambda e: e.copy(w[:, 0:n], s[:, 0:n]), reads=[s], writes=[w])
        return w

    for k in range(KC):
        dst = zb[k] if (has_glu and k < HC) else fb[k]
        load_cast(feat[k * 128:(k + 1) * 128, :], dst, NTK)
        kb.dma('sp', r[k][:, :], xin[k * 128:(k + 1) * 128, :], writes=[r[k]])
    if has_glu:
        for fo in range(HC):
            w = load_w(wglu[fo, :, :], HC * 128)
            for sb_ in range(NTK // 512):
                cs = slice(sb_ * 512, (sb_ + 1) * 512)
                p = pmm[c['p'] % 2]
                c['p'] += 1
                for k in range(HC):
                    kb.mm(p, p[:, :], w, w[:, k * 128:(k + 1) * 128], zb[k], zb[k][:, cs], k == 0, k == HC - 1)
                s = sg[c['g'] % 2]
                c['g'] += 1
                kb.op('act', lambda e: e.activation(s[:, :], p[:, :], AF.Sigmoid, bias=bglu_s[:, fo:fo + 1]),
                      reads=[p, bglu_s], writes=[s])
                kb.op('dve', lambda e: e.tensor_tensor(fb[fo][:, cs], zb[fo][:, cs], s[:, :], ALU.mult),
                      reads=[zb[fo], s], writes=[fb[fo]])
    for dc in range(KC):
        w = load_w(wout[dc, :, :], KC * 128)
        for sb_ in range(NTK // 512):
            cs = slice(sb_ * 512, (sb_ + 1) * 512)
            p = pmm[c['p'] % 2]
            c['p'] += 1
            for k in range(KC):
                kb.mm(p, p[:, :], w, w[:, k * 128:(k + 1) * 128], fb[k], fb[k][:, cs], k == 0, k == KC - 1)
            kb.op('dve', lambda e: e.scalar_tensor_tensor(r[dc][:, cs], r[dc][:, cs], DN_ALPHA, p[:, :], ALU.mult, ALU.add),
                  reads=[r[dc], p], writes=[r[dc]])
    ln_fm(kb, r, None, None, g_s, b_s, ones_s, D, NTK, tmp, pss)
    for k in range(KC):
        rows = slice(k * 128, (k + 1) * 128)
        kb.dma('sp', xo[rows, :], r[k][:, :], reads=[r[k]])
        o = ob[k % 2]
        kb.op('act', lambda e: e.copy(o[:, :], r[k][:, :]), reads=[r[k]], writes=[o])
        kb.dma('sp', xob[rows, :], o[:, :], reads=[o])
    lg = [kb.sb([128, NEXP], F32) for _ in range(2)]
    ee = [kb.sb([128, NEXP], F32) for _ in range(2)]
    t8 = [kb.sb([128, 8], F32) for _ in range(2)]
    sc = [kb.sb([128, 4], F32) for _ in range(2)]
    for tt in range(NTK // 128):
        ts_ = slice(tt * 128, (tt + 1) * 128)
        i = tt % 2
        for k in range(KC):
            kb.mm(prt, prt[:, :], r[k], r[k][:, ts_], wr_s, wr_s[:, k * NEXP:(k + 1) * NEXP], k == 0, k == KC - 1)
        kb.op('dve', lambda e: e.tensor_tensor(lg[i][:, :], prt[:, :], brb_s[:, :], ALU.add), reads=[prt, brb_s], writes=[lg[i]])
        kb.op('dve', lambda e: e.max(t8[i][:, :], lg[i][:, :]), reads=[lg[i]], writes=[t8[i]])
        kb.op('dve', lambda e: e.tensor_scalar(sc[i][:, 0:1], t8[i][:, 0:1], -1.0, None, ALU.mult), reads=[t8[i]], writes=[sc[i]])
        kb.op('act', lambda e: e.activation(ee[i][:, :], lg[i][:, :], AF.Exp, bias=sc[i][:, 0:1]), reads=[lg[i], sc[i]], writes=[ee[i]])
        kb.op('dve', lambda e: e.tensor_scalar(lg[i][:, :], lg[i][:, :], t8[i][:, 3:4], None, ALU.is_ge), reads=[lg[i], t8[i]], writes=[lg[i]])
        kb.op('dve', lambda e: e.tensor_tensor(ee[i][:, :], ee[i][:, :], lg[i][:, :], ALU.mult), reads=[ee[i], lg[i]], writes=[ee[i]])
        kb.op('dve', lambda e: e.reduce_sum(sc[i][:, 1:2], ee[i][:, :], AX.X), reads=[ee[i]], writes=[sc[i]])
        kb.op('dve', lambda e: e.reciprocal(sc[i][:, 2:3], sc[i][:, 1:2]), reads=[sc[i]], writes=[sc[i]])
        kb.op('dve', lambda e: e.tensor_scalar(ee[i][:, :], ee[i][:, :], sc[i][:, 2:3], None, ALU.mult), reads=[ee[i], sc[i]], writes=[ee[i]])
        kb.dma('sp', gout[ts_, :], ee[i][:, :], reads=[ee[i]])
    kb.finish()
    return kb


def wslab(w, KI, KO):
    return np.ascontiguousarray(w.reshape(KI, 128, KO, 128).transpose(2, 1, 0, 3).reshape(KO, 128, KI * 128))


GROUPS = ((128, 1), (512, 4), (2048, 16))
PAD = 2048
NEG = -30000.0


def attn_mask_bias():
    cols = []
    i = np.arange(128)[:, None]
    for L, d in GROUPS:
        W = L + 128
        w = np.arange(W)[None, :]
        delta = i + L - w
        ok = (delta >= 0) & (delta <= L) & (delta % d == 0)
        cols.append(np.where(ok, 0.0, NEG).astype(np.float32))
    return np.ascontiguousarray(np.concatenate(cols, 1))


def build_attn(D, NT, HPC, DH=128):
    kb = KB()
    KC = D // 128
    NTI = NT // 128
    NPT = PAD // 128
    WT = sum(L + 128 for L, _ in GROUPS)
    NJ = WT // 128
    x2b = kb.dram("x2b", [D, NT], BF16, "ExternalInput")
    wq = kb.dram("wq", [HPC, 128, KC * 128], F32, "ExternalInput")
    wk = kb.dram("wk", [HPC, 128, KC * 128], F32, "ExternalInput")
    wv = kb.dram("wv", [HPC, 128, KC * 128], F32, "ExternalInput")
    mbd = kb.dram("mb", [128, WT], F32, "ExternalInput")
    idd = kb.dram("ident", [128, 128], BF16, "ExternalInput")
    o = kb.dram("o", [NT, HPC * DH], F32, "ExternalOutput")
    kb.dma_queue('sp', 8)
    xs = [[kb.sb([128, 512], BF16) for _ in range(KC)] for _ in range(2)]
    stg = [kb.sb([128, KC * 128], F32) for _ in range(2)]
    wqb, wkb, wvb = (kb.sb([128, KC * 128], BF16) for _ in range(3))
    qT = kb.sb([128, NT], BF16)
    kT = kb.sb([128, PAD + NT], BF16)
    vtmp = [kb.sb([128, 512], BF16) for _ in range(2)]
    vtok = kb.sb([128, (NPT + NTI) * 128], BF16)
    mb = kb.sb([128, WT], F32)
    ident = kb.sb([128, 128], BF16)
    Sm = [kb.sb([128, WT], F32) for _ in range(2)]
    P = [kb.sb([128, WT], BF16) for _ in range(2)]
    PT = [kb.sb([128, WT], BF16) for _ in range(2)]
    sc = [kb.sb([128, 4], F32) for _ in range(2)]
    osb = [kb.sb([128, DH], F32) for _ in range(2)]
    pp_ = [kb.ps() for _ in range(2)]
    pt_ = [kb.ps([128, 512], BF16) for _ in range(2)]
    po = [kb.ps([128, DH]) for _ in range(2)]
    kb.dma('sp', mb[:, :], mbd[:, :], writes=[mb])
    kb.dma('sp', ident[:, :], idd[:, :], writes=[ident])
    scale = DH ** -0.5
    c = {'p': 0, 't': 0, 'x': 0}

    def nps():
        p = pp_[c['p'] % 2]
        c['p'] += 1
        return p

    def npt():
        p = pt_[c['t'] % 2]
        c['t'] += 1
        return p

    for h in range(HPC):
        for i, (wd_, wb_) in enumerate(((wq, wqb), (wk, wkb), (wv, wvb))):
            s = stg[i % 2]
            kb.dma('sp', s[:, :], wd_[h, :, :], writes=[s])
            kb.op('act', lambda e: e.copy(wb_[:, :], s[:, :]), reads=[s], writes=[wb_])
        kb.op('pool', lambda e: e.memset(kT[:, 0:PAD], 0.0), writes=[kT])
        kb.op('pool', lambda e: e.memset(vtok[:, 0:NPT * 128], 0.0), writes=[vtok])
        for tb in range(NT // 512):
            xb = xs[c['x'] % 2]
            c['x'] += 1
            cs = slice(tb * 512, (tb + 1) * 512)
            for k in range(KC):
                kb.dma('sp', xb[k][:, :], x2b[k * 128:(k + 1) * 128, cs], writes=[xb[k]])
            p = nps()
            for k in range(KC):
                kb.mm(p, p[:, :], wqb, wqb[:, k * 128:(k + 1) * 128], xb[k], xb[k][:, :], k == 0, k == KC - 1)
            kb.op('act', lambda e: e.mul(qT[:, cs], p[:, :], scale), reads=[p], writes=[qT])
            p = nps()
            for k in range(KC):
                kb.mm(p, p[:, :], wkb, wkb[:, k * 128:(k + 1) * 128], xb[k], xb[k][:, :], k == 0, k == KC - 1)
            kb.op('dve', lambda e: e.tensor_copy(kT[:, PAD + tb * 512:PAD + (tb + 1) * 512], p[:, :]), reads=[p], writes=[kT])
            p = nps()
            for k in range(KC):
                kb.mm(p, p[:, :], wvb, wvb[:, k * 128:(k + 1) * 128], xb[k], xb[k][:, :], k == 0, k == KC - 1)
            vt = vtmp[tb % 2]
            kb.op('act', lambda e: e.copy(vt[:, :], p[:, :]), reads=[p], writes=[vt])
            q = npt()
            for j in range(4):
                kb.op('pe', lambda e: e.transpose(q[:, j * 128:(j + 1) * 128], vt[:, j * 128:(j + 1) * 128], ident[:, :]),
                      reads=[vt, ident], writes=[q])
            a0 = (NPT + tb * 4) * 128
            kb.op('dve', lambda e: e.tensor_copy(vtok[:, a0:a0 + 512], q[:, :]), reads=[q], writes=[vtok])
        for ti in range(NTI):
            s0 = ti * 128
            u = ti % 2
            col = 0
            vt_idx = []
            for L, d in GROUPS:
                W = L + 128
                kstart = PAD + s0 - L
                for c0 in range(0, W, 512):
                    n = min(512, W - c0)
                    p = nps()
                    kb.mm(p, p[:, 0:n], qT, qT[:, s0:s0 + 128], kT, kT[:, kstart + c0:kstart + c0 + n], True, True)
                    kb.op('dve', lambda e: e.tensor_tensor(Sm[u][:, col + c0:col + c0 + n], p[:, 0:n], mb[:, col + c0:col + c0 + n], ALU.add),
                          reads=[p, mb], writes=[Sm[u]])
                if s0 < L:
                    kb.op('pool', lambda e: e.memset(Sm[u][:, col:col + (L - s0)], NEG), writes=[Sm[u]])
                vt_idx += list(range(NPT + ti - L // 128, NPT + ti + 1))
                col += W
            kb.op('dve', lambda e: e.reduce_max(sc[u][:, 0:1], Sm[u][:, :], AX.X), reads=[Sm[u]], writes=[sc[u]])
            kb.op('dve', lambda e: e.tensor_scalar(sc[u][:, 1:2], sc[u][:, 0:1], -1.0, None, ALU.mult), reads=[sc[u]], writes=[sc[u]])
            kb.op('act', lambda e: e.activation(P[u][:, :], Sm[u][:, :], AF.Exp, bias=sc[u][:, 1:2], accum_out=sc[u][:, 2:3]),
                  reads=[Sm[u], sc[u]], writes=[P[u], sc[u]])
            for j4 in range(NJ // 4):
                q = npt()
                for j in range(4):
                    jj = j4 * 4 + j
                    kb.op('pe', lambda e: e.transpose(q[:, j * 128:(j + 1) * 128], P[u][:, jj * 128:(jj + 1) * 128], ident[:, :]),
                          reads=[P[u], ident], writes=[q])
                eng = 'act' if j4 % 2 == 0 else 'dve'
                if eng == 'act':
                    kb.op('act', lambda e: e.copy(PT[u][:, j4 * 512:(j4 + 1) * 512], q[:, :]), reads=[q], writes=[PT[u]])
                else:
                    kb.op('dve', lambda e: e.tensor_copy(PT[u][:, j4 * 512:(j4 + 1) * 512], q[:, :]), reads=[q], writes=[PT[u]])
            pq = po[u]
            for jj in range(NJ):
                a = vt_idx[jj]
                kb.mm(pq, pq[:, :], PT[u], PT[u][:, jj * 128:(jj + 1) * 128], vtok, vtok[:, a * 128:(a + 1) * 128], jj == 0, jj == NJ - 1)
            kb.op('dve', lambda e: e.reciprocal(sc[u][:, 3:4], sc[u][:, 2:3]), reads=[sc[u]], writes=[sc[u]])
            kb.op('dve', lambda e: e.tensor_scalar(osb[u][:, :], pq[:, :], sc[u][:, 3:4], None, ALU.mult), reads=[pq, sc[u]], writes=[osb[u]])
            kb.dma('sp', o[s0:s0 + 128, h * DH:(h + 1) * DH], osb[u][:, :], reads=[osb[u]])
    kb.finish()
    return kb


TWO_PI = 6.283185307179586
MAGIC = 12582912.0
GLA_EPS = 1e-6


def s5_host(lam_re, lam_im, log_step, b_re, b_im, c_re, c_im, d_skip, c):
    lr = np.zeros(512, np.float32)
    li = np.zeros(512, np.float32)
    ls = np.zeros(512, np.float32)
    BreT = np.zeros((128, 512), np.float32)
    BimT = np.zeros((128, 512), np.float32)
    Cre = np.zeros((128, 512), np.float32)
    Cim = np.zeros((128, 512), np.float32)
    for q in range(4):
        for half in range(2):
            gl = 2 * q + half
            g = 8 * c + gl
            ms = slice(q * 128 + half * 64, q * 128 + half * 64 + 64)
            lr[ms] = lam_re[g]
            li[ms] = lam_im[g]
            ls[ms] = log_step[g]
            BreT[16 * gl:16 * gl + 16, ms] = b_re[g].T
            BimT[16 * gl:16 * gl + 16, ms] = b_im[g].T
            Cre[half * 64:half * 64 + 64, q * 128 + 16 * gl:q * 128 + 16 * gl + 16] = c_re[g].T
            Cim[half * 64:half * 64 + 64, q * 128 + 16 * gl:q * 128 + 16 * gl + 16] = c_im[g].T
    rep = lambda v: np.ascontiguousarray(np.tile(v[None, :], (128, 1)))
    return {"s5_lr": rep(lr), "s5_li": rep(li), "s5_ls": rep(ls), "s5_bre": BreT, "s5_bim": BimT,
            "s5_cre": Cre, "s5_cim": Cim,
            "s5_d": np.ascontiguousarray(d_skip[128 * c:128 * (c + 1)].reshape(128, 1))}


def build_mix(D, NT, do_s5=True, do_gla=True):
    kb = KB()
    KC = D // 128
    TB = 512
    NBLK = NT // TB
    DK, DV = 128, 256
    xT = kb.dram("xT", [D, NT], F32, "ExternalInput")
    w5 = kb.dram("w5", [5, 128, KC * 128], F32, "ExternalInput")
    wvd = kb.dram("wv", [128, KC * DV], F32, "ExternalInput")
    wgd = kb.dram("wg", [128, KC * 16], F32, "ExternalInput")
    pnames = ["s5_lr", "s5_li", "s5_ls", "s5_bre", "s5_bim", "s5_cre", "s5_cim"]
    pd = {n: kb.dram(n, [128, 512], F32, "ExternalInput") for n in pnames}
    s5d = kb.dram("s5_d", [128, 1], F32, "ExternalInput")
    wg2d = kb.dram("wg2", [16, 128], F32, "ExternalInput")
    bg2d = kb.dram("bg2", [128, 1], F32, "ExternalInput")
    ngd = kb.dram("ng", [128, 2], F32, "ExternalInput")
    cmaskd = kb.dram("cmask", [128, 128], F32, "ExternalInput")
    rmaskd = kb.dram("rmask", [128, 512], F32, "ExternalInput")
    onesd = kb.dram("ones", [128, 512], F32, "ExternalInput")
    identd = kb.dram("ident", [128, 128], F32, "ExternalInput")
    zT = kb.dram("zT", [128, NT], F32, "ExternalOutput")
    ybT = kb.dram("ybT", [DV, NT], F32, "ExternalOutput")
    kb.dma_queue('sp', 8)

    def ld(shape, src, dt=F32):
        t = kb.sb(shape, dt)
        kb.dma('sp', t[:, :], src[:, :], writes=[t])
        return t
    ones = ld([128, 512], onesd)
    ident = ld([128, 128], identd)
    identb = kb.sb([128, 128], BF16)
    kb.op('act', lambda e: e.copy(identb[:, :], ident[:, :]), reads=[ident], writes=[identb])
    onesb = kb.sb([128, 128], BF16)
    kb.op('act', lambda e: e.copy(onesb[:, :], ones[:, 0:128]), reads=[ones], writes=[onesb])

    wsl = [kb.sb([128, KC * 128], BF16) for _ in range(5)]
    wvb = kb.sb([128, KC * DV], BF16)
    wgb = kb.sb([128, KC * 16], BF16)
    if do_s5:
        bbr, bbi = kb.sb([128, 512], BF16), kb.sb([128, 512], BF16)
        creb, cimb = kb.sb([128, 512], BF16), kb.sb([128, 512], BF16)
        rfull = [kb.sb([128, 512], F32) for _ in range(4)]
        Ec = [kb.sb([128, 512], F32) for _ in range(4)]
        Es = [kb.sb([128, 512], F32) for _ in range(4)]
        sin_ = [kb.sb([128, 2], F32) for _ in range(4)]
        s5dv = ld([128, 1], s5d)
    if do_gla:
        wg2 = ld([16, 128], wg2d)
        bg2 = ld([128, 1], bg2d)
        nbg2 = kb.sb([128, 1], F32)
        ng = ld([128, 2], ngd)
        cmask = ld([128, 128], cmaskd)
        rmask = ld([128, 512], rmaskd)
    kb.push_scope()
    stg = [kb.sb([128, KC * DV], F32) for _ in range(2)]
    ns = [0]

    def ldw(src_ap, dst, n):
        s = stg[ns[0] % 2]
        ns[0] += 1
        kb.dma('sp', s[:, 0:n], src_ap, writes=[s])
        kb.op('act', lambda e: e.copy(dst[:, 0:n], s[:, 0:n]), reads=[s], writes=[dst])
    for i in range(5):
        ldw(w5[i, :, :], wsl[i], KC * 128)
    ldw(wvd[:, :], wvb, KC * DV)
    ldw(wgd[:, :], wgb, KC * 16)

    pmm = [kb.ps() for _ in range(2)]
    pc = [0]

    def nps():
        p = pmm[pc[0] % 2]
        pc[0] += 1
        return p

    V = lambda eng, f, r, w: kb.op(eng, f, reads=r, writes=w)

    if do_s5:
        P = {n: ld([128, 512], pd[n]) for n in pnames}
        W = lambda: kb.sb([128, 512], F32)
        lr, li, dt = P["s5_lr"], P["s5_li"], P["s5_ls"]
        V('dve', lambda e: e.tensor_scalar(lr[:, :], lr[:, :], -1e-4, None, ALU.min), [lr], [lr])
        V('act', lambda e: e.activation(dt[:, :], dt[:, :], AF.Exp), [dt], [dt])
        a_, th = W(), W()
        V('dve', lambda e: e.tensor_tensor(a_[:, :], lr[:, :], dt[:, :], ALU.mult), [lr, dt], [a_])
        V('dve', lambda e: e.tensor_tensor(th[:, :], li[:, :], dt[:, :], ALU.mult), [li, dt], [th])
        mag = W()
        V('act', lambda e: e.activation(mag[:, :], a_[:, :], AF.Exp), [a_], [mag])

        def sincos(src, shift, dst):
            t = W()
            V('dve', lambda e: e.tensor_scalar(t[:, :], src[:, :], shift, 1.0 / TWO_PI, ALU.add, ALU.mult), [src], [t])
            k_ = W()
            V('dve', lambda e: e.tensor_scalar(k_[:, :], t[:, :], MAGIC, None, ALU.add), [t], [k_])
            V('dve', lambda e: e.tensor_scalar(k_[:, :], k_[:, :], MAGIC, None, ALU.subtract), [k_], [k_])
            V('dve', lambda e: e.tensor_tensor(t[:, :], t[:, :], k_[:, :], ALU.subtract), [t, k_], [t])
            V('dve', lambda e: e.tensor_scalar(t[:, :], t[:, :], 0.5, -0.5, ALU.min, ALU.max), [t], [t])
            V('act', lambda e: e.activation(dst[:, :], t[:, :], AF.Sin, scale=TWO_PI), [t], [dst])
        sn, cs_ = W(), W()
        sincos(th, 0.0, sn)
        sincos(th, TWO_PI / 4, cs_)
        abr, abi = W(), W()
        V('dve', lambda e: e.tensor_tensor(abr[:, :], mag[:, :], cs_[:, :], ALU.mult), [mag, cs_], [abr])
        V('dve', lambda e: e.tensor_tensor(abi[:, :], mag[:, :], sn[:, :], ALU.mult), [mag, sn], [abi])
        den, t1, t2 = W(), W(), W()
        V('dve', lambda e: e.tensor_tensor(den[:, :], lr[:, :], lr[:, :], ALU.mult), [lr], [den])
        V('dve', lambda e: e.tensor_tensor(t1[:, :], li[:, :], li[:, :], ALU.mult), [li], [t1])
        V('dve', lambda e: e.tensor_tensor(den[:, :], den[:, :], t1[:, :], ALU.add), [den, t1], [den])
        V('dve', lambda e: e.reciprocal(den[:, :], den[:, :]), [den], [den])
        nr = W()
        V('dve', lambda e: e.tensor_scalar(nr[:, :], abr[:, :], -1.0, None, ALU.add), [abr], [nr])
        fre, fim = W(), W()
        V('dve', lambda e: e.tensor_tensor(t1[:, :], nr[:, :], lr[:, :], ALU.mult), [nr, lr], [t1])
        V('dve', lambda e: e.tensor_tensor(t2[:, :], abi[:, :], li[:, :], ALU.mult), [abi, li], [t2])
        V('dve', lambda e: e.tensor_tensor(t1[:, :], t1[:, :], t2[:, :], ALU.add), [t1, t2], [t1])
        V('dve', lambda e: e.tensor_tensor(fre[:, :], t1[:, :], den[:, :], ALU.mult), [t1, den], [fre])
        V('dve', lambda e: e.tensor_tensor(t1[:, :], abi[:, :], lr[:, :], ALU.mult), [abi, lr], [t1])
        V('dve', lambda e: e.tensor_tensor(t2[:, :], nr[:, :], li[:, :], ALU.mult), [nr, li], [t2])
        V('dve', lambda e: e.tensor_tensor(t1[:, :], t1[:, :], t2[:, :], ALU.subtract), [t1, t2], [t1])
        V('dve', lambda e: e.tensor_tensor(fim[:, :], t1[:, :], den[:, :], ALU.mult), [t1, den], [fim])
        bre, bim = P["s5_bre"], P["s5_bim"]
        V('dve', lambda e: e.tensor_tensor(t1[:, :], fre[:, :], bre[:, :], ALU.mult), [fre, bre], [t1])
        V('dve', lambda e: e.tensor_tensor(t2[:, :], fim[:, :], bim[:, :], ALU.mult), [fim, bim], [t2])
        V('dve', lambda e: e.tensor_tensor(bbr[:, :], t1[:, :], t2[:, :], ALU.subtract), [t1, t2], [bbr])
        V('dve', lambda e: e.tensor_tensor(t1[:, :], fre[:, :], bim[:, :], ALU.mult), [fre, bim], [t1])
        V('dve', lambda e: e.tensor_tensor(t2[:, :], fim[:, :], bre[:, :], ALU.mult), [fim, bre], [t2])
        V('dve', lambda e: e.tensor_tensor(bbi[:, :], t1[:, :], t2[:, :], ALU.add), [t1, t2], [bbi])
        V('act', lambda e: e.copy(creb[:, :], P["s5_cre"][:, :]), [P["s5_cre"]], [creb])
        V('act', lambda e: e.mul(cimb[:, :], P["s5_cim"][:, :], -1.0), [P["s5_cim"]], [cimb])
        for q in range(4):
            qs = slice(q * 128, (q + 1) * 128)
            cols = {}
            for nm, src in (("mag", mag), ("cs", cs_), ("sn", sn)):
                p = nps()
                V('pe', lambda e: e.transpose(p[:, 0:128], src[:, qs], ident[:, :]), [src, ident], [p])
                ct = kb.sb([128, 128], F32)
                V('act', lambda e: e.copy(ct[:, :], p[:, 0:128]), [p], [ct])
                cols[nm] = ct
            rf = rfull[q]
            V('dve', lambda e: e.tensor_scalar(rf[:, :], ones[:, :], cols["mag"][:, 0:1], None, ALU.mult), [ones, cols["mag"]], [rf])
            ec, es = Ec[q], Es[q]
            V('act', lambda e: e.copy(ec[:, 0:1], cols["cs"][:, 0:1]), [cols["cs"]], [ec])
            V('act', lambda e: e.copy(es[:, 0:1], cols["sn"][:, 0:1]), [cols["sn"]], [es])
            pw = kb.sb([128, 8], F32)
            V('act', lambda e: e.copy(pw[:, 0:1], cols["cs"][:, 0:1]), [cols["cs"]], [pw])
            V('act', lambda e: e.copy(pw[:, 1:2], cols["sn"][:, 0:1]), [cols["sn"]], [pw])
            n = 1
            tmpw = W()
            while n < TB:
                V('dve', lambda e: e.tensor_scalar(pw[:, 2:3], pw[:, 1:2], -1.0, None, ALU.mult), [pw], [pw])
                V('dve', lambda e: e.tensor_scalar(tmpw[:, 0:n], ec[:, 0:n], pw[:, 0:1], None, ALU.mult), [ec, pw], [tmpw])
                V('dve', lambda e: e.scalar_tensor_tensor(ec[:, n:2 * n], es[:, 0:n], pw[:, 2:3], tmpw[:, 0:n], ALU.mult, ALU.add),
                  [es, pw, tmpw], [ec])
                V('dve', lambda e: e.tensor_scalar(tmpw[:, 0:n], es[:, 0:n], pw[:, 0:1], None, ALU.mult), [es, pw], [tmpw])
                V('dve', lambda e: e.scalar_tensor_tensor(es[:, n:2 * n], ec[:, 0:n], pw[:, 1:2], tmpw[:, 0:n], ALU.mult, ALU.add),
                  [ec, pw, tmpw], [es])
                V('dve', lambda e: e.tensor_tensor(pw[:, 3:4], pw[:, 0:1], pw[:, 0:1], ALU.mult), [pw], [pw])
                V('dve', lambda e: e.tensor_tensor(pw[:, 4:5], pw[:, 1:2], pw[:, 1:2], ALU.mult), [pw], [pw])
                V('dve', lambda e: e.tensor_tensor(pw[:, 5:6], pw[:, 0:1], pw[:, 1:2], ALU.mult), [pw], [pw])
                V('dve', lambda e: e.tensor_tensor(pw[:, 0:1], pw[:, 3:4], pw[:, 4:5], ALU.subtract), [pw], [pw])
                V('dve', lambda e: e.tensor_scalar(pw[:, 1:2], pw[:, 5:6], 2.0, None, ALU.mult), [pw], [pw])
                n *= 2
        for q in range(4):
            V('pool', lambda e: e.memset(sin_[q][:, :], 0.0), [], [sin_[q]])
    kb.pop_scope()
    if do_s5:
        s5w = {n_: [kb.sb([128, 512], F32) for _ in range(2)] for n_ in ("t1", "t2", "xr", "xi", "zr", "zi", "sr", "si")}
        s5b = {n_: [kb.sb([128, 512], BF16) for _ in range(2)] for n_ in ("srb", "sib")}
        yw = [kb.sb([128, 512], F32) for _ in range(4)]
        py = kb.ps()

    if do_gla:
        V('dve', lambda e: e.tensor_scalar(nbg2[:, :], bg2[:, :], -1.0, None, ALU.mult), [bg2], [nbg2])
        S = kb.sb([128, DV], F32)
        Sb = kb.sb([128, DV], BF16)
        V('pool', lambda e: e.memset(S[:, :], 0.0), [], [S])
        V('pool', lambda e: e.memset(Sb[:, :], 0.0), [], [Sb])
        glT = kb.sb([16, 512], F32)
        qt = kb.sb([128, 512], BF16)
        kt = kb.sb([128, 512], BF16)
        q32 = kb.sb([128, 512], F32)
        k32 = kb.sb([128, 512], F32)
        Bc = kb.sb([128, 512], F32)
        eb = kb.sb([128, 512], F32)
        enb = kb.sb([128, 512], F32)
        l1 = kb.sb([128, 512], F32)
        sr_ = [kb.sb([128, 512], F32) for _ in range(2)]
        vtk = [kb.sb([128, DV], BF16) for _ in range(4)]
        kdT = kb.sb([128, 128], BF16)
        kd = kb.sb([128, 128], BF16)
        AT = kb.sb([128, 128], BF16)
        sq = [kb.sb([128, 128], BF16) for _ in range(2)]
        rs = kb.sb([128, 128], F32)
        on = [kb.sb([128, 128], F32) for _ in range(2)]
        ybo = [kb.sb([128, 512], F32) for _ in range(2)]
        pA = kb.ps([128, 128])
        pO = [kb.ps([128, 128]) for _ in range(2)]
        pT = kb.ps([128, 128], BF16)
        pKV = kb.ps([128, DV])

    xst = [kb.sb([128, TB], F32) for _ in range(3)]
    xb = [[kb.sb([128, TB], BF16) for _ in range(KC)] for _ in range(2)]
    ub = [kb.sb([128, TB], BF16) for _ in range(2)]
    nx = [0]

    for blk in range(NBLK):
        cs = slice(blk * TB, (blk + 1) * TB)
        xs = xb[blk % 2]
        for k in range(KC):
            s = xst[nx[0] % 3]
            nx[0] += 1
            kb.dma('sp', s[:, :], xT[k * 128:(k + 1) * 128, cs], writes=[s])
            eng = 'act' if k % 2 == 0 else 'pool'
            if eng == 'act':
                V('act', lambda e: e.copy(xs[k][:, :], s[:, :]), [s], [xs[k]])
            else:
                V('pool', lambda e: e.tensor_copy(xs[k][:, :], s[:, :]), [s], [xs[k]])

        def proj(w, ncols, wcols=128):
            p = nps()
            for k in range(KC):
                kb.mm(p, p[0:ncols, :], w, w[:, k * wcols:k * wcols + ncols], xs[k], xs[k][:, :], k == 0, k == KC - 1)
            return p

        if do_s5:
            u = ub[blk % 2]
            p = proj(wsl[0], 128)
            V('act', lambda e: e.copy(u[:, :], p[:, :]), [p], [u])
            for q in range(4):
                qs = slice(q * 128, (q + 1) * 128)
                i = q % 2
                w_ = {n_: s5w[n_][i] for n_ in s5w}
                pxr, pxi = nps(), nps()
                kb.mm(pxr, pxr[:, :], bbr, bbr[:, qs], u, u[:, :], True, True)
                kb.mm(pxi, pxi[:, :], bbi, bbi[:, qs], u, u[:, :], True, True)
                ec, es = Ec[q], Es[q]
                V('dve', lambda e: e.tensor_tensor(w_["t1"][:, :], pxr[:, :], ec[:, :], ALU.mult), [pxr, ec], [w_["t1"]])
                V('dve', lambda e: e.tensor_tensor(w_["t2"][:, :], pxi[:, :], es[:, :], ALU.mult), [pxi, es], [w_["t2"]])
                V('pool', lambda e: e.tensor_tensor(w_["xr"][:, :], w_["t1"][:, :], w_["t2"][:, :], ALU.add), [w_["t1"], w_["t2"]], [w_["xr"]])
                V('dve', lambda e: e.tensor_tensor(w_["t1"][:, :], pxi[:, :], ec[:, :], ALU.mult), [pxi, ec], [w_["t1"]])
                V('dve', lambda e: e.tensor_tensor(w_["t2"][:, :], pxr[:, :], es[:, :], ALU.mult), [pxr, es], [w_["t2"]])
                V('pool', lambda e: e.tensor_tensor(w_["xi"][:, :], w_["t1"][:, :], w_["t2"][:, :], ALU.subtract), [w_["t1"], w_["t2"]], [w_["xi"]])
                V('dve', lambda e: e.tensor_tensor_scan(w_["zr"][:, :], rfull[q][:, :], w_["xr"][:, :], sin_[q][:, 0:1], ALU.mult, ALU.add),
                  [rfull[q], w_["xr"], sin_[q]], [w_["zr"]])
                V('dve', lambda e: e.tensor_tensor_scan(w_["zi"][:, :], rfull[q][:, :], w_["xi"][:, :], sin_[q][:, 1:2], ALU.mult, ALU.add),
                  [rfull[q], w_["xi"], sin_[q]], [w_["zi"]])
                V('dve', lambda e: e.tensor_tensor(w_["t1"][:, :], w_["zr"][:, :], ec[:, :], ALU.mult), [w_["zr"], ec], [w_["t1"]])
                V('pool', lambda e: e.tensor_tensor(w_["t2"][:, :], w_["zi"][:, :], es[:, :], ALU.mult), [w_["zi"], es], [w_["t2"]])
                V('pool', lambda e: e.tensor_tensor(w_["sr"][:, :], w_["t1"][:, :], w_["t2"][:, :], ALU.subtract), [w_["t1"], w_["t2"]], [w_["sr"]])
                V('dve', lambda e: e.tensor_tensor(w_["t1"][:, :], w_["zr"][:, :], es[:, :], ALU.mult), [w_["zr"], es], [w_["t1"]])
                V('pool', lambda e: e.tensor_tensor(w_["t2"][:, :], w_["zi"][:, :], ec[:, :], ALU.mult), [w_["zi"], ec], [w_["t2"]])
                V('pool', lambda e: e.tensor_tensor(w_["si"][:, :], w_["t1"][:, :], w_["t2"][:, :], ALU.add), [w_["t1"], w_["t2"]], [w_["si"]])
                srb, sib = s5b["srb"][i], s5b["sib"][i]
                V('act', lambda e: e.copy(srb[:, :], w_["sr"][:, :]), [w_["sr"]], [srb])
                V('act', lambda e: e.copy(sib[:, :], w_["si"][:, :]), [w_["si"]], [sib])
                V('act', lambda e: e.copy(sin_[q][:, 0:1], w_["sr"][:, TB - 1:TB]), [w_["sr"]], [sin_[q]])
                V('act', lambda e: e.copy(sin_[q][:, 1:2], w_["si"][:, TB - 1:TB]), [w_["si"]], [sin_[q]])
                kb.mm(py, py[:, :], creb, creb[:, qs], srb, srb[:, :], q == 0, False)
                kb.mm(py, py[:, :], cimb, cimb[:, qs], sib, sib[:, :], False, q == 3)
            y, y2, y3, zz = yw
            V('dve', lambda e: e.scalar_tensor_tensor(y[:, :], u[:, :], s5dv[:, 0:1], py[:, :], ALU.mult, ALU.add), [u, s5dv, py], [y])
            V('pool', lambda e: e.tensor_tensor(y2[:, :], y[:, :], y[:, :], ALU.mult), [y], [y2])
            V('pool', lambda e: e.tensor_scalar(y2[:, :], y2[:, :], 0.044715, 1.0, ALU.mult, ALU.add), [y2], [y2])
            V('pool', lambda e: e.tensor_tensor(y3[:, :], y2[:, :], y[:, :], ALU.mult), [y2, y], [y3])
            V('act', lambda e: e.activation(y3[:, :], y3[:, :], AF.Sigmoid, scale=1.5957691216057308), [y3], [y3])
            V('dve', lambda e: e.tensor_tensor(zz[:, :], y[:, :], y3[:, :], ALU.mult), [y, y3], [zz])
            kb.dma('sp', zT[:, cs], zz[:, :], reads=[zz])

        if do_gla:
            p = proj(wsl[1], 128)
            V('act', lambda e: e.mul(q32[:, :], p[:, :], DK ** -0.5), [p], [q32])
            p = proj(wsl[2], 128)
            V('act', lambda e: e.copy(k32[:, :], p[:, :]), [p], [k32])
            for a in range(2):
                p = proj(wsl[3 + a], 128)
                V('act', lambda e: e.activation(sr_[a][:, :], p[:, :], AF.Sigmoid), [p], [sr_[a]])
                V('dve', lambda e: e.tensor_tensor(sr_[a][:, :], sr_[a][:, :], p[:, :], ALU.mult), [sr_[a], p], [sr_[a]])
            p = proj(wgb, 16, 16)
            V('act', lambda e: e.copy(glT[:, :], p[0:16, :]), [p], [glT])
            for t in range(4):
                pv = nps()
                for k in range(KC):
                    kb.mm(pv, pv[:, 0:DV], xs[k], xs[k][:, t * 128:(t + 1) * 128], wvb, wvb[:, k * DV:(k + 1) * DV], k == 0, k == KC - 1)
                V('act', lambda e: e.copy(vtk[t][:, :], pv[:, 0:DV]), [pv], [vtk[t]])
            p = nps()
            kb.mm(p, p[:, :], wg2, wg2[:, :], glT, glT[:, :], True, True)
            V('act', lambda e: e.activation(l1[:, :], p[:, :], AF.Exp, bias=nbg2[:, 0:1], scale=-1.0), [p, nbg2], [l1])
            V('dve', lambda e: e.tensor_scalar(l1[:, :], l1[:, :], 1.0, None, ALU.add), [l1], [l1])
            V('act', lambda e: e.activation(l1[:, :], l1[:, :], AF.Ln), [l1], [l1])
            V('dve', lambda e: e.tensor_tensor_scan(Bc[:, :], rmask[:, :], l1[:, :], 0.0, ALU.mult, ALU.add), [rmask, l1], [Bc])
            V('act', lambda e: e.activation(eb[:, :], Bc[:, :], AF.Exp, scale=-1.0 / 16), [Bc], [eb])
            V('act', lambda e: e.activation(enb[:, :], Bc[:, :], AF.Exp, scale=1.0 / 16), [Bc], [enb])
            V('dve', lambda e: e.tensor_tensor(qt[:, :], q32[:, :], eb[:, :], ALU.mult), [q32, eb], [qt])
            V('dve', lambda e: e.tensor_tensor(k32[:, :], k32[:, :], enb[:, :], ALU.mult), [k32, enb], [k32])
            V('act', lambda e: e.copy(kt[:, :], k32[:, :]), [k32], [kt])
            yo = ybo
            for c_ in range(4):
                ch = slice(c_ * 128, (c_ + 1) * 128)
                el = eb[:, c_ * 128 + 127:c_ * 128 + 128]
                V('dve', lambda e: e.tensor_scalar(kdT[:, :], k32[:, ch], el, None, ALU.mult), [k32, eb], [kdT])
                V('pe', lambda e: e.transpose(pT[:, :], kdT[:, :], identb[:, :]), [kdT, identb], [pT])
                V('act', lambda e: e.copy(kd[:, :], pT[:, :]), [pT], [kd])
                kb.mm(pA, pA[:, :], kt, kt[:, ch], qt, qt[:, ch], True, True)
                V('dve', lambda e: e.tensor_tensor(AT[:, :], pA[:, :], cmask[:, :], ALU.mult), [pA, cmask], [AT])
                for a in range(2):
                    kb.mm(pO[a], pO[a][:, :], vtk[c_], vtk[c_][:, a * 128:(a + 1) * 128], AT, AT[:, :], True, False)
                    kb.mm(pO[a], pO[a][:, :], Sb, Sb[:, a * 128:(a + 1) * 128], qt, qt[:, ch], False, True)
                kb.mm(pKV, pKV[:, :], kd, kd[:, :], vtk[c_], vtk[c_][:, :], True, True)
                V('dve', lambda e: e.scalar_tensor_tensor(S[:, :], S[:, :], el, pKV[:, :], ALU.mult, ALU.add), [S, eb, pKV], [S])
                V('act', lambda e: e.copy(Sb[:, :], S[:, :]), [S], [Sb])
                for a in range(2):
                    V('act', lambda e: e.activation(sq[a][:, :], pO[a][:, :], AF.Square), [pO[a]], [sq[a]])
                pn = nps()
                for a in range(2):
                    kb.mm(pn, pn[:, 0:128], onesb, onesb[:, :], sq[a], sq[a][:, :], a == 0, a == 1)
                V('dve', lambda e: e.tensor_scalar(rs[:, :], pn[:, 0:128], 1.0 / DV, GLA_EPS, ALU.mult, ALU.add), [pn], [rs])
                V('act', lambda e: e.activation(rs[:, :], rs[:, :], AF.Sqrt), [rs], [rs])
                V('dve', lambda e: e.reciprocal(rs[:, :], rs[:, :]), [rs], [rs])
                for a in range(2):
                    V('dve', lambda e: e.scalar_tensor_tensor(on[a][:, :], pO[a][:, :], ng[:, a:a + 1], rs[:, :], ALU.mult, ALU.mult),
                      [pO[a], ng, rs], [on[a]])
                    V('pool', lambda e: e.tensor_tensor(yo[a][:, ch], on[a][:, :], sr_[a][:, ch], ALU.mult), [on[a], sr_[a]], [yo[a]])
            for a in range(2):
                kb.dma('sp', ybT[a * 128:(a + 1) * 128, cs], yo[a][:, :], reads=[yo[a]])
    kb.finish()
    return kb

import ml_dtypes
from concourse.bass_utils import run_bass_kernel_spmd

NCORES = 8
_PROGS = {}


def _prog(key, fn):
    if key not in _PROGS:
        _PROGS[key] = fn()
    return _PROGS[key]


def _run(kb, ins):
    res = run_bass_kernel_spmd(kb.nc, ins, core_ids=list(range(NCORES)))
    return res.results


def kernel(**inp):
    x = np.asarray(inp["x"])[0]
    NT, D = x.shape
    KC = D // 128
    NTK = NT // NCORES
    F = inp["moe_w_gu"].shape[-1] // 2
    NE = inp["moe_w_gu"].shape[1]
    EL = NE // NCORES
    xT = np.ascontiguousarray(x.T)
    ones512 = np.ones((128, 512), np.float32)
    ones128 = np.ones((128, 128), np.float32)
    ident = np.eye(128, dtype=np.float32)
    ts = [slice(c * NTK, (c + 1) * NTK) for c in range(NCORES)]
    sh = lambda a, c: np.ascontiguousarray(a[:, ts[c]])

    w_in = inp["ab_w_in"][0]
    s1 = 1024
    s2 = s1 + 512
    s3 = s2 + 512
    s4 = s3 + 1024
    s5_ = s4 + 16
    kbA = _prog(("mix", D, NT), lambda: build_mix(D, NT))
    insA = []
    for c in range(NCORES):
        h = c // 2
        sl = lambda a, b: wslab(w_in[:, a:b], KC, 1)[0]
        w5 = np.stack([sl(128 * c, 128 * c + 128), sl(s1 + 128 * h, s1 + 128 * h + 128), sl(s2 + 128 * h, s2 + 128 * h + 128),
                       sl(s5_ + 256 * h, s5_ + 256 * h + 128), sl(s5_ + 256 * h + 128, s5_ + 256 * h + 256)])
        wv = np.ascontiguousarray(w_in[:, s3 + 256 * h:s3 + 256 * h + 256].reshape(KC, 128, 256).transpose(1, 0, 2).reshape(128, KC * 256))
        wg = np.ascontiguousarray(w_in[:, s4:s4 + 16].reshape(KC, 128, 16).transpose(1, 0, 2).reshape(128, KC * 16))
        d = {"xT": xT, "w5": w5, "wv": wv, "wg": wg,
             "wg2": np.ascontiguousarray(inp["gla_w_gate2"][0][:, 128 * h:128 * h + 128]),
             "bg2": np.ascontiguousarray(inp["gla_b_gate2"][0][128 * h:128 * h + 128].reshape(128, 1)),
             "ng": np.ascontiguousarray(inp["gla_norm_g"][0].reshape(2, 128).T),
             "cmask": np.triu(np.ones((128, 128), np.float32)),
             "rmask": np.ascontiguousarray((np.arange(512) % 128 != 0).astype(np.float32)[None, :].repeat(128, 0)),
             "ones": ones512, "ident": ident}
        d.update(s5_host(inp["s5_lam_re"][0], inp["s5_lam_im"][0], inp["s5_log_step"][0], inp["s5_b_re"][0], inp["s5_b_im"][0],
                         inp["s5_c_re"][0], inp["s5_c_im"][0], inp["s5_d"][0], c))
        insA.append(d)
    rA = _run(kbA, insA)
    feat = np.concatenate([rA[c]["zT"] for c in range(NCORES)] + [rA[2 * h]["ybT"] for h in range(4)], 0)
    del rA, insA

    def post(feat, xin_sh, layer, wout, glu):
        kbB = _prog(("post", D, NTK, glu), lambda: build_post(D, NTK, glu, NE))
        wo = wslab(wout, KC, KC)
        wr = np.ascontiguousarray(inp["moe_w_router"][layer].reshape(KC, 128, NE).transpose(1, 0, 2).reshape(128, KC * NE))
        brb = np.ascontiguousarray(np.tile(inp["moe_b_router"][layer][None, :], (128, 1)))
        ins = []
        for c in range(NCORES):
            d = {"feat": sh(feat, c), "wout": wo, "xin": xin_sh[c], "lng": pp(inp["ln1_g"][layer], KC), "lnb": pp(inp["ln1_b"][layer], KC),
                 "ones": ones128, "wr": wr, "brb": brb}
            if glu:
                d["wglu"] = wslab(inp["s5_w_glu"][0], KC // 2, KC // 2)
                d["bglu"] = pp(inp["s5_b_glu"][0], KC // 2)
            ins.append(d)
        r = _run(kbB, ins)
        return [r[c]["xo"] for c in range(NCORES)], np.concatenate([r[c]["xob"] for c in range(NCORES)], 1), \
            np.concatenate([r[c]["gout"] for c in range(NCORES)], 0)

    def moe_ln(x1_sh, x1b, G, layer):
        kbM = _prog(("moe", D, F, NT, EL), lambda: build_moe(D, F, NT, EL, TB=1024))
        ins = [moe_host_inputs(x1b, G, inp["moe_w_gu"][layer], inp["moe_b_gu"][layer], inp["moe_w_down"][layer],
                               inp["moe_b_down"][layer], c, EL) for c in range(NCORES)]
        r = _run(kbM, ins)
        del ins
        parts = [r[c]["y"] for c in range(NCORES)]
        kbN = _prog(("sumln", D, NTK), lambda: build_sumln(D, NTK, NCORES))
        ins = [{"xin": x1_sh[c], "part": np.ascontiguousarray(np.stack([p[:, ts[c]] for p in parts])),
                "lng": pp(inp["ln2_g"][layer], KC), "lnb": pp(inp["ln2_b"][layer], KC), "ones": ones128} for c in range(NCORES)]
        r = _run(kbN, ins)
        return [r[c]["xo"] for c in range(NCORES)], np.concatenate([r[c]["xob"] for c in range(NCORES)], 1)

    x_sh = [sh(xT, c) for c in range(NCORES)]
    x1_sh, x1b, G = post(feat, x_sh, 0, inp["ab_w_out"][0], True)
    x2_sh, x2b = moe_ln(x1_sh, x1b, G, 0)

    NH = D // 128
    HPC = NH // NCORES
    wqkv = inp["c_w_qkv"][0].reshape(D, 3, NH, 128)
    kbC = _prog(("attn", D, NT, HPC), lambda: build_attn(D, NT, HPC))
    mbias = attn_mask_bias()
    identb = ident.astype(ml_dtypes.bfloat16)
    insC = []
    for c in range(NCORES):
        d = {"x2b": x2b, "mb": mbias, "ident": identb}
        for i, nm in enumerate(("wq", "wk", "wv")):
            w = np.ascontiguousarray(wqkv[:, i, c * HPC:(c + 1) * HPC, :].reshape(D, HPC * 128))
            d[nm] = wslab(w, KC, HPC)
        insC.append(d)
    rC = _run(kbC, insC)
    attn = np.concatenate([rC[c]["o"] for c in range(NCORES)], 1)
    feat1 = np.ascontiguousarray(attn.T)
    del rC, insC
    x3_sh, x3b, G1 = post(feat1, x2_sh, 1, inp["c_w_o"][0], False)
    x4_sh, _ = moe_ln(x3_sh, x3b, G1, 1)
    out = np.concatenate(x4_sh, 1)
    return np.ascontiguousarray(out.T)[None].astype(np.float32)
```

```python
import contextlib
import numpy as np
import concourse.bass as bass
import concourse.mybir as mybir

F32 = mybir.dt.float32
BF16 = mybir.dt.bfloat16
ALU = mybir.AluOpType
AF = mybir.ActivationFunctionType
AX = mybir.AxisListType


class T:
    def __init__(self, t):
        self.t = t
        self.w = None
        self.r = {}

    def __getitem__(self, idx):
        return self.t[idx]


class KB:
    EPOCH = 30000

    def __init__(self):
        self.nc = bass.Bass("TRN2", target_bir_lowering=False)
        self.st = contextlib.ExitStack()
        nc = self.nc
        self.E = {'pe': nc.tensor, 'act': nc.scalar, 'dve': nc.vector, 'pool': nc.gpsimd, 'sp': nc.sync}
        self.semobj = {}
        self.cur = {}
        self.cnt = {}
        self.epoch = {}
        for e in ('pe', 'act', 'dve', 'pool'):
            self.epoch[e] = 0
            self._new_epoch(e)
        self.waited = {}
        self.dq = {}
        self.dq_i = {}
        self.dval = {}
        self.nsb = 0
        self.scope = None

    def _new_epoch(self, e):
        key = f"{e}{self.epoch[e]}"
        self.epoch[e] += 1
        self.semobj[key] = self.st.enter_context(self.nc.semaphore("s_" + key))
        self.cur[e] = key
        self.cnt[e] = 0

    def dma_queue(self, q, nsem=8):
        keys = []
        for i in range(nsem):
            key = f"d_{q}_{i}"
            self.semobj[key] = self.st.enter_context(self.nc.semaphore(key))
            self.dval[key] = 0
            keys.append(key)
        self.dq[q] = keys
        self.dq_i[q] = 0

    def sb(self, shape, dt, name=None):
        self.nsb += 1
        st = self.scope if self.scope is not None else self.st
        return T(st.enter_context(self.nc.sbuf_tensor(name or f"sb{self.nsb}", list(shape), dt)))

    def push_scope(self):
        assert self.scope is None
        self.scope = contextlib.ExitStack()

    def barrier(self):
        engs = ('pe', 'act', 'dve', 'pool', 'sp')
        for e in engs:
            for o in ('pe', 'act', 'dve', 'pool'):
                if o != e:
                    self._wait(e, self.cur[o], self.cnt[o])
            for q in self.dq:
                for key in self.dq[q]:
                    self._wait(e, key, self.dval[key])

    def pop_scope(self):
        self.barrier()
        self.scope.close()
        self.scope = None

    def ps(self, shape=(128, 512), dt=F32, name=None):
        self.nsb += 1
        return T(self.st.enter_context(self.nc.psum_tensor(name or f"ps{self.nsb}", list(shape), dt)))

    def dram(self, name, shape, dt, kind="Internal"):
        if kind == "Internal":
            return self.nc.dram_tensor(name, list(shape), dt)
        return self.nc.dram_tensor(name, list(shape), dt, kind=kind)

    def _wait(self, eng, semkey, val):
        if self.waited.get((eng, semkey), 0) >= val:
            return
        self.E[eng].wait_ge(self.semobj[semkey], val)
        self.waited[(eng, semkey)] = val

    def _deps(self, eng, reads, writes):
        deps = {}

        def add(ev):
            if ev is None:
                return
            k, v = ev
            if deps.get(k, 0) < v:
                deps[k] = v
        for t in reads:
            add(t.w)
        for t in writes:
            add(t.w)
            for k, v in t.r.items():
                add((k, v))
        for k, v in deps.items():
            if eng == 'pe' and k.startswith('pe'):
                continue
            self._wait(eng, k, v)

    def _mark(self, ev, reads, writes):
        k, v = ev
        for t in reads:
            if t.r.get(k, 0) < v:
                t.r[k] = v
        for t in writes:
            t.w = ev
            t.r = {}

    def op(self, eng, fn, reads=(), writes=()):
        if self.cnt[eng] >= self.EPOCH:
            self._new_epoch(eng)
        self._deps(eng, reads, writes)
        inst = fn(self.E[eng])
        self.cnt[eng] += 1
        key = self.cur[eng]
        inst.then_inc(self.semobj[key], 1)
        self._mark((key, self.cnt[eng]), reads, writes)
        return inst

    def dma(self, q, out, in_, reads=(), writes=(), **kw):
        self._deps(q, reads, writes)
        keys = self.dq[q]
        key = keys[self.dq_i[q] % len(keys)]
        self.dq_i[q] += 1
        self._wait(q, key, self.dval[key])
        self.E[q].dma_start(out=out, in_=in_, **kw).then_inc(self.semobj[key], 16)
        self.dval[key] += 16
        self._mark((key, self.dval[key]), reads, writes)

    def finish(self):
        for q in self.dq:
            for key in self.dq[q]:
                self._wait(q if q in self.E else 'sp', key, self.dval[key])
        for e in ('pe', 'act', 'dve', 'pool'):
            self._wait('sp', self.cur[e], self.cnt[e])
        self.st.close()

    def mm(self, out_t, out_ap, lhsT_t, lhsT_ap, rhs_t, rhs_ap, start, stop):
        return self.op('pe', lambda e: e.matmul(out_ap, lhsT_ap, rhs_ap, start=start, stop=stop),
                       reads=[lhsT_t, rhs_t], writes=[out_t])


def build_moe(D, F, NT, EL, TB=1024):
    kb = KB()
    nc = kb.nc
    KC = D // 128
    FC = F // 128
    NSB = TB // 512
    NB = NT // TB
    x1b = kb.dram("x1b", [D, NT], BF16, "ExternalInput")
    gt = kb.dram("gt", [EL, NT], F32, "ExternalInput")
    wgu = kb.dram("wgu", [EL, 2 * FC, 128, KC * 128], F32, "ExternalInput")
    bgu = kb.dram("bgu", [128, EL * 2 * FC], F32, "ExternalInput")
    wd = kb.dram("wd", [EL, KC, 128, FC * 128], F32, "ExternalInput")
    bd = kb.dram("bd", [EL, D], F32, "ExternalInput")
    sel = kb.dram("sel", [EL, EL * 128], F32, "ExternalInput")
    y = kb.dram("y", [D, NT], F32, "ExternalOutput")
    kb.dma_queue('sp', 8)

    xb = [kb.sb([128, TB], BF16) for _ in range(KC)]
    acc = [kb.sb([128, TB], F32) for _ in range(KC)]
    act = [kb.sb([128, TB], BF16) for _ in range(FC)]
    gb = kb.sb([128, TB], F32)
    gts = kb.sb([EL, TB], F32)
    bgu_s = kb.sb([128, EL * 2 * FC], F32)
    bd_s = kb.sb([EL, D], F32)
    sel_s = kb.sb([EL, EL * 128], F32)
    NSTG = 3
    stg = [kb.sb([128, max(KC, FC) * 128], F32) for _ in range(NSTG)]
    NWB = 4
    wgb = [kb.sb([128, KC * 128], BF16) for _ in range(NWB)]
    wdb = [kb.sb([128, FC * 128], BF16) for _ in range(2)]
    g32 = [kb.sb([128, 512], F32) for _ in range(2)]
    sig = [kb.sb([128, 512], F32) for _ in range(2)]
    u1 = [kb.sb([128, 512], F32) for _ in range(2)]
    pg = [kb.ps() for _ in range(2)]
    pu = [kb.ps() for _ in range(2)]
    pd = [kb.ps() for _ in range(2)]
    pm = [kb.ps() for _ in range(2)]

    kb.dma('sp', bgu_s[:, :], bgu[:, :], writes=[bgu_s])
    kb.dma('sp', bd_s[:, :], bd[:, :], writes=[bd_s])
    kb.dma('sp', sel_s[:, :], sel[:, :], writes=[sel_s])

    cnt = {'stg': 0, 'wg': 0, 'wd': 0, 'u': 0, 'pd': 0, 'pm': 0}

    def load_slab(src_ap, ncols, dst):
        s = stg[cnt['stg'] % NSTG]
        cnt['stg'] += 1
        kb.dma('sp', s[:, 0:ncols], src_ap, writes=[s])
        kb.op('act', lambda e: e.copy(dst[:, 0:ncols], s[:, 0:ncols]), reads=[s], writes=[dst])

    for b in range(NB):
        t0 = b * TB
        for k in range(KC):
            kb.dma('sp', xb[k][:, :], x1b[k * 128:(k + 1) * 128, t0:t0 + TB], writes=[xb[k]])
        kb.dma('sp', gts[:, :], gt[:, t0:t0 + TB], writes=[gts])
        for e in range(EL):
            for sb_ in range(NSB):
                p = pm[cnt['pm'] % 2]
                cnt['pm'] += 1
                kb.mm(p, p[:, :], sel_s, sel_s[:, e * 128:(e + 1) * 128], gts, gts[:, sb_ * 512:(sb_ + 1) * 512], True, True)
                kb.op('act', lambda en, p=p, sb_=sb_: en.copy(gb[:, sb_ * 512:(sb_ + 1) * 512], p[:, :]), reads=[p], writes=[gb])
            for fc in range(FC):
                wg_ = wgb[cnt['wg'] % NWB]
                wu_ = wgb[(cnt['wg'] + 1) % NWB]
                cnt['wg'] += 2
                load_slab(wgu[e, fc, :, :], KC * 128, wg_)
                load_slab(wgu[e, FC + fc, :, :], KC * 128, wu_)
                bg_ap = bgu_s[:, e * 2 * FC + fc: e * 2 * FC + fc + 1]
                bu_ap = bgu_s[:, e * 2 * FC + FC + fc: e * 2 * FC + FC + fc + 1]
                for sb_ in range(NSB):
                    i = cnt['u'] % 2
                    cnt['u'] += 1
                    cs = slice(sb_ * 512, (sb_ + 1) * 512)
                    for k in range(KC):
                        kb.mm(pg[i], pg[i][:, :], wg_, wg_[:, k * 128:(k + 1) * 128], xb[k], xb[k][:, cs], k == 0, k == KC - 1)
                    for k in range(KC):
                        kb.mm(pu[i], pu[i][:, :], wu_, wu_[:, k * 128:(k + 1) * 128], xb[k], xb[k][:, cs], k == 0, k == KC - 1)
                    kb.op('dve', lambda en, i=i: en.tensor_scalar(g32[i][:, :], pg[i][:, :], bg_ap, 7.0, ALU.add, ALU.min),
                          reads=[pg[i], bgu_s], writes=[g32[i]])
                    kb.op('act', lambda en, i=i: en.activation(sig[i][:, :], g32[i][:, :], AF.Sigmoid, scale=1.702),
                          reads=[g32[i]], writes=[sig[i]])
                    kb.op('dve', lambda en, i=i: en.tensor_scalar(u1[i][:, :], pu[i][:, :], bu_ap, 7.0, ALU.add, ALU.min),
                          reads=[pu[i], bgu_s], writes=[u1[i]])
                    kb.op('dve', lambda en, i=i: en.tensor_scalar(u1[i][:, :], u1[i][:, :], -7.0, 1.0, ALU.max, ALU.add),
                          reads=[u1[i]], writes=[u1[i]])
                    kb.op('pool', lambda en, i=i: en.tensor_tensor(g32[i][:, :], g32[i][:, :], sig[i][:, :], ALU.mult),
                          reads=[g32[i], sig[i]], writes=[g32[i]])
                    kb.op('pool', lambda en, i=i: en.tensor_tensor(g32[i][:, :], g32[i][:, :], u1[i][:, :], ALU.mult),
                          reads=[g32[i], u1[i]], writes=[g32[i]])
                    kb.op('dve', lambda en, i=i, cs=cs: en.tensor_tensor(act[fc][:, cs], g32[i][:, :], gb[:, cs], ALU.mult),
                          reads=[g32[i], gb], writes=[act[fc]])
            for dc in range(KC):
                w_ = wdb[cnt['wd'] % 2]
                cnt['wd'] += 1
                load_slab(wd[e, dc, :, :], FC * 128, w_)
                for sb_ in range(NSB):
                    cs = slice(sb_ * 512, (sb_ + 1) * 512)
                    p = pd[cnt['pd'] % 2]
                    cnt['pd'] += 1
                    for f in range(FC):
                        kb.mm(p, p[:, :], w_, w_[:, f * 128:(f + 1) * 128], act[f], act[f][:, cs], f == 0, f == FC - 1)
                    if e == 0:
                        q = pm[cnt['pm'] % 2]
                        cnt['pm'] += 1
                        kb.mm(q, q[:, :], bd_s, bd_s[:, dc * 128:(dc + 1) * 128], gts, gts[:, cs], True, True)
                        kb.op('dve', lambda en, q=q, cs=cs: en.tensor_copy(acc[dc][:, cs], q[:, :]), reads=[q], writes=[acc[dc]])
                    kb.op('dve', lambda en, p=p, cs=cs: en.tensor_tensor(acc[dc][:, cs], p[:, :], acc[dc][:, cs], ALU.add),
                          reads=[p, acc[dc]], writes=[acc[dc]])
        for dc in range(KC):
            kb.dma('sp', y[dc * 128:(dc + 1) * 128, t0:t0 + TB], acc[dc][:, :], reads=[acc[dc]])
    kb.finish()
    return kb


def moe_host_inputs(x1T, G, w_gu, b_gu, w_down, b_down, c, EL):
    import ml_dtypes
    D = w_gu.shape[1]
    F2 = w_gu.shape[2]
    F = F2 // 2
    KC = D // 128
    FC = F // 128
    es = slice(c * EL, (c + 1) * EL)
    wg = w_gu[es].reshape(EL, KC, 128, 2 * FC, 128).transpose(0, 3, 2, 1, 4).reshape(EL, 2 * FC, 128, KC * 128)
    wdl = w_down[es].reshape(EL, FC, 128, KC, 128).transpose(0, 3, 2, 1, 4).reshape(EL, KC, 128, FC * 128)
    bg = b_gu[es].reshape(EL, 2 * FC, 128).transpose(2, 0, 1).reshape(128, EL * 2 * FC)
    sel = np.zeros((EL, EL * 128), np.float32)
    for e in range(EL):
        sel[e, e * 128:(e + 1) * 128] = 1.0
    return {
        "x1b": x1T,
        "gt": np.ascontiguousarray(G[:, es].T),
        "wgu": np.ascontiguousarray(wg),
        "bgu": np.ascontiguousarray(bg),
        "wd": np.ascontiguousarray(wdl),
        "bd": np.ascontiguousarray(b_down[es]),
        "sel": sel,
    }


DN_ALPHA = 4 ** 0.25
LN_EPS = 1e-5


def ln_fm(kb, r, out32, outb, g_s, b_s, ones32, D, NTK, tmp, pss):
    KC = len(r)
    for sb_ in range(NTK // 512):
        cs = slice(sb_ * 512, (sb_ + 1) * 512)
        ps_sum, ps_sq = pss
        for k in range(KC):
            kb.mm(ps_sum, ps_sum[:, :], ones32, ones32[:, :], r[k], r[k][:, cs], k == 0, k == KC - 1)
        for k in range(KC):
            sq = tmp['sq'][k % 2]
            kb.op('act', lambda e: e.activation(sq[:, :], r[k][:, cs], AF.Square), reads=[r[k]], writes=[sq])
            kb.mm(ps_sq, ps_sq[:, :], ones32, ones32[:, :], sq, sq[:, :], k == 0, k == KC - 1)
        mean, msq, rstd = tmp['mean'], tmp['msq'], tmp['rstd']
        kb.op('act', lambda e: e.mul(mean[:, :], ps_sum[:, :], 1.0 / D), reads=[ps_sum], writes=[mean])
        kb.op('dve', lambda e: e.tensor_tensor(msq[:, :], mean[:, :], mean[:, :], ALU.mult), reads=[mean], writes=[msq])
        kb.op('dve', lambda e: e.scalar_tensor_tensor(msq[:, :], ps_sq[:, :], 1.0 / D, msq[:, :], ALU.mult, ALU.subtract),
              reads=[ps_sq, msq], writes=[msq])
        kb.op('dve', lambda e: e.tensor_scalar(msq[:, :], msq[:, :], LN_EPS, None, ALU.add), reads=[msq], writes=[msq])
        kb.op('act', lambda e: e.activation(rstd[:, :], msq[:, :], AF.Sqrt), reads=[msq], writes=[rstd])
        kb.op('dve', lambda e: e.reciprocal(rstd[:, :], rstd[:, :]), reads=[rstd], writes=[rstd])
        for k in range(KC):
            kb.op('pool', lambda e: e.tensor_tensor(r[k][:, cs], r[k][:, cs], mean[:, :], ALU.subtract),
                  reads=[r[k], mean], writes=[r[k]])
            kb.op('dve', lambda e: e.tensor_tensor(r[k][:, cs], r[k][:, cs], rstd[:, :], ALU.mult),
                  reads=[r[k], rstd], writes=[r[k]])
            dst = out32[k] if out32 is not None else r[k]
            kb.op('dve', lambda e: e.tensor_scalar(dst[:, cs], r[k][:, cs], g_s[:, k:k + 1], b_s[:, k:k + 1], ALU.mult, ALU.add),
                  reads=[r[k], g_s, b_s], writes=[dst])
            if outb is not None:
                kb.op('act', lambda e: e.copy(outb[k][:, cs], dst[:, cs]), reads=[dst], writes=[outb[k]])


def build_sumln(D, NTK, NP):
    kb = KB()
    KC = D // 128
    xin = kb.dram("xin", [D, NTK], F32, "ExternalInput")
    part = kb.dram("part", [NP, D, NTK], F32, "ExternalInput")
    lng = kb.dram("lng", [128, KC], F32, "ExternalInput")
    lnb = kb.dram("lnb", [128, KC], F32, "ExternalInput")
    ones = kb.dram("ones", [128, 128], F32, "ExternalInput")
    xo = kb.dram("xo", [D, NTK], F32, "ExternalOutput")
    xob = kb.dram("xob", [D, NTK], BF16, "ExternalOutput")
    kb.dma_queue('sp', 8)
    r = [kb.sb([128, NTK], F32) for _ in range(KC)]
    ob = [kb.sb([128, NTK], BF16) for _ in range(2)]
    pb = [kb.sb([128, NTK], F32) for _ in range(3)]
    g_s = kb.sb([128, KC], F32)
    b_s = kb.sb([128, KC], F32)
    ones_s = kb.sb([128, 128], F32)
    tmp = {'sq': [kb.sb([128, 512], F32) for _ in range(2)], 'mean': kb.sb([128, 512], F32),
           'msq': kb.sb([128, 512], F32), 'rstd': kb.sb([128, 512], F32)}
    pss = [kb.ps(), kb.ps()]
    kb.dma('sp', g_s[:, :], lng[:, :], writes=[g_s])
    kb.dma('sp', b_s[:, :], lnb[:, :], writes=[b_s])
    kb.dma('sp', ones_s[:, :], ones[:, :], writes=[ones_s])
    n = 0
    for k in range(KC):
        rows = slice(k * 128, (k + 1) * 128)
        kb.dma('sp', r[k][:, :], xin[rows, :], writes=[r[k]])
        for c in range(NP):
            p = pb[n % 3]
            n += 1
            kb.dma('sp', p[:, :], part[c, rows, :], writes=[p])
            eng = 'dve' if c % 2 == 0 else 'pool'
            if c == 0:
                kb.op('dve', lambda e: e.scalar_tensor_tensor(r[k][:, :], r[k][:, :], DN_ALPHA, p[:, :], ALU.mult, ALU.add),
                      reads=[r[k], p], writes=[r[k]])
            else:
                kb.op(eng, lambda e: e.tensor_tensor(r[k][:, :], r[k][:, :], p[:, :], ALU.add), reads=[r[k], p], writes=[r[k]])
    ln_fm(kb, r, None, None, g_s, b_s, ones_s, D, NTK, tmp, pss)
    for k in range(KC):
        rows = slice(k * 128, (k + 1) * 128)
        kb.dma('sp', xo[rows, :], r[k][:, :], reads=[r[k]])
        o = ob[k % 2]
        kb.op('act', lambda e: e.copy(o[:, :], r[k][:, :]), reads=[r[k]], writes=[o])
        kb.dma('sp', xob[rows, :], o[:, :], reads=[o])
    kb.finish()
    return kb


def pp(v, KC):
    return np.ascontiguousarray(v.reshape(KC, 128).T)


def build_post(D, NTK, has_glu, NEXP=32):
    kb = KB()
    KC = D // 128
    HC = KC // 2
    feat = kb.dram("feat", [D, NTK], F32, "ExternalInput")
    wout = kb.dram("wout", [KC, 128, KC * 128], F32, "ExternalInput")
    if has_glu:
        wglu = kb.dram("wglu", [HC, 128, HC * 128], F32, "ExternalInput")
        bglu = kb.dram("bglu", [128, HC], F32, "ExternalInput")
    xin = kb.dram("xin", [D, NTK], F32, "ExternalInput")
    lng = kb.dram("lng", [128, KC], F32, "ExternalInput")
    lnb = kb.dram("lnb", [128, KC], F32, "ExternalInput")
    ones = kb.dram("ones", [128, 128], F32, "ExternalInput")
    wr = kb.dram("wr", [128, KC * NEXP], F32, "ExternalInput")
    brb = kb.dram("brb", [128, NEXP], F32, "ExternalInput")
    xo = kb.dram("xo", [D, NTK], F32, "ExternalOutput")
    xob = kb.dram("xob", [D, NTK], BF16, "ExternalOutput")
    gout = kb.dram("gout", [NTK, NEXP], F32, "ExternalOutput")
    kb.dma_queue('sp', 8)
    fb = [kb.sb([128, NTK], BF16) for _ in range(KC)]
    zb = [kb.sb([128, NTK], BF16) for _ in range(HC)] if has_glu else None
    r = [kb.sb([128, NTK], F32) for _ in range(KC)]
    ob = [kb.sb([128, NTK], BF16) for _ in range(2)]
    st32 = [kb.sb([128, NTK], F32) for _ in range(3)]
    stg = [kb.sb([128, KC * 128], F32) for _ in range(3)]
    wb = [kb.sb([128, KC * 128], BF16) for _ in range(2)]
    g_s = kb.sb([128, KC], F32)
    b_s = kb.sb([128, KC], F32)
    ones_s = kb.sb([128, 128], F32)
    wr_s = kb.sb([128, KC * NEXP], F32)
    brb_s = kb.sb([128, NEXP], F32)
    sg = [kb.sb([128, 512], F32) for _ in range(2)]
    tmp = {'sq': [kb.sb([128, 512], F32) for _ in range(2)], 'mean': kb.sb([128, 512], F32),
           'msq': kb.sb([128, 512], F32), 'rstd': kb.sb([128, 512], F32)}
    pss = [kb.ps(), kb.ps()]
    pmm = [kb.ps(), kb.ps()]
    prt = kb.ps([128, NEXP])
    for s_, d_ in ((g_s, lng), (b_s, lnb), (ones_s, ones), (wr_s, wr), (brb_s, brb)):
        kb.dma('sp', s_[:, :], d_[:, :], writes=[s_])
    if has_glu:
        bglu_s = kb.sb([128, HC], F32)
        kb.dma('sp', bglu_s[:, :], bglu[:, :], writes=[bglu_s])
    c = {'s': 0, 'w': 0, 'p': 0, 'g': 0}

    def load_cast(src_ap, dst, n):
        s = st32[c['s'] % 3]
        c['s'] += 1
        kb.dma('sp', s[:, 0:n], src_ap, writes=[s])
        kb.op('act', lambda e: e.copy(dst[:, 0:n], s[:, 0:n]), reads=[s], writes=[dst])

    def load_w(src_ap, n):
        s = stg[c['w'] % 3]
        w = wb[c['w'] % 2]
        c['w'] += 1
        kb.dma('sp', s[:, 0:n], src_ap, writes=[s])
        kb.op('act', lambda e: e.copy(w[:, 0:n], s[:, 0:n]), reads=[s], writes=[w])
        return w

    for k in range(KC):
        dst = zb[k] if (has_glu and k < HC) else fb[k]
        load_cast(feat[k * 128:(k + 1) * 128, :], dst, NTK)
        kb.dma('sp', r[k][:, :], xin[k * 128:(k + 1) * 128, :], writes=[r[k]])
    if has_glu:
        for fo in range(HC):
            w = load_w(wglu[fo, :, :], HC * 128)
            for sb_ in range(NTK // 512):
                cs = slice(sb_ * 512, (sb_ + 1) * 512)
                p = pmm[c['p'] % 2]
                c['p'] += 1
                for k in range(HC):
                    kb.mm(p, p[:, :], w, w[:, k * 128:(k + 1) * 128], zb[k], zb[k][:, cs], k == 0, k == HC - 1)
                s = sg[c['g'] % 2]
                c['g'] += 1
                kb.op('act', lambda e: e.activation(s[:, :], p[:, :], AF.Sigmoid, bias=bglu_s[:, fo:fo + 1]),
                      reads=[p, bglu_s], writes=[s])
                kb.op('dve', lambda e: e.tensor_tensor(fb[fo][:, cs], zb[fo][:, cs], s[:, :], ALU.mult),
                      reads=[zb[fo], s], writes=[fb[fo]])
    for dc in range(KC):
        w = load_w(wout[dc, :, :], KC * 128)
        for sb_ in range(NTK // 512):
            cs = slice(sb_ * 512, (sb_ + 1) * 512)
            p = pmm[c['p'] % 2]
            c['p'] += 1
            for k in range(KC):
                kb.mm(p, p[:, :], w, w[:, k * 128:(k + 1) * 128], fb[k], fb[k][:, cs], k == 0, k == KC - 1)
            kb.op('dve', lambda e: e.scalar_tensor_tensor(r[dc][:, cs], r[dc][:, cs], DN_ALPHA, p[:, :], ALU.mult, ALU.add),
                  reads=[r[dc], p], writes=[r[dc]])
    ln_fm(kb, r, None, None, g_s, b_s, ones_s, D, NTK, tmp, pss)
    for k in range(KC):
        rows = slice(k * 128, (k + 1) * 128)
        kb.dma('sp', xo[rows, :], r[k][:, :], reads=[r[k]])
        o = ob[k % 2]
        kb.op('act', lambda e: e.copy(o[:, :], r[k][:, :]), reads=[r[k]], writes=[o])
        kb.dma('sp', xob[rows, :], o[:, :], reads=[o])
    lg = [kb.sb([128, NEXP], F32) for _ in range(2)]
    ee = [kb.sb([128, NEXP], F32) for _ in range(2)]
    t8 = [kb.sb([128, 8], F32) for _ in range(2)]
    sc = [kb.sb([128, 4], F32) for _ in range(2)]
    for tt in range(NTK // 128):
        ts_ = slice(tt * 128, (tt + 1) * 128)
        i = tt % 2
        for k in range(KC):
            kb.mm(prt, prt[:, :], r[k], r[k][:, ts_], wr_s, wr_s[:, k * NEXP:(k + 1) * NEXP], k == 0, k == KC - 1)
        kb.op('dve', lambda e: e.tensor_tensor(lg[i][:, :], prt[:, :], brb_s[:, :], ALU.add), reads=[prt, brb_s], writes=[lg[i]])
        kb.op('dve', lambda e: e.max(t8[i][:, :], lg[i][:, :]), reads=[lg[i]], writes=[t8[i]])
        kb.op('dve', lambda e: e.tensor_scalar(sc[i][:, 0:1], t8[i][:, 0:1], -1.0, None, ALU.mult), reads=[t8[i]], writes=[sc[i]])
        kb.op('act', lambda e: e.activation(ee[i][:, :], lg[i][:, :], AF.Exp, bias=sc[i][:, 0:1]), reads=[lg[i], sc[i]], writes=[ee[i]])
        kb.op('dve', lambda e: e.tensor_scalar(lg[i][:, :], lg[i][:, :], t8[i][:, 3:4], None, ALU.is_ge), reads=[lg[i], t8[i]], writes=[lg[i]])
        kb.op('dve', lambda e: e.tensor_tensor(ee[i][:, :], ee[i][:, :], lg[i][:, :], ALU.mult), reads=[ee[i], lg[i]], writes=[ee[i]])
        kb.op('dve', lambda e: e.reduce_sum(sc[i][:, 1:2], ee[i][:, :], AX.X), reads=[ee[i]], writes=[sc[i]])
        kb.op('dve', lambda e: e.reciprocal(sc[i][:, 2:3], sc[i][:, 1:2]), reads=[sc[i]], writes=[sc[i]])
        kb.op('dve', lambda e: e.tensor_scalar(ee[i][:, :], ee[i][:, :], sc[i][:, 2:3], None, ALU.mult), reads=[ee[i], sc[i]], writes=[ee[i]])
        kb.dma('sp', gout[ts_, :], ee[i][:, :], reads=[ee[i]])
    kb.finish()
    return kb


def wslab(w, KI, KO):
    return np.ascontiguousarray(w.reshape(KI, 128, KO, 128).transpose(2, 1, 0, 3).reshape(KO, 128, KI * 128))


GROUPS = ((128, 1), (512, 4), (2048, 16))
PAD = 2048
NEG = -30000.0


def attn_mask_bias():
    cols = []
    i = np.arange(128)[:, None]
    for L, d in GROUPS:
        W = L + 128
        w = np.arange(W)[None, :]
        delta = i + L - w
        ok = (delta >= 0) & (delta <= L) & (delta % d == 0)
        cols.append(np.where(ok, 0.0, NEG).astype(np.float32))
    return np.ascontiguousarray(np.concatenate(cols, 1))


def build_attn(D, NT, HPC, DH=128):
    kb = KB()
    KC = D // 128
    NTI = NT // 128
    NPT = PAD // 128
    WT = sum(L + 128 for L, _ in GROUPS)
    NJ = WT // 128
    x2b = kb.dram("x2b", [D, NT], BF16, "ExternalInput")
    wq = kb.dram("wq", [HPC, 128, KC * 128], F32, "ExternalInput")
    wk = kb.dram("wk", [HPC, 128, KC * 128], F32, "ExternalInput")
    wv = kb.dram("wv", [HPC, 128, KC * 128], F32, "ExternalInput")
    mbd = kb.dram("mb", [128, WT], F32, "ExternalInput")
    idd = kb.dram("ident", [128, 128], BF16, "ExternalInput")
    o = kb.dram("o", [NT, HPC * DH], F32, "ExternalOutput")
    kb.dma_queue('sp', 8)
    xs = [[kb.sb([128, 512], BF16) for _ in range(KC)] for _ in range(2)]
    stg = [kb.sb([128, KC * 128], F32) for _ in range(2)]
    wqb, wkb, wvb = (kb.sb([128, KC * 128], BF16) for _ in range(3))
    qT = kb.sb([128, NT], BF16)
    kT = kb.sb([128, PAD + NT], BF16)
    vtmp = [kb.sb([128, 512], BF16) for _ in range(2)]
    vtok = kb.sb([128, (NPT + NTI) * 128], BF16)
    mb = kb.sb([128, WT], F32)
    ident = kb.sb([128, 128], BF16)
    Sm = [kb.sb([128, WT], F32) for _ in range(2)]
    P = [kb.sb([128, WT], BF16) for _ in range(2)]
    PT = [kb.sb([128, WT], BF16) for _ in range(2)]
    sc = [kb.sb([128, 4], F32) for _ in range(2)]
    osb = [kb.sb([128, DH], F32) for _ in range(2)]
    pp_ = [kb.ps() for _ in range(2)]
    pt_ = [kb.ps([128, 512], BF16) for _ in range(2)]
    po = [kb.ps([128, DH]) for _ in range(2)]
    kb.dma('sp', mb[:, :], mbd[:, :], writes=[mb])
    kb.dma('sp', ident[:, :], idd[:, :], writes=[ident])
    scale = DH ** -0.5
    c = {'p': 0, 't': 0, 'x': 0}

    def nps():
        p = pp_[c['p'] % 2]
        c['p'] += 1
        return p

    def npt():
        p = pt_[c['t'] % 2]
        c['t'] += 1
        return p

    for h in range(HPC):
        for i, (wd_, wb_) in enumerate(((wq, wqb), (wk, wkb), (wv, wvb))):
            s = stg[i % 2]
            kb.dma('sp', s[:, :], wd_[h, :, :], writes=[s])
            kb.op('act', lambda e: e.copy(wb_[:, :], s[:, :]), reads=[s], writes=[wb_])
        kb.op('pool', lambda e: e.memset(kT[:, 0:PAD], 0.0), writes=[kT])
        kb.op('pool', lambda e: e.memset(vtok[:, 0:NPT * 128], 0.0), writes=[vtok])
        for tb in range(NT // 512):
            xb = xs[c['x'] % 2]
            c['x'] += 1
            cs = slice(tb * 512, (tb + 1) * 512)
            for k in range(KC):
                kb.dma('sp', xb[k][:, :], x2b[k * 128:(k + 1) * 128, cs], writes=[xb[k]])
            p = nps()
            for k in range(KC):
                kb.mm(p, p[:, :], wqb, wqb[:, k * 128:(k + 1) * 128], xb[k], xb[k][:, :], k == 0, k == KC - 1)
            kb.op('act', lambda e: e.mul(qT[:, cs], p[:, :], scale), reads=[p], writes=[qT])
            p = nps()
            for k in range(KC):
                kb.mm(p, p[:, :], wkb, wkb[:, k * 128:(k + 1) * 128], xb[k], xb[k][:, :], k == 0, k == KC - 1)
            kb.op('dve', lambda e: e.tensor_copy(kT[:, PAD + tb * 512:PAD + (tb + 1) * 512], p[:, :]), reads=[p], writes=[kT])
            p = nps()
            for k in range(KC):
                kb.mm(p, p[:, :], wvb, wvb[:, k * 128:(k + 1) * 128], xb[k], xb[k][:, :], k == 0, k == KC - 1)
            vt = vtmp[tb % 2]
            kb.op('act', lambda e: e.copy(vt[:, :], p[:, :]), reads=[p], writes=[vt])
            q = npt()
            for j in range(4):
                kb.op('pe', lambda e: e.transpose(q[:, j * 128:(j + 1) * 128], vt[:, j * 128:(j + 1) * 128], ident[:, :]),
                      reads=[vt, ident], writes=[q])
            a0 = (NPT + tb * 4) * 128
            kb.op('dve', lambda e: e.tensor_copy(vtok[:, a0:a0 + 512], q[:, :]), reads=[q], writes=[vtok])
        def stage1(ti):
            s0 = ti * 128
            u = ti % 2
            col = 0
            for L, d in GROUPS:
                W = L + 128
                kstart = PAD + s0 - L
                for c0 in range(0, W, 512):
                    n = min(512, W - c0)
                    p = nps()
                    kb.mm(p, p[:, 0:n], qT, qT[:, s0:s0 + 128], kT, kT[:, kstart + c0:kstart + c0 + n], True, True)
                    kb.op('dve', lambda e: e.tensor_tensor(Sm[u][:, col + c0:col + c0 + n], p[:, 0:n], mb[:, col + c0:col + c0 + n], ALU.add),
                          reads=[p, mb], writes=[Sm[u]])
                if s0 < L:
                    kb.op('pool', lambda e: e.memset(Sm[u][:, col:col + (L - s0)], NEG), writes=[Sm[u]])
                col += W
            kb.op('dve', lambda e: e.reduce_max(sc[u][:, 0:1], Sm[u][:, :], AX.X), reads=[Sm[u]], writes=[sc[u]])
            kb.op('dve', lambda e: e.tensor_scalar(sc[u][:, 1:2], sc[u][:, 0:1], -1.0, None, ALU.mult), reads=[sc[u]], writes=[sc[u]])
            kb.op('act', lambda e: e.activation(P[u][:, :], Sm[u][:, :], AF.Exp, bias=sc[u][:, 1:2], accum_out=sc[u][:, 2:3]),
                  reads=[Sm[u], sc[u]], writes=[P[u], sc[u]])

        def stage2(ti):
            s0 = ti * 128
            u = ti % 2
            vt_idx = []
            for L, d in GROUPS:
                vt_idx += list(range(NPT + ti - L // 128, NPT + ti + 1))
            for j4 in range(NJ // 4):
                q = npt()
                for j in range(4):
                    jj = j4 * 4 + j
                    kb.op('pe', lambda e: e.transpose(q[:, j * 128:(j + 1) * 128], P[u][:, jj * 128:(jj + 1) * 128], ident[:, :]),
                          reads=[P[u], ident], writes=[q])
                if j4 % 2 == 0:
                    kb.op('act', lambda e: e.copy(PT[u][:, j4 * 512:(j4 + 1) * 512], q[:, :]), reads=[q], writes=[PT[u]])
                else:
                    kb.op('dve', lambda e: e.tensor_copy(PT[u][:, j4 * 512:(j4 + 1) * 512], q[:, :]), reads=[q], writes=[PT[u]])
            pq = po[u]
            for jj in range(NJ):
                a = vt_idx[jj]
                kb.mm(pq, pq[:, :], PT[u], PT[u][:, jj * 128:(jj + 1) * 128], vtok, vtok[:, a * 128:(a + 1) * 128], jj == 0, jj == NJ - 1)
            kb.op('dve', lambda e: e.reciprocal(sc[u][:, 3:4], sc[u][:, 2:3]), reads=[sc[u]], writes=[sc[u]])
            kb.op('dve', lambda e: e.tensor_scalar(osb[u][:, :], pq[:, :], sc[u][:, 3:4], None, ALU.mult), reads=[pq, sc[u]], writes=[osb[u]])
            kb.dma('sp', o[s0:s0 + 128, h * DH:(h + 1) * DH], osb[u][:, :], reads=[osb[u]])

        stage1(0)
        for ti in range(NTI):
            if ti + 1 < NTI:
                stage1(ti + 1)
            stage2(ti)
    kb.finish()
    return kb


TWO_PI = 6.283185307179586
MAGIC = 12582912.0
GLA_EPS = 1e-6


def s5_host(lam_re, lam_im, log_step, b_re, b_im, c_re, c_im, d_skip, c):
    lr = np.zeros(512, np.float32)
    li = np.zeros(512, np.float32)
    ls = np.zeros(512, np.float32)
    BreT = np.zeros((128, 512), np.float32)
    BimT = np.zeros((128, 512), np.float32)
    Cre = np.zeros((128, 512), np.float32)
    Cim = np.zeros((128, 512), np.float32)
    for q in range(4):
        for half in range(2):
            gl = 2 * q + half
            g = 8 * c + gl
            ms = slice(q * 128 + half * 64, q * 128 + half * 64 + 64)
            lr[ms] = lam_re[g]
            li[ms] = lam_im[g]
            ls[ms] = log_step[g]
            BreT[16 * gl:16 * gl + 16, ms] = b_re[g].T
            BimT[16 * gl:16 * gl + 16, ms] = b_im[g].T
            Cre[half * 64:half * 64 + 64, q * 128 + 16 * gl:q * 128 + 16 * gl + 16] = c_re[g].T
            Cim[half * 64:half * 64 + 64, q * 128 + 16 * gl:q * 128 + 16 * gl + 16] = c_im[g].T
    rep = lambda v: np.ascontiguousarray(np.tile(v[None, :], (128, 1)))
    return {"s5_lr": rep(lr), "s5_li": rep(li), "s5_ls": rep(ls), "s5_bre": BreT, "s5_bim": BimT,
            "s5_cre": Cre, "s5_cim": Cim,
            "s5_d": np.ascontiguousarray(d_skip[128 * c:128 * (c + 1)].reshape(128, 1))}


def build_mix(D, NT, do_s5=True, do_gla=True):
    kb = KB()
    KC = D // 128
    TB = 512
    NBLK = NT // TB
    DK, DV = 128, 256
    xT = kb.dram("xT", [D, NT], F32, "ExternalInput")
    w5 = kb.dram("w5", [5, 128, KC * 128], F32, "ExternalInput")
    wvd = kb.dram("wv", [128, KC * DV], F32, "ExternalInput")
    wgd = kb.dram("wg", [128, KC * 16], F32, "ExternalInput")
    pnames = ["s5_lr", "s5_li", "s5_ls", "s5_bre", "s5_bim", "s5_cre", "s5_cim"]
    pd = {n: kb.dram(n, [128, 512], F32, "ExternalInput") for n in pnames}
    s5d = kb.dram("s5_d", [128, 1], F32, "ExternalInput")
    wg2d = kb.dram("wg2", [16, 128], F32, "ExternalInput")
    bg2d = kb.dram("bg2", [128, 1], F32, "ExternalInput")
    ngd = kb.dram("ng", [128, 2], F32, "ExternalInput")
    cmaskd = kb.dram("cmask", [128, 128], F32, "ExternalInput")
    rmaskd = kb.dram("rmask", [128, 512], F32, "ExternalInput")
    onesd = kb.dram("ones", [128, 512], F32, "ExternalInput")
    identd = kb.dram("ident", [128, 128], F32, "ExternalInput")
    zT = kb.dram("zT", [128, NT], F32, "ExternalOutput")
    ybT = kb.dram("ybT", [DV, NT], F32, "ExternalOutput")
    kb.dma_queue('sp', 8)

    def ld(shape, src, dt=F32):
        t = kb.sb(shape, dt)
        kb.dma('sp', t[:, :], src[:, :], writes=[t])
        return t
    ones = ld([128, 512], onesd)
    ident = ld([128, 128], identd)
    identb = kb.sb([128, 128], BF16)
    kb.op('act', lambda e: e.copy(identb[:, :], ident[:, :]), reads=[ident], writes=[identb])
    onesb = kb.sb([128, 128], BF16)
    kb.op('act', lambda e: e.copy(onesb[:, :], ones[:, 0:128]), reads=[ones], writes=[onesb])

    wsl = [kb.sb([128, KC * 128], BF16) for _ in range(5)]
    wvb = kb.sb([128, KC * DV], BF16)
    wgb = kb.sb([128, KC * 16], BF16)
    if do_s5:
        bbr, bbi = kb.sb([128, 512], BF16), kb.sb([128, 512], BF16)
        creb, cimb = kb.sb([128, 512], BF16), kb.sb([128, 512], BF16)
        rfull = [kb.sb([128, 512], F32) for _ in range(4)]
        Ec = [kb.sb([128, 512], F32) for _ in range(4)]
        Es = [kb.sb([128, 512], F32) for _ in range(4)]
        sin_ = [kb.sb([128, 2], F32) for _ in range(4)]
        s5dv = ld([128, 1], s5d)
    if do_gla:
        wg2 = ld([16, 128], wg2d)
        bg2 = ld([128, 1], bg2d)
        nbg2 = kb.sb([128, 1], F32)
        ng = ld([128, 2], ngd)
        cmask = ld([128, 128], cmaskd)
        rmask = ld([128, 512], rmaskd)
    kb.push_scope()
    stg = [kb.sb([128, KC * DV], F32) for _ in range(2)]
    ns = [0]

    def ldw(src_ap, dst, n):
        s = stg[ns[0] % 2]
        ns[0] += 1
        kb.dma('sp', s[:, 0:n], src_ap, writes=[s])
        kb.op('act', lambda e: e.copy(dst[:, 0:n], s[:, 0:n]), reads=[s], writes=[dst])
    for i in range(5):
        ldw(w5[i, :, :], wsl[i], KC * 128)
    ldw(wvd[:, :], wvb, KC * DV)
    ldw(wgd[:, :], wgb, KC * 16)

    pmm = [kb.ps() for _ in range(2)]
    pc = [0]

    def nps():
        p = pmm[pc[0] % 2]
        pc[0] += 1
        return p

    V = lambda eng, f, r, w: kb.op(eng, f, reads=r, writes=w)

    if do_s5:
        P = {n: ld([128, 512], pd[n]) for n in pnames}
        W = lambda: kb.sb([128, 512], F32)
        lr, li, dt = P["s5_lr"], P["s5_li"], P["s5_ls"]
        V('dve', lambda e: e.tensor_scalar(lr[:, :], lr[:, :], -1e-4, None, ALU.min), [lr], [lr])
        V('act', lambda e: e.activation(dt[:, :], dt[:, :], AF.Exp), [dt], [dt])
        a_, th = W(), W()
        V('dve', lambda e: e.tensor_tensor(a_[:, :], lr[:, :], dt[:, :], ALU.mult), [lr, dt], [a_])
        V('dve', lambda e: e.tensor_tensor(th[:, :], li[:, :], dt[:, :], ALU.mult), [li, dt], [th])
        mag = W()
        V('act', lambda e: e.activation(mag[:, :], a_[:, :], AF.Exp), [a_], [mag])

        def sincos(src, shift, dst):
            t = W()
            V('dve', lambda e: e.tensor_scalar(t[:, :], src[:, :], shift, 1.0 / TWO_PI, ALU.add, ALU.mult), [src], [t])
            k_ = W()
            V('dve', lambda e: e.tensor_scalar(k_[:, :], t[:, :], MAGIC, None, ALU.add), [t], [k_])
            V('dve', lambda e: e.tensor_scalar(k_[:, :], k_[:, :], MAGIC, None, ALU.subtract), [k_], [k_])
            V('dve', lambda e: e.tensor_tensor(t[:, :], t[:, :], k_[:, :], ALU.subtract), [t, k_], [t])
            V('dve', lambda e: e.tensor_scalar(t[:, :], t[:, :], 0.5, -0.5, ALU.min, ALU.max), [t], [t])
            V('act', lambda e: e.activation(dst[:, :], t[:, :], AF.Sin, scale=TWO_PI), [t], [dst])
        sn, cs_ = W(), W()
        sincos(th, 0.0, sn)
        sincos(th, TWO_PI / 4, cs_)
        abr, abi = W(), W()
        V('dve', lambda e: e.tensor_tensor(abr[:, :], mag[:, :], cs_[:, :], ALU.mult), [mag, cs_], [abr])
        V('dve', lambda e: e.tensor_tensor(abi[:, :], mag[:, :], sn[:, :], ALU.mult), [mag, sn], [abi])
        den, t1, t2 = W(), W(), W()
        V('dve', lambda e: e.tensor_tensor(den[:, :], lr[:, :], lr[:, :], ALU.mult), [lr], [den])
        V('dve', lambda e: e.tensor_tensor(t1[:, :], li[:, :], li[:, :], ALU.mult), [li], [t1])
        V('dve', lambda e: e.tensor_tensor(den[:, :], den[:, :], t1[:, :], ALU.add), [den, t1], [den])
        V('dve', lambda e: e.reciprocal(den[:, :], den[:, :]), [den], [den])
        nr = W()
        V('dve', lambda e: e.tensor_scalar(nr[:, :], abr[:, :], -1.0, None, ALU.add), [abr], [nr])
        fre, fim = W(), W()
        V('dve', lambda e: e.tensor_tensor(t1[:, :], nr[:, :], lr[:, :], ALU.mult), [nr, lr], [t1])
        V('dve', lambda e: e.tensor_tensor(t2[:, :], abi[:, :], li[:, :], ALU.mult), [abi, li], [t2])
        V('dve', lambda e: e.tensor_tensor(t1[:, :], t1[:, :], t2[:, :], ALU.add), [t1, t2], [t1])
        V('dve', lambda e: e.tensor_tensor(fre[:, :], t1[:, :], den[:, :], ALU.mult), [t1, den], [fre])
        V('dve', lambda e: e.tensor_tensor(t1[:, :], abi[:, :], lr[:, :], ALU.mult), [abi, lr], [t1])
        V('dve', lambda e: e.tensor_tensor(t2[:, :], nr[:, :], li[:, :], ALU.mult), [nr, li], [t2])
        V('dve', lambda e: e.tensor_tensor(t1[:, :], t1[:, :], t2[:, :], ALU.subtract), [t1, t2], [t1])
        V('dve', lambda e: e.tensor_tensor(fim[:, :], t1[:, :], den[:, :], ALU.mult), [t1, den], [fim])
        bre, bim = P["s5_bre"], P["s5_bim"]
        V('dve', lambda e: e.tensor_tensor(t1[:, :], fre[:, :], bre[:, :], ALU.mult), [fre, bre], [t1])
        V('dve', lambda e: e.tensor_tensor(t2[:, :], fim[:, :], bim[:, :], ALU.mult), [fim, bim], [t2])
        V('dve', lambda e: e.tensor_tensor(bbr[:, :], t1[:, :], t2[:, :], ALU.subtract), [t1, t2], [bbr])
        V('dve', lambda e: e.tensor_tensor(t1[:, :], fre[:, :], bim[:, :], ALU.mult), [fre, bim], [t1])
        V('dve', lambda e: e.tensor_tensor(t2[:, :], fim[:, :], bre[:, :], ALU.mult), [fim, bre], [t2])
        V('dve', lambda e: e.tensor_tensor(bbi[:, :], t1[:, :], t2[:, :], ALU.add), [t1, t2], [bbi])
        V('act', lambda e: e.copy(creb[:, :], P["s5_cre"][:, :]), [P["s5_cre"]], [creb])
        V('act', lambda e: e.mul(cimb[:, :], P["s5_cim"][:, :], -1.0), [P["s5_cim"]], [cimb])
        for q in range(4):
            qs = slice(q * 128, (q + 1) * 128)
            cols = {}
            for nm, src in (("mag", mag), ("cs", cs_), ("sn", sn)):
                p = nps()
                V('pe', lambda e: e.transpose(p[:, 0:128], src[:, qs], ident[:, :]), [src, ident], [p])
                ct = kb.sb([128, 128], F32)
                V('act', lambda e: e.copy(ct[:, :], p[:, 0:128]), [p], [ct])
                cols[nm] = ct
            rf = rfull[q]
            V('dve', lambda e: e.tensor_scalar(rf[:, :], ones[:, :], cols["mag"][:, 0:1], None, ALU.mult), [ones, cols["mag"]], [rf])
            ec, es = Ec[q], Es[q]
            V('act', lambda e: e.copy(ec[:, 0:1], cols["cs"][:, 0:1]), [cols["cs"]], [ec])
            V('act', lambda e: e.copy(es[:, 0:1], cols["sn"][:, 0:1]), [cols["sn"]], [es])
            pw = kb.sb([128, 8], F32)
            V('act', lambda e: e.copy(pw[:, 0:1], cols["cs"][:, 0:1]), [cols["cs"]], [pw])
            V('act', lambda e: e.copy(pw[:, 1:2], cols["sn"][:, 0:1]), [cols["sn"]], [pw])
            n = 1
            tmpw = W()
            while n < TB:
                V('dve', lambda e: e.tensor_scalar(pw[:, 2:3], pw[:, 1:2], -1.0, None, ALU.mult), [pw], [pw])
                V('dve', lambda e: e.tensor_scalar(tmpw[:, 0:n], ec[:, 0:n], pw[:, 0:1], None, ALU.mult), [ec, pw], [tmpw])
                V('dve', lambda e: e.scalar_tensor_tensor(ec[:, n:2 * n], es[:, 0:n], pw[:, 2:3], tmpw[:, 0:n], ALU.mult, ALU.add),
                  [es, pw, tmpw], [ec])
                V('dve', lambda e: e.tensor_scalar(tmpw[:, 0:n], es[:, 0:n], pw[:, 0:1], None, ALU.mult), [es, pw], [tmpw])
                V('dve', lambda e: e.scalar_tensor_tensor(es[:, n:2 * n], ec[:, 0:n], pw[:, 1:2], tmpw[:, 0:n], ALU.mult, ALU.add),
                  [ec, pw, tmpw], [es])
                V('dve', lambda e: e.tensor_tensor(pw[:, 3:4], pw[:, 0:1], pw[:, 0:1], ALU.mult), [pw], [pw])
                V('dve', lambda e: e.tensor_tensor(pw[:, 4:5], pw[:, 1:2], pw[:, 1:2], ALU.mult), [pw], [pw])
                V('dve', lambda e: e.tensor_tensor(pw[:, 5:6], pw[:, 0:1], pw[:, 1:2], ALU.mult), [pw], [pw])
                V('dve', lambda e: e.tensor_tensor(pw[:, 0:1], pw[:, 3:4], pw[:, 4:5], ALU.subtract), [pw], [pw])
                V('dve', lambda e: e.tensor_scalar(pw[:, 1:2], pw[:, 5:6], 2.0, None, ALU.mult), [pw], [pw])
                n *= 2
        for q in range(4):
            V('pool', lambda e: e.memset(sin_[q][:, :], 0.0), [], [sin_[q]])
    kb.pop_scope()
    if do_s5:
        s5w = {n_: [kb.sb([128, 512], F32) for _ in range(2)] for n_ in ("t1", "t2", "xr", "xi", "zr", "zi", "sr", "si")}
        s5b = {n_: [kb.sb([128, 512], BF16) for _ in range(2)] for n_ in ("srb", "sib")}
        yw = [kb.sb([128, 512], F32) for _ in range(4)]
        py = kb.ps()

    if do_gla:
        V('dve', lambda e: e.tensor_scalar(nbg2[:, :], bg2[:, :], -1.0, None, ALU.mult), [bg2], [nbg2])
        S = kb.sb([128, DV], F32)
        Sb = kb.sb([128, DV], BF16)
        V('pool', lambda e: e.memset(S[:, :], 0.0), [], [S])
        V('pool', lambda e: e.memset(Sb[:, :], 0.0), [], [Sb])
        glT = kb.sb([16, 512], F32)
        qt = kb.sb([128, 512], BF16)
        kt = kb.sb([128, 512], BF16)
        q32 = kb.sb([128, 512], F32)
        k32 = kb.sb([128, 512], F32)
        Bc = kb.sb([128, 512], F32)
        eb = kb.sb([128, 512], F32)
        enb = kb.sb([128, 512], F32)
        l1 = kb.sb([128, 512], F32)
        sr_ = [kb.sb([128, 512], F32) for _ in range(2)]
        vtk = [kb.sb([128, DV], BF16) for _ in range(4)]
        kdT = kb.sb([128, 128], BF16)
        kd = kb.sb([128, 128], BF16)
        AT = kb.sb([128, 128], BF16)
        sq = [kb.sb([128, 128], BF16) for _ in range(2)]
        rs = kb.sb([128, 128], F32)
        on = [kb.sb([128, 128], F32) for _ in range(2)]
        ybo = [kb.sb([128, 512], F32) for _ in range(2)]
        pA = kb.ps([128, 128])
        pO = [kb.ps([128, 128]) for _ in range(2)]
        pT = kb.ps([128, 128], BF16)
        pKV = kb.ps([128, DV])

    xst = [kb.sb([128, TB], F32) for _ in range(3)]
    xb = [[kb.sb([128, TB], BF16) for _ in range(KC)] for _ in range(2)]
    ub = [kb.sb([128, TB], BF16) for _ in range(2)]
    nx = [0]

    for blk in range(NBLK):
        cs = slice(blk * TB, (blk + 1) * TB)
        xs = xb[blk % 2]
        for k in range(KC):
            s = xst[nx[0] % 3]
            nx[0] += 1
            kb.dma('sp', s[:, :], xT[k * 128:(k + 1) * 128, cs], writes=[s])
            eng = 'act' if k % 2 == 0 else 'pool'
            if eng == 'act':
                V('act', lambda e: e.copy(xs[k][:, :], s[:, :]), [s], [xs[k]])
            else:
                V('pool', lambda e: e.tensor_copy(xs[k][:, :], s[:, :]), [s], [xs[k]])

        def proj(w, ncols, wcols=128):
            p = nps()
            for k in range(KC):
                kb.mm(p, p[0:ncols, :], w, w[:, k * wcols:k * wcols + ncols], xs[k], xs[k][:, :], k == 0, k == KC - 1)
            return p

        if do_s5:
            u = ub[blk % 2]
            p = proj(wsl[0], 128)
            V('act', lambda e: e.copy(u[:, :], p[:, :]), [p], [u])
            for q in range(4):
                qs = slice(q * 128, (q + 1) * 128)
                i = q % 2
                w_ = {n_: s5w[n_][i] for n_ in s5w}
                pxr, pxi = nps(), nps()
                kb.mm(pxr, pxr[:, :], bbr, bbr[:, qs], u, u[:, :], True, True)
                kb.mm(pxi, pxi[:, :], bbi, bbi[:, qs], u, u[:, :], True, True)
                ec, es = Ec[q], Es[q]
                V('dve', lambda e: e.tensor_tensor(w_["t1"][:, :], pxr[:, :], ec[:, :], ALU.mult), [pxr, ec], [w_["t1"]])
                V('dve', lambda e: e.tensor_tensor(w_["t2"][:, :], pxi[:, :], es[:, :], ALU.mult), [pxi, es], [w_["t2"]])
                V('pool', lambda e: e.tensor_tensor(w_["xr"][:, :], w_["t1"][:, :], w_["t2"][:, :], ALU.add), [w_["t1"], w_["t2"]], [w_["xr"]])
                V('dve', lambda e: e.tensor_tensor(w_["t1"][:, :], pxi[:, :], ec[:, :], ALU.mult), [pxi, ec], [w_["t1"]])
                V('dve', lambda e: e.tensor_tensor(w_["t2"][:, :], pxr[:, :], es[:, :], ALU.mult), [pxr, es], [w_["t2"]])
                V('pool', lambda e: e.tensor_tensor(w_["xi"][:, :], w_["t1"][:, :], w_["t2"][:, :], ALU.subtract), [w_["t1"], w_["t2"]], [w_["xi"]])
                V('dve', lambda e: e.tensor_tensor_scan(w_["zr"][:, :], rfull[q][:, :], w_["xr"][:, :], sin_[q][:, 0:1], ALU.mult, ALU.add),
                  [rfull[q], w_["xr"], sin_[q]], [w_["zr"]])
                V('dve', lambda e: e.tensor_tensor_scan(w_["zi"][:, :], rfull[q][:, :], w_["xi"][:, :], sin_[q][:, 1:2], ALU.mult, ALU.add),
                  [rfull[q], w_["xi"], sin_[q]], [w_["zi"]])
                V('dve', lambda e: e.tensor_tensor(w_["t1"][:, :], w_["zr"][:, :], ec[:, :], ALU.mult), [w_["zr"], ec], [w_["t1"]])
                V('pool', lambda e: e.tensor_tensor(w_["t2"][:, :], w_["zi"][:, :], es[:, :], ALU.mult), [w_["zi"], es], [w_["t2"]])
                V('pool', lambda e: e.tensor_tensor(w_["sr"][:, :], w_["t1"][:, :], w_["t2"][:, :], ALU.subtract), [w_["t1"], w_["t2"]], [w_["sr"]])
                V('dve', lambda e: e.tensor_tensor(w_["t1"][:, :], w_["zr"][:, :], es[:, :], ALU.mult), [w_["zr"], es], [w_["t1"]])
                V('pool', lambda e: e.tensor_tensor(w_["t2"][:, :], w_["zi"][:, :], ec[:, :], ALU.mult), [w_["zi"], ec], [w_["t2"]])
                V('pool', lambda e: e.tensor_tensor(w_["si"][:, :], w_["t1"][:, :], w_["t2"][:, :], ALU.add), [w_["t1"], w_["t2"]], [w_["si"]])
                srb, sib = s5b["srb"][i], s5b["sib"][i]
                V('act', lambda e: e.copy(srb[:, :], w_["sr"][:, :]), [w_["sr"]], [srb])
                V('act', lambda e: e.copy(sib[:, :], w_["si"][:, :]), [w_["si"]], [sib])
                V('act', lambda e: e.copy(sin_[q][:, 0:1], w_["sr"][:, TB - 1:TB]), [w_["sr"]], [sin_[q]])
                V('act', lambda e: e.copy(sin_[q][:, 1:2], w_["si"][:, TB - 1:TB]), [w_["si"]], [sin_[q]])
                kb.mm(py, py[:, :], creb, creb[:, qs], srb, srb[:, :], q == 0, False)
                kb.mm(py, py[:, :], cimb, cimb[:, qs], sib, sib[:, :], False, q == 3)
            y, y2, y3, zz = yw
            V('dve', lambda e: e.scalar_tensor_tensor(y[:, :], u[:, :], s5dv[:, 0:1], py[:, :], ALU.mult, ALU.add), [u, s5dv, py], [y])
            V('pool', lambda e: e.tensor_tensor(y2[:, :], y[:, :], y[:, :], ALU.mult), [y], [y2])
            V('pool', lambda e: e.tensor_scalar(y2[:, :], y2[:, :], 0.044715, 1.0, ALU.mult, ALU.add), [y2], [y2])
            V('pool', lambda e: e.tensor_tensor(y3[:, :], y2[:, :], y[:, :], ALU.mult), [y2, y], [y3])
            V('act', lambda e: e.activation(y3[:, :], y3[:, :], AF.Sigmoid, scale=1.5957691216057308), [y3], [y3])
            V('dve', lambda e: e.tensor_tensor(zz[:, :], y[:, :], y3[:, :], ALU.mult), [y, y3], [zz])
            kb.dma('sp', zT[:, cs], zz[:, :], reads=[zz])

        if do_gla:
            p = proj(wsl[1], 128)
            V('act', lambda e: e.mul(q32[:, :], p[:, :], DK ** -0.5), [p], [q32])
            p = proj(wsl[2], 128)
            V('act', lambda e: e.copy(k32[:, :], p[:, :]), [p], [k32])
            for a in range(2):
                p = proj(wsl[3 + a], 128)
                V('act', lambda e: e.activation(sr_[a][:, :], p[:, :], AF.Sigmoid), [p], [sr_[a]])
                V('dve', lambda e: e.tensor_tensor(sr_[a][:, :], sr_[a][:, :], p[:, :], ALU.mult), [sr_[a], p], [sr_[a]])
            p = proj(wgb, 16, 16)
            V('act', lambda e: e.copy(glT[:, :], p[0:16, :]), [p], [glT])
            for t in range(4):
                pv = nps()
                for k in range(KC):
                    kb.mm(pv, pv[:, 0:DV], xs[k], xs[k][:, t * 128:(t + 1) * 128], wvb, wvb[:, k * DV:(k + 1) * DV], k == 0, k == KC - 1)
                V('act', lambda e: e.copy(vtk[t][:, :], pv[:, 0:DV]), [pv], [vtk[t]])
            p = nps()
            kb.mm(p, p[:, :], wg2, wg2[:, :], glT, glT[:, :], True, True)
            V('act', lambda e: e.activation(l1[:, :], p[:, :], AF.Exp, bias=nbg2[:, 0:1], scale=-1.0), [p, nbg2], [l1])
            V('dve', lambda e: e.tensor_scalar(l1[:, :], l1[:, :], 1.0, None, ALU.add), [l1], [l1])
            V('act', lambda e: e.activation(l1[:, :], l1[:, :], AF.Ln), [l1], [l1])
            V('dve', lambda e: e.tensor_tensor_scan(Bc[:, :], rmask[:, :], l1[:, :], 0.0, ALU.mult, ALU.add), [rmask, l1], [Bc])
            V('act', lambda e: e.activation(eb[:, :], Bc[:, :], AF.Exp, scale=-1.0 / 16), [Bc], [eb])
            V('act', lambda e: e.activation(enb[:, :], Bc[:, :], AF.Exp, scale=1.0 / 16), [Bc], [enb])
            V('dve', lambda e: e.tensor_tensor(qt[:, :], q32[:, :], eb[:, :], ALU.mult), [q32, eb], [qt])
            V('dve', lambda e: e.tensor_tensor(k32[:, :], k32[:, :], enb[:, :], ALU.mult), [k32, enb], [k32])
            V('act', lambda e: e.copy(kt[:, :], k32[:, :]), [k32], [kt])
            yo = ybo
            for c_ in range(4):
                ch = slice(c_ * 128, (c_ + 1) * 128)
                el = eb[:, c_ * 128 + 127:c_ * 128 + 128]
                V('dve', lambda e: e.tensor_scalar(kdT[:, :], k32[:, ch], el, None, ALU.mult), [k32, eb], [kdT])
                V('pe', lambda e: e.transpose(pT[:, :], kdT[:, :], identb[:, :]), [kdT, identb], [pT])
                V('act', lambda e: e.copy(kd[:, :], pT[:, :]), [pT], [kd])
                kb.mm(pA, pA[:, :], kt, kt[:, ch], qt, qt[:, ch], True, True)
                V('dve', lambda e: e.tensor_tensor(AT[:, :], pA[:, :], cmask[:, :], ALU.mult), [pA, cmask], [AT])
                for a in range(2):
                    kb.mm(pO[a], pO[a][:, :], vtk[c_], vtk[c_][:, a * 128:(a + 1) * 128], AT, AT[:, :], True, False)
                    kb.mm(pO[a], pO[a][:, :], Sb, Sb[:, a * 128:(a + 1) * 128], qt, qt[:, ch], False, True)
                kb.mm(pKV, pKV[:, :], kd, kd[:, :], vtk[c_], vtk[c_][:, :], True, True)
                V('dve', lambda e: e.scalar_tensor_tensor(S[:, :], S[:, :], el, pKV[:, :], ALU.mult, ALU.add), [S, eb, pKV], [S])
                V('act', lambda e: e.copy(Sb[:, :], S[:, :]), [S], [Sb])
                for a in range(2):
                    V('act', lambda e: e.activation(sq[a][:, :], pO[a][:, :], AF.Square), [pO[a]], [sq[a]])
                pn = nps()
                for a in range(2):
                    kb.mm(pn, pn[:, 0:128], onesb, onesb[:, :], sq[a], sq[a][:, :], a == 0, a == 1)
                V('dve', lambda e: e.tensor_scalar(rs[:, :], pn[:, 0:128], 1.0 / DV, GLA_EPS, ALU.mult, ALU.add), [pn], [rs])
                V('act', lambda e: e.activation(rs[:, :], rs[:, :], AF.Sqrt), [rs], [rs])
                V('dve', lambda e: e.reciprocal(rs[:, :], rs[:, :]), [rs], [rs])
                for a in range(2):
                    V('dve', lambda e: e.scalar_tensor_tensor(on[a][:, :], pO[a][:, :], ng[:, a:a + 1], rs[:, :], ALU.mult, ALU.mult),
                      [pO[a], ng, rs], [on[a]])
                    V('pool', lambda e: e.tensor_tensor(yo[a][:, ch], on[a][:, :], sr_[a][:, ch], ALU.mult), [on[a], sr_[a]], [yo[a]])
            for a in range(2):
                kb.dma('sp', ybT[a * 128:(a + 1) * 128, cs], yo[a][:, :], reads=[yo[a]])
    kb.finish()
    return kb

import ml_dtypes
from concourse.bass_utils import run_bass_kernel_spmd

NCORES = 8
_PROGS = {}


def _prog(key, fn):
    if key not in _PROGS:
        _PROGS[key] = fn()
    return _PROGS[key]


def _run(kb, ins):
    res = run_bass_kernel_spmd(kb.nc, ins, core_ids=list(range(NCORES)))
    return res.results


def kernel(**inp):
    x = np.asarray(inp["x"])[0]
    NT, D = x.shape
    KC = D // 128
    NTK = NT // NCORES
    F = inp["moe_w_gu"].shape[-1] // 2
    NE = inp["moe_w_gu"].shape[1]
    EL = NE // NCORES
    xT = np.ascontiguousarray(x.T)
    ones512 = np.ones((128, 512), np.float32)
    ones128 = np.ones((128, 128), np.float32)
    ident = np.eye(128, dtype=np.float32)
    ts = [slice(c * NTK, (c + 1) * NTK) for c in range(NCORES)]
    sh = lambda a, c: np.ascontiguousarray(a[:, ts[c]])

    w_in = inp["ab_w_in"][0]
    s1 = 1024
    s2 = s1 + 512
    s3 = s2 + 512
    s4 = s3 + 1024
    s5_ = s4 + 16
    kbA = _prog(("mix", D, NT), lambda: build_mix(D, NT))
    insA = []
    for c in range(NCORES):
        h = c // 2
        sl = lambda a, b: wslab(w_in[:, a:b], KC, 1)[0]
        w5 = np.stack([sl(128 * c, 128 * c + 128), sl(s1 + 128 * h, s1 + 128 * h + 128), sl(s2 + 128 * h, s2 + 128 * h + 128),
                       sl(s5_ + 256 * h, s5_ + 256 * h + 128), sl(s5_ + 256 * h + 128, s5_ + 256 * h + 256)])
        wv = np.ascontiguousarray(w_in[:, s3 + 256 * h:s3 + 256 * h + 256].reshape(KC, 128, 256).transpose(1, 0, 2).reshape(128, KC * 256))
        wg = np.ascontiguousarray(w_in[:, s4:s4 + 16].reshape(KC, 128, 16).transpose(1, 0, 2).reshape(128, KC * 16))
        d = {"xT": xT, "w5": w5, "wv": wv, "wg": wg,
             "wg2": np.ascontiguousarray(inp["gla_w_gate2"][0][:, 128 * h:128 * h + 128]),
             "bg2": np.ascontiguousarray(inp["gla_b_gate2"][0][128 * h:128 * h + 128].reshape(128, 1)),
             "ng": np.ascontiguousarray(inp["gla_norm_g"][0].reshape(2, 128).T),
             "cmask": np.triu(np.ones((128, 128), np.float32)),
             "rmask": np.ascontiguousarray((np.arange(512) % 128 != 0).astype(np.float32)[None, :].repeat(128, 0)),
             "ones": ones512, "ident": ident}
        d.update(s5_host(inp["s5_lam_re"][0], inp["s5_lam_im"][0], inp["s5_log_step"][0], inp["s5_b_re"][0], inp["s5_b_im"][0],
                         inp["s5_c_re"][0], inp["s5_c_im"][0], inp["s5_d"][0], c))
        insA.append(d)
    rA = _run(kbA, insA)
    feat = np.concatenate([rA[c]["zT"] for c in range(NCORES)] + [rA[2 * h]["ybT"] for h in range(4)], 0)
    del rA, insA

    def post(feat, xin_sh, layer, wout, glu):
        kbB = _prog(("post", D, NTK, glu), lambda: build_post(D, NTK, glu, NE))
        wo = wslab(wout, KC, KC)
        wr = np.ascontiguousarray(inp["moe_w_router"][layer].reshape(KC, 128, NE).transpose(1, 0, 2).reshape(128, KC * NE))
        brb = np.ascontiguousarray(np.tile(inp["moe_b_router"][layer][None, :], (128, 1)))
        ins = []
        for c in range(NCORES):
            d = {"feat": sh(feat, c), "wout": wo, "xin": xin_sh[c], "lng": pp(inp["ln1_g"][layer], KC), "lnb": pp(inp["ln1_b"][layer], KC),
                 "ones": ones128, "wr": wr, "brb": brb}
            if glu:
                d["wglu"] = wslab(inp["s5_w_glu"][0], KC // 2, KC // 2)
                d["bglu"] = pp(inp["s5_b_glu"][0], KC // 2)
            ins.append(d)
        r = _run(kbB, ins)
        return [r[c]["xo"] for c in range(NCORES)], np.concatenate([r[c]["xob"] for c in range(NCORES)], 1), \
            np.concatenate([r[c]["gout"] for c in range(NCORES)], 0)

    def moe_ln(x1_sh, x1b, G, layer):
        kbM = _prog(("moe", D, F, NT, EL), lambda: build_moe(D, F, NT, EL, TB=1024))
        ins = [moe_host_inputs(x1b, G, inp["moe_w_gu"][layer], inp["moe_b_gu"][layer], inp["moe_w_down"][layer],
                               inp["moe_b_down"][layer], c, EL) for c in range(NCORES)]
        r = _run(kbM, ins)
        del ins
        parts = [r[c]["y"] for c in range(NCORES)]
        kbN = _prog(("sumln", D, NTK), lambda: build_sumln(D, NTK, NCORES))
        ins = [{"xin": x1_sh[c], "part": np.ascontiguousarray(np.stack([p[:, ts[c]] for p in parts])),
                "lng": pp(inp["ln2_g"][layer], KC), "lnb": pp(inp["ln2_b"][layer], KC), "ones": ones128} for c in range(NCORES)]
        r = _run(kbN, ins)
        return [r[c]["xo"] for c in range(NCORES)], np.concatenate([r[c]["xob"] for c in range(NCORES)], 1)

    x_sh = [sh(xT, c) for c in range(NCORES)]
    x1_sh, x1b, G = post(feat, x_sh, 0, inp["ab_w_out"][0], True)
    x2_sh, x2b = moe_ln(x1_sh, x1b, G, 0)

    NH = D // 128
    HPC = NH // NCORES
    wqkv = inp["c_w_qkv"][0].reshape(D, 3, NH, 128)
    kbC = _prog(("attn", D, NT, HPC), lambda: build_attn(D, NT, HPC))
    mbias = attn_mask_bias()
    identb = ident.astype(ml_dtypes.bfloat16)
    insC = []
    for c in range(NCORES):
        d = {"x2b": x2b, "mb": mbias, "ident": identb}
        for i, nm in enumerate(("wq", "wk", "wv")):
            w = np.ascontiguousarray(wqkv[:, i, c * HPC:(c + 1) * HPC, :].reshape(D, HPC * 128))
            d[nm] = wslab(w, KC, HPC)
        insC.append(d)
    rC = _run(kbC, insC)
    attn = np.concatenate([rC[c]["o"] for c in range(NCORES)], 1)
    feat1 = np.ascontiguousarray(attn.T)
    del rC, insC
    x3_sh, x3b, G1 = post(feat1, x2_sh, 1, inp["c_w_o"][0], False)
    x4_sh, _ = moe_ln(x3_sh, x3b, G1, 1)
    out = np.concatenate(x4_sh, 1)
    return np.ascontiguousarray(out.T)[None].astype(np.float32)
```
